# Optimizing a Trainium2 kernel written in Bass

```python
import jax, jax.numpy as jnp
from jax import lax
import numpy as np

D_MODEL = 1024
BATCH = 8
SEQ = 4096
DEPTH = 4

CTX_LEN = 256
GRID_W = 64
N_MIXERS = 2
EXPAND = 2
D_INNER = EXPAND * D_MODEL
MLSTM_HEADS = 4
MLSTM_HEAD_DIM = D_INNER // MLSTM_HEADS
CHUNK = 64
CONV_K = 3
FOURIER_GROUPS = 8
FOURIER_GROUP_DIM = D_INNER // FOURIER_GROUPS
N_MLSTM_LAYERS = (DEPTH + 1) // 2
N_FOURIER_LAYERS = DEPTH // 2
LAST_MLSTM = ((DEPTH - 1) // N_MIXERS) * N_MIXERS
EPS = 1e-6

kernel_name = "mlstm_fourier_hybrid_dit_prefix"

H = MLSTM_HEADS
DH = MLSTM_HEAD_DIM


def rmsnorm(x, g):
    xf = x.astype(jnp.float32)
    y = xf * lax.rsqrt(jnp.mean(xf * xf, axis=-1, keepdims=True) + EPS)
    return (y * g.astype(jnp.float32)).astype(x.dtype)


def mlstm_chunk_scan(q, k, v, li, lf, state, emit):
    b, h, n, d = q.shape
    nc = n // CHUNK

    def to_chunks(a):
        a = a.reshape(b, h, nc, CHUNK, *a.shape[3:])
        return jnp.moveaxis(a, 2, 0)

    lower = jnp.tril(jnp.ones((CHUNK, CHUNK), dtype=bool))

    def step(carry, inp):
        C, nv, m = carry
        qc, kc, vc, lic, lfc = inp
        bcum = jnp.cumsum(lfc, axis=-1)
        logw = bcum[..., :, None] - bcum[..., None, :] + lic[..., None, :]
        logw = jnp.where(lower, logw, -jnp.inf)
        log_prev = bcum + m[..., None]
        m_new = jnp.maximum(log_prev, jnp.max(logw, axis=-1))
        w_intra = jnp.exp(logw - m_new[..., None])
        w_prev = jnp.exp(log_prev - m_new)
        out = None
        if emit:
            scores = jnp.einsum('bhjd,bhsd->bhjs', qc, kc) * w_intra
            num = jnp.einsum('bhjs,bhsd->bhjd', scores, vc) + \
                w_prev[..., None] * jnp.einsum('bhvk,bhjk->bhjv', C, qc)
            den = jnp.sum(scores, axis=-1) + w_prev * jnp.einsum('bhk,bhjk->bhj', nv, qc)
            out = num / jnp.maximum(jnp.abs(den), jnp.exp(-m_new))[..., None]
        m_end = m_new[..., -1]
        w_src = jnp.exp(bcum[..., -1:] - bcum + lic - m_end[..., None])
        decay = jnp.exp(bcum[..., -1] + m - m_end)
        C_new = decay[..., None, None] * C + jnp.einsum('bhsv,bhsk->bhvk', w_src[..., None] * vc, kc)
        n_new = decay[..., None] * nv + jnp.einsum('bhs,bhsk->bhk', w_src, kc)
        return (C_new, n_new, m_end), out

    state, hs = lax.scan(step, state, tuple(to_chunks(a) for a in (q, k, v, li, lf)))
    if emit:
        hs = jnp.moveaxis(hs, 0, 2).reshape(b, h, n, d)
    return hs, state


def mlstm_inputs(hmod, grid_rows, grid_cols, w_in, conv_w, conv_b, w_q, w_k, w_if, b_i, b_f):
    b, n, _ = hmod.shape
    xm, o_pre, z = jnp.split(hmod @ w_in, 3, axis=-1)
    xg = xm.reshape(b, grid_rows, grid_cols, D_INNER)
    xconv = lax.conv_general_dilated(xg, conv_w, (1, 1), 'SAME',
                                     dimension_numbers=('NHWC', 'HWIO', 'NHWC'),
                                     feature_group_count=D_INNER)
    xconv = jax.nn.silu(xconv.reshape(b, n, D_INNER) + conv_b)
    xh = xconv.reshape(b, n, H, DH)
    q = jnp.einsum('bnhd,hde->bhne', xh, w_q).astype(jnp.float32)
    k = (jnp.einsum('bnhd,hde->bhne', xh, w_k) * (DH ** -0.5)).astype(jnp.float32)
    v = xm.reshape(b, n, H, DH).transpose(0, 2, 1, 3).astype(jnp.float32)
    wq_g = w_if[:, 0].reshape(2, H, DH, 2 * H).astype(jnp.float32)
    wk_g = w_if[:, 1].reshape(2, H, DH, 2 * H).astype(jnp.float32)
    wv_g = w_if[:, 2].reshape(2, H, DH, 2 * H).astype(jnp.float32)
    gates = (jnp.einsum('bhne,rheg->rbng', q, wq_g) + jnp.einsum('bhne,rheg->rbng', k, wk_g)
             + jnp.einsum('bhne,rheg->rbng', v, wv_g))
    li = gates[..., :H] + b_i.astype(jnp.float32)[:, None, None, :]
    lf = jax.nn.log_sigmoid(gates[..., H:] + b_f.astype(jnp.float32)[:, None, None, :])
    li = jnp.moveaxis(li, -1, 2)
    lf = jnp.moveaxis(lf, -1, 2)
    return (q, k, v, li, lf), o_pre, z, xconv


def mlstm_output(hsum, o_pre, z, xconv, ln_w, skip, w_out, dtype):
    b, _, n, _ = hsum.shape
    hh = hsum.transpose(0, 2, 1, 3)
    mu = jnp.mean(hh, axis=-1, keepdims=True)
    var = jnp.mean(jnp.square(hh - mu), axis=-1, keepdims=True)
    hn = ((hh - mu) * lax.rsqrt(var + EPS)).reshape(b, n, D_INNER) * ln_w.astype(jnp.float32)
    hn = jax.nn.sigmoid(o_pre.astype(jnp.float32)) * hn
    y = (hn.astype(dtype) + skip * xconv) * jax.nn.silu(z)
    return y @ w_out


def mlstm_branch(h_lat, h_ctx, rows, emit_ctx, w_in, conv_w, conv_b, w_q, w_k, w_if,
                 b_i, b_f, ln_w, skip, w_out):
    lat, o_l, z_l, xc_l = mlstm_inputs(h_lat, rows, GRID_W, w_in, conv_w, conv_b, w_q, w_k, w_if, b_i, b_f)
    ctx, o_c, z_c, xc_c = mlstm_inputs(h_ctx, 1, h_ctx.shape[1], w_in, conv_w, conv_b, w_q, w_k, w_if, b_i, b_f)
    b = h_lat.shape[0]
    zero = (jnp.zeros((b, H, DH, DH), jnp.float32), jnp.zeros((b, H, DH), jnp.float32),
            jnp.zeros((b, H), jnp.float32))
    h_lat_sum = 0.0
    h_ctx_sum = 0.0
    for r, rev in ((0, False), (1, True)):
        fl = (lambda a: jnp.flip(a, 2)) if rev else (lambda a: a)
        qc, kc, vc, lic, lfc = ctx
        ql, kl, vl, lil, lfl = lat
        hc, st = mlstm_chunk_scan(fl(qc), fl(kc), fl(vc), fl(lic[r]), fl(lfc[r]), zero, emit_ctx)
        hl, _ = mlstm_chunk_scan(fl(ql), fl(kl), fl(vl), fl(lil[r]), fl(lfl[r]), st, True)
        h_lat_sum = h_lat_sum + fl(hl)
        if emit_ctx:
            h_ctx_sum = h_ctx_sum + fl(hc)
    y_lat = mlstm_output(h_lat_sum, o_l, z_l, xc_l, ln_w, skip, w_out, h_lat.dtype)
    y_ctx = mlstm_output(h_ctx_sum, o_c, z_c, xc_c, ln_w, skip, w_out, h_ctx.dtype) if emit_ctx else None
    return y_lat, y_ctx


def fourier_branch(hmod, w_in, w_out):
    b, n, _ = hmod.shape
    u, z = jnp.split(hmod @ w_in, 2, axis=-1)
    ug = u.astype(jnp.float32).reshape(b, n, FOURIER_GROUPS, FOURIER_GROUP_DIM)
    f = jnp.fft.fft2(ug, axes=(1, 3), norm='ortho').real
    y = f.reshape(b, n, D_INNER).astype(hmod.dtype) * jax.nn.silu(z)
    return y @ w_out


def setup_inputs(seed: int = 0) -> dict:
    key = jax.random.key(seed)
    ks = jax.random.split(key, 24)
    nA, nB, E = N_MLSTM_LAYERS, N_FOURIER_LAYERS, D_INNER
    nrm = jax.random.normal
    return {
        "x": nrm(ks[0], (BATCH, SEQ, D_MODEL), jnp.float32),
        "c": nrm(ks[1], (BATCH, D_MODEL), jnp.float32),
        "ctx": nrm(ks[2], (BATCH, CTX_LEN, D_MODEL), jnp.float32),
        "c_ctx": nrm(ks[3], (D_MODEL,), jnp.float32),
        "norm_g": 1.0 + 0.1 * nrm(ks[4], (DEPTH, D_MODEL), jnp.float32),
        "w_ada": nrm(ks[5], (DEPTH, D_MODEL, 3 * D_MODEL), jnp.float32) * D_MODEL ** -0.5,
        "b_ada": 0.02 * nrm(ks[6], (DEPTH, 3 * D_MODEL), jnp.float32),
        "m_w_in": nrm(ks[7], (nA, D_MODEL, 3 * E), jnp.float32) * D_MODEL ** -0.5,
        "m_conv_w": nrm(ks[8], (nA, CONV_K, CONV_K, 1, E), jnp.float32) / CONV_K,
        "m_conv_b": 0.02 * nrm(ks[9], (nA, E), jnp.float32),
        "m_w_q": nrm(ks[10], (nA, H, DH, DH), jnp.float32) * DH ** -0.5,
        "m_w_k": nrm(ks[11], (nA, H, DH, DH), jnp.float32) * DH ** -0.5,
        "m_w_if": nrm(ks[12], (nA, 2, 3, E, 2 * H), jnp.float32) * (3 * E) ** -0.5,
        "m_b_i": 0.1 * nrm(ks[13], (nA, 2, H), jnp.float32),
        "m_b_f": jnp.linspace(3.0, 6.0, H, dtype=jnp.float32) + 0.1 * nrm(ks[14], (nA, 2, H), jnp.float32),
        "m_ln_w": 1.0 + 0.1 * nrm(ks[15], (nA, E), jnp.float32),
        "m_skip": 1.0 + 0.1 * nrm(ks[16], (nA, E), jnp.float32),
        "m_w_out": nrm(ks[17], (nA, E, D_MODEL), jnp.float32) * E ** -0.5,
        "f_w_in": nrm(ks[18], (nB, D_MODEL, 2 * E), jnp.float32) * D_MODEL ** -0.5,
        "f_w_out": nrm(ks[19], (nB, E, D_MODEL), jnp.float32) * E ** -0.5,
        "norm_f": 1.0 + 0.1 * nrm(ks[20], (D_MODEL,), jnp.float32),
    }


def reference(x, c, ctx, c_ctx, norm_g, w_ada, b_ada, m_w_in, m_conv_w, m_conv_b, m_w_q, m_w_k,
              m_w_if, m_b_i, m_b_f, m_ln_w, m_skip, m_w_out, f_w_in, f_w_out, norm_f):
    rows = x.shape[1] // GRID_W
    xc = ctx
    sc_lat = jax.nn.silu(c)
    sc_ctx = jax.nn.silu(c_ctx)
    for i in range(DEPTH):
        j = i // N_MIXERS
        update_ctx = i < LAST_MLSTM
        sh, sc, g = jnp.split(sc_lat @ w_ada[i] + b_ada[i], 3, axis=-1)
        h = rmsnorm(x, norm_g[i]) * (1.0 + sc[:, None, :]) + sh[:, None, :]
        need_ctx_in = update_ctx or (i % N_MIXERS == 0)
        if need_ctx_in:
            shc, scc, gc = jnp.split(sc_ctx @ w_ada[i] + b_ada[i], 3, axis=-1)
            hc = rmsnorm(xc, norm_g[i]) * (1.0 + scc) + shc
        if i % N_MIXERS == 0:
            y, yc = mlstm_branch(h, hc, rows, update_ctx, m_w_in[j], m_conv_w[j], m_conv_b[j],
                                 m_w_q[j], m_w_k[j], m_w_if[j], m_b_i[j], m_b_f[j],
                                 m_ln_w[j], m_skip[j], m_w_out[j])
        else:
            y = fourier_branch(h, f_w_in[j], f_w_out[j])
            yc = fourier_branch(hc, f_w_in[j], f_w_out[j]) if update_ctx else None
        x = x + g[:, None, :] * y
        if update_ctx:
            xc = xc + gc * yc
    return rmsnorm(x, norm_f)
```

```python
import math
from contextlib import ExitStack

import numpy as np
import ml_dtypes
import concourse.bass as bass
import concourse.mybir as mybir
from concourse.bass_utils import run_bass_kernel_spmd

F32 = mybir.dt.float32
BF16 = mybir.dt.bfloat16
AF = mybir.ActivationFunctionType
ALU = mybir.AluOpType
AX = mybir.AxisListType

D = 1024
E = 2048
NH = 4
DH = 512
CTX = 256
GW = 64
EPS = 1e-6
NDS = 12


class Buf:
    def __init__(self, ap, name=""):
        self.ap = ap
        self.name = name
        self.ws = []
        self.r = []
        self.prev = []
        self.excl = False

    def begin(self):
        self.prev = self.ws + self.r
        self.ws = []
        self.r = []
        return self

    def __getitem__(self, k):
        return self.ap[k]


class Prog:
    CE = ("pe", "dve", "act", "pool")
    DQ = ("sp", "act", "pool")

    def __init__(self, nc, es):
        self.nc = nc
        self.es = es
        self.q = {e: [] for e in ("pe", "dve", "act", "pool", "sp")}
        self.sems = {}
        self.cnt = {}
        for e in self.CE:
            self.sems[e] = es.enter_context(nc.semaphore("s_" + e))
            self.cnt[e] = 0
        self.dq_i = {}
        for qn in self.DQ:
            self.dq_i[qn] = 0
            for i in range(NDS):
                k = ("d", qn, i)
                self.sems[k] = es.enter_context(nc.semaphore("d_%s%d" % (qn, i)))
                self.cnt[k] = 0
        self.waited = {e: {} for e in self.q}
        self.n_ops = 0

    def _needed(self, eng, toks):
        out = []
        w = self.waited[eng]
        best = {}
        for t in toks:
            if t is None:
                continue
            k, v = t
            if k == eng and eng == "pe":
                continue
            if w.get(k, 0) >= v:
                continue
            if best.get(k, 0) < v:
                best[k] = v
        for k, v in best.items():
            w[k] = v
            out.append((k, v))
        return out

    def op(self, eng, fn, reads=(), writes=(), extra=(), pw=()):
        toks = list(extra)
        for b in pw:
            toks.extend(b.prev)
        for b in reads:
            toks.extend(b.ws)
            if b.excl:
                toks.extend(t for t in b.r if t[0] != eng)
        for b in writes:
            toks.extend(b.ws)
            toks.extend(b.r)
        waits = self._needed(eng, toks)
        self.cnt[eng] += 1
        tok = (eng, self.cnt[eng])
        self.q[eng].append((waits, fn, eng, 1))
        for b in reads:
            b.r.append(tok)
        for b in writes:
            b.ws = [tok]
            b.r = []
        for b in pw:
            b.ws.append(tok)
        self.n_ops += 1
        return tok

    def dma(self, qn, out_ap, in_ap, reads=(), writes=(), extra=(), also=(), **kw):
        i = self.dq_i[qn] % NDS
        self.dq_i[qn] += 1
        k = ("d", qn, i)
        toks = list(extra)
        if self.cnt[k] > 0:
            toks.append((k, self.cnt[k]))
        for b in reads:
            toks.extend(b.ws)
        for b in writes:
            toks.extend(b.ws)
            toks.extend(b.r)
        waits = self._needed(qn, toks)
        self.cnt[k] += 16
        tok = (k, self.cnt[k])

        def fn(e, out_ap=out_ap, in_ap=in_ap, kw=kw):
            return e.dma_start(out=out_ap, in_=in_ap, **kw)

        self.q[qn].append((waits, fn, k, 16))
        for b in reads:
            b.r.append(tok)
        for b in writes:
            b.ws = [tok]
            b.r = []
        for b in also:
            b.ws.append(tok)
        return tok

    def wait_all(self, eng, toks):
        waits = self._needed(eng, toks)
        self.q[eng].append((waits, None, None, 0))

    def cut(self):
        for name in self.q:
            self.q[name].append("CUT")

    def replay(self):
        nc = self.nc
        sems = self.sems
        segs = {}
        nseg = 0
        for name, items in self.q.items():
            cur = []
            lst = [cur]
            for it in items:
                if it == "CUT":
                    cur = []
                    lst.append(cur)
                else:
                    cur.append(it)
            segs[name] = lst
            nseg = max(nseg, len(lst))
        for si in range(nseg):
            if not any(len(segs[n][si]) for n in segs if si < len(segs[n])):
                continue
            with nc.Block() as block:
                def run(eng, name, si=si):
                    for waits, fn, sk, inc in segs[name][si]:
                        for k, v in waits:
                            eng.wait_ge(sems[k], v)
                        if fn is not None:
                            ins = fn(eng)
                            ins.then_inc(sems[sk], inc)

                @block.tensor
                def _(e):
                    run(e, "pe")

                @block.vector
                def _(e):
                    run(e, "dve")

                @block.scalar
                def _(e):
                    run(e, "act")

                @block.gpsimd
                def _(e):
                    run(e, "pool")

                @block.sync
                def _(e):
                    run(e, "sp")


class Pool:
    def __init__(self, bufs):
        self.bufs = bufs
        self.i = 0

    def next(self):
        b = self.bufs[self.i % len(self.bufs)]
        self.i += 1
        return b


class Arena:
    def __init__(self, ap):
        self.ap = ap
        self.top = 0
        self.W = ap.shape[1]
        self.peak = 0

    def alloc(self, name, free_shape, dt):
        n = 1
        for s in free_shape:
            n *= s
        esz = 4 if dt == F32 else 2
        words = (n * esz + 3) // 4
        words = (words + 7) // 8 * 8
        a = self.top
        self.top += words
        self.peak = max(self.peak, self.top)
        assert self.top <= self.W, "SBUF arena overflow at %s: %d > %d" % (name, self.top, self.W)
        v = self.ap[:, a:a + words]
        if dt != F32:
            v = v.bitcast(dt)
        v = v[:, 0:n]
        if len(free_shape) == 2:
            v = v.rearrange("p (a b) -> p a b", a=free_shape[0])
        elif len(free_shape) == 3:
            v = v.rearrange("p (a b c) -> p a b c", a=free_shape[0], b=free_shape[1])
        elif len(free_shape) == 4:
            v = v.rearrange("p (a b c d) -> p a b c d", a=free_shape[0], b=free_shape[1], c=free_shape[2])
        return Buf(v, name)

    def mark(self):
        return self.top

    def release(self, m):
        self.top = m


class K:
    pass


def fourier_tile_rows(dr, t):
    v = dr.rearrange("(n1 n2) d -> n2 n1 d", n2=GW)
    return [(0, 64, v[2 * t]), (64, 128, v[2 * t + 1])]


def natural_tile_rows(dr, t):
    return [(0, 128, dr[t * 128:(t + 1) * 128, :])]


def _prod(s):
    n = 1
    for v in s:
        n *= v
    return n


class KB:
    def __init__(self, N=4096, layers=(0, 1, 2, 3), final=True, dbg=()):
        self.N = N
        self.T = CTX + N
        self.layers = tuple(layers)
        self.final = final
        self.dbg = tuple(dbg)
        self.nc = bass.Bass("TRN2", target_bir_lowering=False)
        self.din = {}
        self.es = ExitStack()

    def inp(self, name, shape, dt=F32):
        t = self.nc.dram_tensor(name, list(shape), dt, kind="ExternalInput").ap()
        self.din[name] = t
        return t

    def outp(self, name, shape, dt=F32):
        return self.nc.dram_tensor(name, list(shape), dt, kind="ExternalOutput").ap()

    def scratch(self, name, shape, dt):
        kind = "ExternalOutput" if name in self.dbg else "Internal"
        return self.nc.dram_tensor(name, list(shape), dt, kind=kind).ap()

    def dbuf(self, key):
        b = self.dbufs.get(key)
        if b is None:
            b = Buf(None, str(key))
            self.dbufs[key] = b
        return b

    def barrier(self):
        P = self.P
        toks = [(k, v) for k, v in P.cnt.items() if v > 0]
        for e in ("pe", "dve", "act", "pool", "sp"):
            P.wait_all(e, toks)
        P.cut()

    def build(self):
        nc, es = self.nc, self.es
        N, T = self.N, self.T
        self.x_in = self.inp("x", [N, D])
        self.ctx_in = self.inp("ctx", [CTX, D])
        self.cvec = self.inp("cvec", [128, 8, 2])
        self.norm_g = self.inp("norm_g", [4, D])
        self.w_ada = self.inp("w_ada", [4, 128, 8, 3 * D])
        self.b_ada = self.inp("b_ada", [4, 3 * D])
        self.f_w_in = self.inp("f_w_in", [2, 128, 8, 2 * E])
        self.f_w_out = self.inp("f_w_out", [2, 128, 16, D])
        self.norm_f = self.inp("norm_f", [1, D])
        self.identb = self.inp("identb", [128, 128], BF16)
        self.c_cs = self.inp("c_cs", [128, 2, 512], BF16)
        self.c_a = self.inp("c_a", [128, 3, 128], BF16)
        self.c_bk = self.inp("c_bk", [128, 64, 64], BF16)
        self.c_d256 = self.inp("c_d256", [128, 2, 2, 256], BF16)
        self.declare_mlstm_inputs()
        self.out = self.outp("out", [N, D])
        self.xs = self.scratch("xs", [N, D], F32)
        self.xcs = self.scratch("xcs", [CTX, D], F32)
        self.dbufs = {}
        self.scr = {}
        with es:
            self.P = Prog(nc, es)
            sb_all = es.enter_context(nc.sbuf_tensor("sb_all", [128, 50 * 1024], F32))
            self.A = Arena(sb_all)
            self.psum = es.enter_context(nc.psum_tensor("ps_all", [128, 8 * 512], F32))
            self.pbank = [Buf(self.psum[:, b * 512:(b + 1) * 512], "bank%d" % b) for b in range(8)]
            for b_ in self.pbank:
                b_.excl = True
            A = self.A
            self.ident = A.alloc("ident", [128], BF16)
            self.P.dma("sp", self.ident[:], self.identb[:, :], writes=[self.ident])
            self.g_bc = A.alloc("g_bc", [D], F32)
            self.gc_bc = A.alloc("gc_bc", [D], F32)
            x_src, c_src = self.x_in, self.ctx_in
            for li in self.layers:
                upd_ctx = li < 2
                need_ctx = upd_ctx or (li % 2 == 0)
                fourier = (li % 2 == 1)
                mL = A.mark()
                if fourier:
                    self.fTc = A.alloc("fTc", [16, 256], F32)
                else:
                    self.G = A.alloc("G", [T // 128, 16], F32)
                    self.bif = A.alloc("bif", [16], F32)
                self.m_hT = A.mark()
                self.hT = A.alloc("hT", [8, T], BF16)
                m = A.mark()
                self.prep(li, x_src, c_src, need_ctx, fourier)
                self.barrier()
                A.release(m)
                if fourier:
                    self.fourier_layer(li, x_src, c_src, upd_ctx)
                else:
                    self.mlstm_layer(li, x_src, c_src, upd_ctx)
                self.barrier()
                A.release(mL)
                x_src = self.xs
                if upd_ctx:
                    c_src = self.xcs
            if self.final:
                self.final_norm(x_src)
            self.barrier()
            self.P.replay()
        return nc

    def declare_mlstm_inputs(self):
        pass

    def mlstm_layer(self, li, x_src, c_src, upd_ctx):
        raise NotImplementedError

    def tiles(self, fourier, need_ctx):
        out = []
        if need_ctx:
            for t in range(CTX // 128):
                out.append(("ctx", t, t * 128))
        for t in range(self.N // 128):
            out.append(("lat", t, CTX + t * 128))
        return out

    def tile_rows(self, stream, t, fourier, dr):
        if stream == "lat" and fourier:
            return fourier_tile_rows(dr, t)
        return natural_tile_rows(dr, t)

    def load_rows(self, q, buf, stream, t, fourier, dr, key):
        P = self.P
        toks = []
        db = self.dbuf((key, stream, t))
        first = True
        for (p0, p1, ap) in self.tile_rows(stream, t, fourier, dr):
            if first:
                tok = P.dma(q, buf.ap[p0:p1], ap, reads=[db], writes=[buf])
                first = False
            else:
                tok = P.dma(q, buf.ap[p0:p1], ap, reads=[db], also=[buf])
            toks.append(tok)
        return toks

    def store_rows(self, q, buf, stream, t, fourier, dr, key, extra=()):
        P = self.P
        db = self.dbuf((key, stream, t))
        toks = []
        for (p0, p1, ap) in self.tile_rows(stream, t, fourier, dr):
            toks.append(P.dma(q, ap, buf.ap[p0:p1], reads=[buf], writes=[db], extra=extra))
        return toks

    def prep(self, li, x_src, c_src, need_ctx, fourier):
        P, A = self.P, self.A
        cv = A.alloc("cv", [8, 2], F32)
        sv = A.alloc("sv", [8, 2], F32)
        ones = A.alloc("ones", [128], F32)
        sbc = A.alloc("sbc", [2, 8, 128], F32)
        mod = [A.alloc("mod%d" % s, [3 * D], F32) for s in range(2)]
        bbc = A.alloc("bbc", [3 * D], F32)
        gn = A.alloc("gn", [D], F32)
        abc = [A.alloc("abc%d" % s, [D], F32) for s in range(2)]
        wst = Pool([A.alloc("wst%d" % i, [8, 256], F32) for i in range(2)])
        P.dma("sp", cv[:], self.cvec[:, :, :], writes=[cv])
        P.dma("sp", bbc[:], self.b_ada[li].partition_broadcast(128), writes=[bbc])
        P.dma("sp", gn[:], self.norm_g[li].partition_broadcast(128), writes=[gn])
        P.op("act", lambda e: e.activation(sv[:], cv[:], AF.Silu), reads=[cv], writes=[sv])
        P.op("pool", lambda e: e.memset(ones[:], 1.0), writes=[ones])
        nstream = 2 if need_ctx else 1

        def mk_sbc(e):
            for s in range(nstream):
                for kc in range(8):
                    ins = e.tensor_scalar(sbc[:, s, kc, :], ones[:], sv[:, kc, s:s + 1], None, ALU.mult)
            return ins
        P.op("dve", mk_sbc, reads=[ones, sv], writes=[sbc])
        pb = Pool([self.pbank[0], self.pbank[1]])
        for blk in range(12):
            w = wst.next()
            P.dma("sp", w[:], self.w_ada[li, :, :, blk * 256:(blk + 1) * 256], writes=[w])
            for s in range(nstream):
                pm = pb.next()

                def mm(e, s=s, w=w, pm=pm):
                    for kc in range(8):
                        ins = e.matmul(pm[:, 0:256], sbc[:, s, kc, :], w[:, kc, :], start=(kc == 0), stop=(kc == 7))
                    return ins
                P.op("pe", mm, reads=[sbc, w], writes=[pm])
                sl = slice(blk * 256, (blk + 1) * 256)
                P.op("dve", lambda e, s=s, pm=pm, sl=sl: e.tensor_tensor(mod[s][:, sl], pm[:, 0:256], bbc[:, sl], ALU.add),
                     reads=[pm, bbc], writes=[mod[s]])
        for s in range(nstream):
            P.op("dve", lambda e, s=s: e.scalar_tensor_tensor(abc[s][:], mod[s][:, D:2 * D], 1.0, gn[:], ALU.add, ALU.mult),
                 reads=[mod[s], gn], writes=[abc[s]])
        P.op("pool", lambda e: e.tensor_copy(self.g_bc[:], mod[0][:, 2 * D:3 * D]), reads=[mod[0]], writes=[self.g_bc])
        if need_ctx:
            P.op("pool", lambda e: e.tensor_copy(self.gc_bc[:], mod[1][:, 2 * D:3 * D]), reads=[mod[1]], writes=[self.gc_bc])
        xt_pool = Pool([A.alloc("xt%d" % i, [D], F32) for i in range(3)])
        junk = A.alloc("junk", [D], F32)
        t1_pool = Pool([A.alloc("t1_%d" % i, [D], F32) for i in range(2)])
        xn_pool = Pool([A.alloc("xn%d" % i, [D], BF16) for i in range(2)])
        st_pool = Pool([A.alloc("st%d" % i, [2], F32) for i in range(4)])
        pt_pool = Pool([self.pbank[2], self.pbank[3]])
        key = "xc" if c_src is self.xcs else "cin"
        keyx = "xs" if x_src is self.xs else "xin"
        self.hT.begin()
        for ti, (stream, t, pos0) in enumerate(self.tiles(fourier, need_ctx)):
            s = 1 if stream == "ctx" else 0
            dr = c_src if stream == "ctx" else x_src
            xt = xt_pool.next()
            self.load_rows("sp", xt, stream, t, fourier, dr, key if stream == "ctx" else keyx)
            st = st_pool.next()
            P.op("act", lambda e, xt=xt, st=st: e.activation(junk[:], xt[:], AF.Square, accum_out=st[:, 0:1]),
                 reads=[xt], writes=[junk, st])
            P.op("act", lambda e, st=st: e.activation(st[:, 1:2], st[:, 0:1], AF.Sqrt, bias=EPS, scale=1.0 / D),
                 reads=[st], writes=[st])
            P.op("dve", lambda e, st=st: e.reciprocal(st[:, 1:2], st[:, 1:2]), reads=[st], writes=[st])
            t1 = t1_pool.next()
            P.op("dve", lambda e, xt=xt, st=st, t1=t1, s=s: e.scalar_tensor_tensor(
                t1[:], xt[:], st[:, 1:2], abc[s][:], ALU.mult, ALU.mult), reads=[xt, st, abc[s]], writes=[t1])
            xn = xn_pool.next()
            P.op("pool", lambda e, t1=t1, xn=xn, s=s: e.tensor_tensor(xn[:], t1[:], mod[s][:, 0:D], ALU.add),
                 reads=[t1, mod[s]], writes=[xn])
            pt = pt_pool.next()
            ptv = pt.ap.bitcast(BF16)

            def tr(e, xn=xn, ptv=ptv):
                for c in range(8):
                    ins = e.transpose(ptv[:, c * 128:(c + 1) * 128], xn[:, c * 128:(c + 1) * 128], self.ident[:])
                return ins
            P.op("pe", tr, reads=[xn, self.ident], writes=[pt])
            eng = "act" if ti % 2 == 0 else "dve"
            hv = self.hT[:, :, pos0:pos0 + 128]
            pv = ptv[:, 0:1024].rearrange("p (c t) -> p c t", c=8)
            if eng == "act":
                P.op("act", lambda e, hv=hv, pv=pv: e.activation(hv, pv, AF.Copy), reads=[pt], pw=[self.hT])
            else:
                P.op("dve", lambda e, hv=hv, pv=pv: e.tensor_copy(hv, pv), reads=[pt], pw=[self.hT])

    def final_norm(self, x_src):
        P, A = self.P, self.A
        m = A.mark()
        nf = A.alloc("nf", [D], F32)
        P.dma("sp", nf[:], self.norm_f[0].partition_broadcast(128), writes=[nf])
        xt_pool = Pool([A.alloc("fxt%d" % i, [D], F32) for i in range(3)])
        yo_pool = Pool([A.alloc("fyo%d" % i, [D], F32) for i in range(3)])
        junk = A.alloc("fjunk", [D], F32)
        st_pool = Pool([A.alloc("fst%d" % i, [2], F32) for i in range(4)])
        keyx = "xs" if x_src is self.xs else "xin"
        last = []
        for t in range(self.N // 128):
            xt = xt_pool.next()
            self.load_rows("sp", xt, "lat", t, False, x_src, keyx)
            st = st_pool.next()
            P.op("act", lambda e, xt=xt, st=st: e.activation(junk[:], xt[:], AF.Square, accum_out=st[:, 0:1]),
                 reads=[xt], writes=[junk, st])
            P.op("act", lambda e, st=st: e.activation(st[:, 1:2], st[:, 0:1], AF.Sqrt, bias=EPS, scale=1.0 / D),
                 reads=[st], writes=[st])
            P.op("dve", lambda e, st=st: e.reciprocal(st[:, 1:2], st[:, 1:2]), reads=[st], writes=[st])
            yo = yo_pool.next()
            P.op("dve", lambda e, xt=xt, st=st, yo=yo: e.scalar_tensor_tensor(
                yo[:], xt[:], st[:, 1:2], nf[:], ALU.mult, ALU.mult), reads=[xt, st, nf], writes=[yo])
            last += self.store_rows("sp", yo, "lat", t, False, self.out, "out")
        A.release(m)


def _load_w(self, dst_view, src_ap, kc, ncols, stage_pool, engs=("pool",)):
    P = self.P
    piece = max(1, stage_pool.bufs[0].ap.shape[1] // kc)
    c0 = 0
    i = 0
    while c0 < ncols:
        w = min(piece, ncols - c0)
        st = stage_pool.next()
        sv = st.ap[:, 0:kc * w].rearrange("p (k n) -> p k n", k=kc)
        P.dma("sp", sv, src_ap[:, :, c0:c0 + w], writes=[st])
        eng = engs[i % len(engs)]
        dv = dst_view.ap[:, :, c0:c0 + w] if isinstance(dst_view, Buf) else dst_view[:, :, c0:c0 + w]
        if eng == "act":
            P.op("act", lambda e, dv=dv, sv=sv: e.activation(dv, sv, AF.Copy), reads=[st], pw=[self._lw_buf])
        else:
            P.op(eng, lambda e, dv=dv, sv=sv: e.tensor_copy(dv, sv), reads=[st], pw=[self._lw_buf])
        c0 += w
        i += 1


def load_w(self, dst_buf, src_ap, kc, ncols, stage_pool, engs=("pool",), view=None):
    self._lw_buf = dst_buf
    dst_buf.begin()
    _load_w(self, dst_buf if view is None else view, src_ap, kc, ncols, stage_pool, engs)


KB.load_w = load_w


def fourier_layer(self, li, x_src, c_src, upd_ctx):
    P, A = self.P, self.A
    hT = self.hT
    N, T = self.N, self.T
    j = li // 2
    NT = N // 128
    Y = self.scratch("Y%d" % li, [64, 2, 64, E], BF16)
    ZS = self.scratch("ZS%d" % li, [16, 128, T], BF16)
    pb = Pool(self.pbank)
    fTc = self.fTc
    m0 = A.mark()
    cs = A.alloc("cs", [2, 512], BF16)
    ca = A.alloc("ca", [3, 128], BF16)
    d256 = A.alloc("d256", [2, 2, 256], BF16)
    P.dma("sp", cs[:], self.c_cs[:, :, :], writes=[cs])
    P.dma("sp", ca[:], self.c_a[:, :, :], writes=[ca])
    P.dma("sp", d256[:], self.c_d256[:, :, :, :], writes=[d256])
    stage = Pool([A.alloc("wstg%d" % i, [2048], F32) for i in range(2)])
    wb_pool = Pool([A.alloc("wb%d" % i, [8, 512], BF16) for i in range(2)])
    uT = A.alloc("uT", [4, T], BF16)
    UT_pool = Pool([A.alloc("UT%d" % i, [2, 2, 256], BF16) for i in range(3)])
    UTc = A.alloc("UTc", [2, 2, 2, 256], BF16) if upd_ctx else None
    Ysb_pool = Pool([A.alloc("Ysb%d" % i, [2, 512], BF16) for i in range(3)])
    zsb_pool = Pool([A.alloc("zsb%d" % i, [4, 512], BF16) for i in range(2)])
    ttiles = []
    if upd_ctx:
        ttiles.append((0, 256))
    for i in range(N // 512):
        ttiles.append((CTX + i * 512, 512))
    ptiles = []
    if upd_ctx:
        ptiles += [("ctx", 0, 0), ("ctx", 1, 128)]
    ptiles += [("lat", t, CTX + t * 128) for t in range(NT)]
    ev = 0
    if upd_ctx:
        fTc.begin()
    for fb in range(4):
        wb = wb_pool.next()
        self.load_w(wb, self.f_w_in[j, :, :, fb * 512:(fb + 1) * 512], 8, 512, stage)
        uT.begin()
        for (pos, w) in ttiles:
            for cc in range(4):
                pm = pb.next()

                def mm(e, pm=pm, wb=wb, cc=cc, pos=pos, w=w):
                    for kc in range(8):
                        ins = e.matmul(pm[:, 0:w], wb[:, kc, cc * 128:(cc + 1) * 128], hT[:, kc, pos:pos + w],
                                       start=(kc == 0), stop=(kc == 7))
                    return ins
                P.op("pe", mm, reads=[wb, hT], writes=[pm])
                ev += 1
                if ev % 2 == 0:
                    P.op("act", lambda e, pm=pm, cc=cc, pos=pos, w=w: e.activation(uT[:, cc, pos:pos + w], pm[:, 0:w], AF.Copy),
                         reads=[pm], pw=[uT])
                else:
                    P.op("dve", lambda e, pm=pm, cc=cc, pos=pos, w=w: e.tensor_copy(uT[:, cc, pos:pos + w], pm[:, 0:w]),
                         reads=[pm], pw=[uT])
        if upd_ctx:
            UTc.begin()
        for (stream, t, pos) in ptiles:
            UT = UT_pool.next().begin() if stream == "lat" else None
            for g in range(2):
                pm = pb.next()

                def mm2(e, pm=pm, g=g, pos=pos):
                    for cc in range(2):
                        ins = e.matmul(pm[:, :], uT[:, 2 * g + cc, pos:pos + 128], cs[:, cc, :],
                                       start=(cc == 0), stop=(cc == 1))
                    return ins
                P.op("pe", mm2, reads=[uT, cs], writes=[pm])
                if stream == "lat":
                    ov = UT[:, :, g, :]
                    ob = UT
                else:
                    ov = UTc[:, t, :, g, :]
                    ob = UTc
                pv = pm.ap.rearrange("p (a b) -> p a b", a=2)
                ev += 1
                if ev % 2 == 0:
                    P.op("act", lambda e, ov=ov, pv=pv: e.activation(ov, pv, AF.Copy), reads=[pm], pw=[ob])
                else:
                    P.op("dve", lambda e, ov=ov, pv=pv: e.tensor_copy(ov, pv), reads=[pm], pw=[ob])
            if stream == "lat":
                p_r = pb.next()
                p_i = pb.next()
                uc = UT[:, 0, :, :].rearrange("p g f -> p (g f)")
                us = UT[:, 1, :, :].rearrange("p g f -> p (g f)")

                def mmA(e, p_r=p_r, p_i=p_i, uc=uc, us=us):
                    e.matmul(p_r[:, :], ca[:, 0, :], uc, start=True, stop=False)
                    e.matmul(p_r[:, :], ca[:, 1, :], us, start=False, stop=True)
                    e.matmul(p_i[:, :], ca[:, 1, :], uc, start=True, stop=False)
                    return e.matmul(p_i[:, :], ca[:, 2, :], us, start=False, stop=True)
                P.op("pe", mmA, reads=[UT, ca], writes=[p_r, p_i])
                Ysb = Ysb_pool.next().begin()
                P.op("act", lambda e, Ysb=Ysb, p_r=p_r: e.activation(Ysb[:, 0, :], p_r[:, :], AF.Copy), reads=[p_r], pw=[Ysb])
                P.op("dve", lambda e, Ysb=Ysb, p_i=p_i: e.tensor_copy(Ysb[:, 1, :], p_i[:, :]), reads=[p_i], pw=[Ysb])
                for jj in range(2):
                    P.dma("sp", Y[:, :, 2 * t + jj, fb * 512:(fb + 1) * 512], Ysb.ap[jj * 64:(jj + 1) * 64, :, :],
                          reads=[Ysb], writes=[self.dbuf(("Y", t, fb, jj))])
        if upd_ctx:
            for g in range(2):
                for half in range(2):
                    pm = pb.next()

                    def mmc(e, pm=pm, g=g, half=half):
                        n = 0
                        for tile in range(2):
                            for cs_ in range(2):
                                ins = e.matmul(pm[:, 0:256], UTc[:, tile, cs_, g, half * 128:(half + 1) * 128],
                                               d256[:, tile, cs_, :], start=(n == 0), stop=(n == 3))
                                n += 1
                        return ins
                    P.op("pe", mmc, reads=[UTc, d256], writes=[pm])
                    c = fb * 4 + g * 2 + half
                    P.op("dve", lambda e, pm=pm, c=c: e.tensor_copy(fTc[:, c, :], pm[:, 0:256]), reads=[pm], pw=[fTc])
        wz = wb_pool.next()
        self.load_w(wz, self.f_w_in[j, :, :, E + fb * 512:E + (fb + 1) * 512], 8, 512, stage)
        for (pos, w) in ttiles:
            zsb = zsb_pool.next().begin()
            for cc in range(4):
                pm = pb.next()

                def mmz(e, pm=pm, wz=wz, cc=cc, pos=pos, w=w):
                    for kc in range(8):
                        ins = e.matmul(pm[:, 0:w], wz[:, kc, cc * 128:(cc + 1) * 128], hT[:, kc, pos:pos + w],
                                       start=(kc == 0), stop=(kc == 7))
                    return ins
                P.op("pe", mmz, reads=[wz, hT], writes=[pm])
                P.op("act", lambda e, zsb=zsb, pm=pm, cc=cc, w=w: e.activation(zsb[:, cc, 0:w], pm[:, 0:w], AF.Silu),
                     reads=[pm], pw=[zsb])
            P.dma("sp", ZS[fb * 4:(fb + 1) * 4, :, pos:pos + w].rearrange("c p t -> p c t"), zsb.ap[:, :, 0:w],
                  reads=[zsb], writes=[self.dbuf(("ZS", fb, pos))])
    self.barrier()
    A.release(self.m_hT)
    wout = A.alloc("wout", [16, D], BF16)
    stage = Pool([A.alloc("wstg%d" % i, [1024], F32) for i in range(2)])
    self.load_w(wout, self.f_w_out[j], 16, D, stage, engs=("pool", "dve"))
    bk = A.alloc("bk", [64, 64], BF16)
    P.dma("sp", bk[:], self.c_bk[:, :, :], writes=[bk])
    zs_pool = Pool([A.alloc("zs%d" % i, [16, 512], BF16) for i in range(2)])
    Yk_pool = Pool([A.alloc("Yk%d" % i, [E], BF16) for i in range(9)])
    yT_pool = Pool([A.alloc("yT%d" % i, [16, 512], BF16) for i in range(2)])
    xt_pool = Pool([A.alloc("fx%d" % i, [D], F32) for i in range(2)])
    xn_pool = Pool([A.alloc("fxn%d" % i, [D], F32) for i in range(2)])
    keyx = "xs" if x_src is self.xs else "xin"

    def out_stage(yT, width_off, stream, t, gb, src, key_in, key_out, dst):
        xt = xt_pool.next()
        self.load_rows("sp", xt, stream, t, stream == "lat", src, key_in)
        xn = xn_pool.next().begin()
        for half in range(2):
            po = pb.next()

            def mmo(e, po=po, half=half):
                for c in range(16):
                    ins = e.matmul(po[:, :], yT[:, c, width_off:width_off + 128], wout[:, c, half * 512:(half + 1) * 512],
                                   start=(c == 0), stop=(c == 15))
                return ins
            P.op("pe", mmo, reads=[yT, wout], writes=[po])
            hs = slice(half * 512, (half + 1) * 512)
            P.op("dve", lambda e, po=po, hs=hs: e.tensor_tensor(xn[:, hs], po[:, :], gb[:, hs], ALU.mult),
                 reads=[po, gb], pw=[xn])
        P.op("pool", lambda e: e.tensor_tensor(xn[:], xn[:], xt[:], ALU.add), reads=[xt], writes=[xn])
        self.store_rows("sp", xn, stream, t, stream == "lat", dst, key_out)

    for st in range(N // 512):
        pos = CTX + st * 512
        zs = zs_pool.next()
        P.dma("sp", zs[:], ZS[:, :, pos:pos + 512].rearrange("c p t -> p c t"), writes=[zs])
        Yks = []
        for kk in range(8):
            k1 = st * 8 + kk
            Yk = Yk_pool.next()
            P.dma("sp", Yk[:], Y[k1].rearrange("r n f -> (r n) f"), writes=[Yk])
            Yks.append(Yk)
        yT = yT_pool.next().begin()
        for c in range(16):
            pf = pb.next()

            def mmf(e, pf=pf, c=c, Yks=Yks, st=st):
                for kk in range(8):
                    ins = e.matmul(pf[:, kk * 64:(kk + 1) * 64], Yks[kk][:, c * 128:(c + 1) * 128], bk[:, st * 8 + kk, :],
                                   start=True, stop=True)
                return ins
            P.op("pe", mmf, reads=Yks + [bk], writes=[pf])
            P.op("dve", lambda e, pf=pf, c=c, yT=yT, zs=zs: e.tensor_tensor(yT[:, c, :], pf[:, :], zs[:, c, :], ALU.mult),
                 reads=[pf, zs], pw=[yT])
        for i in range(4):
            out_stage(yT, i * 128, "lat", st * 4 + i, self.g_bc, x_src, keyx, "xs", self.xs)
    if upd_ctx:
        zs = zs_pool.next()
        P.dma("sp", zs.ap[:, :, 0:256], ZS[:, :, 0:256].rearrange("c p t -> p c t"), writes=[zs])
        yT = yT_pool.next()
        P.op("dve", lambda e: e.tensor_tensor(yT.ap[:, :, 0:256], fTc[:, :, :], zs.ap[:, :, 0:256], ALU.mult),
             reads=[fTc, zs], writes=[yT])
        keyc = "xc" if c_src is self.xcs else "cin"
        for t in range(2):
            out_stage(yT, t * 128, "ctx", t, self.gc_bc, c_src, keyc, "xc", self.xcs)


KB.fourier_layer = fourier_layer


def _bf(a):
    return np.ascontiguousarray(a.astype(np.float32)).astype(ml_dtypes.bfloat16)


def make_consts():
    c = {}
    c["identb"] = _bf(np.eye(128))
    p = np.arange(128)
    cc = np.arange(2)
    ch = (cc[None, :] * 128 + p[:, None])
    k = np.arange(256)
    ang = 2 * np.pi * ((ch[:, :, None] * k[None, None, :]) % 256) / 256.0
    c["c_cs"] = _bf(np.concatenate([np.cos(ang), np.sin(ang)], axis=-1) / 16.0)
    n1 = np.arange(64)
    a64 = 2 * np.pi * ((n1[:, None] * n1[None, :]) % 64) / 64.0
    C, S = np.cos(a64), np.sin(a64)
    Z = np.zeros((64, 64))
    bd = lambda M: np.block([[M, Z], [Z, M]])
    c["c_a"] = _bf(np.stack([bd(C), bd(-S), bd(-C)], axis=1))
    n2 = np.arange(64)[:, None, None]
    k1 = np.arange(64)[None, :, None]
    k2 = np.arange(64)[None, None, :]
    num = (n2 * k2 * 64 + n2 * k1) % 4096
    th = 2 * np.pi * num / 4096.0
    c["c_bk"] = _bf(np.concatenate([np.cos(th), np.sin(th)], axis=0) / 64.0)
    n = (np.arange(2)[None, :] * 128 + p[:, None])
    a256 = 2 * np.pi * ((n[:, :, None] * k[None, None, :]) % 256) / 256.0
    c["c_d256"] = _bf(np.stack([np.cos(a256), -np.sin(a256)], axis=2) / 16.0)
    return c


def _pk(w, kc):
    n = w.shape[-1]
    return np.ascontiguousarray(w.reshape(kc, 128, n).transpose(1, 0, 2))


def host_shared(inp):
    f = lambda a: np.asarray(a, dtype=np.float32)
    sh = {}
    sh["norm_g"] = f(inp["norm_g"])
    sh["w_ada"] = np.stack([_pk(f(inp["w_ada"][l]), 8) for l in range(4)])
    sh["b_ada"] = f(inp["b_ada"])
    sh["f_w_in"] = np.stack([_pk(f(inp["f_w_in"][l]), 8) for l in range(2)])
    sh["f_w_out"] = np.stack([_pk(f(inp["f_w_out"][l]), 16) for l in range(2)])
    sh["norm_f"] = f(inp["norm_f"]).reshape(1, D)
    sh.update(make_consts())
    sh.update(host_shared_mlstm(inp))
    return sh


def host_shared_mlstm(inp):
    return {}


def host_core(inp, b):
    f = lambda a: np.asarray(a, dtype=np.float32)
    d = {}
    d["x"] = np.ascontiguousarray(f(inp["x"][b]))
    d["ctx"] = np.ascontiguousarray(f(inp["ctx"][b]))
    cv = np.stack([f(inp["c"][b]), f(inp["c_ctx"])], axis=-1)
    d["cvec"] = np.ascontiguousarray(cv.reshape(8, 128, 2).transpose(1, 0, 2))
    return d


def declare_mlstm_inputs(self):
    self.m_w_in = self.inp("m_w_in", [2, 128, 8, 3 * E])
    self.m_conv_w = self.inp("m_conv_w", [2, 128, 16, 9])
    self.m_vecs = self.inp("m_vecs", [2, 128, 3, 16])
    self.m_w_q = self.inp("m_w_q", [2, 4, 128, 4, DH])
    self.m_w_k = self.inp("m_w_k", [2, 4, 128, 4, DH])
    self.m_w_if = self.inp("m_w_if", [2, 128, 3, 16, 16])
    self.m_bif = self.inp("m_bif", [2, 16])
    self.m_w_out = self.inp("m_w_out", [2, 128, 16, D])
    self.c_tri = self.inp("c_tri", [128, 3, 128])
    self.c_mask = self.inp("c_mask", [128, 2, 128])


KB.declare_mlstm_inputs = declare_mlstm_inputs


def host_shared_mlstm(inp):
    f = lambda a: np.asarray(a, dtype=np.float32)
    sh = {}
    sh["m_w_in"] = np.stack([_pk(f(inp["m_w_in"][l]), 8) for l in range(2)])
    cw = f(inp["m_conv_w"]).reshape(2, 9, E)
    sh["m_conv_w"] = np.ascontiguousarray(cw.reshape(2, 9, 16, 128).transpose(0, 3, 2, 1))
    vec = np.stack([f(inp["m_conv_b"]), f(inp["m_ln_w"]), f(inp["m_skip"])], axis=1)
    sh["m_vecs"] = np.ascontiguousarray(vec.reshape(2, 3, 16, 128).transpose(0, 3, 1, 2))
    sh["m_w_q"] = np.stack([np.stack([_pk(f(inp["m_w_q"][l][h]), 4) for h in range(4)]) for l in range(2)])
    sh["m_w_k"] = np.stack([np.stack([_pk(f(inp["m_w_k"][l][h]), 4) for h in range(4)]) for l in range(2)])
    wif = f(inp["m_w_if"])
    wif = wif.reshape(2, 2, 3, 16, 128, 8).transpose(0, 4, 2, 3, 1, 5)
    sh["m_w_if"] = np.ascontiguousarray(wif.reshape(2, 128, 3, 16, 16))
    bif = np.concatenate([f(inp["m_b_i"]), f(inp["m_b_f"])], axis=-1)
    sh["m_bif"] = np.ascontiguousarray(bif.reshape(2, 16))
    sh["m_w_out"] = np.stack([_pk(f(inp["m_w_out"][l]), 16) for l in range(2)])
    s_ = np.arange(128)[:, None]
    j_ = np.arange(128)[None, :]
    mf = (s_ <= j_).astype(np.float32)
    mb = (s_ >= j_).astype(np.float32)
    sh["c_tri"] = np.ascontiguousarray(np.stack([-mf, -mb, -np.ones((128, 128), np.float32)], axis=1))
    sh["c_mask"] = np.ascontiguousarray(np.stack([mf, mb], axis=1))
    return sh


def out_stage(self, yT, off, stream, t, fourier, gb, src, key_in, key_out, dst, wout, xt_pool, xn_pool, pb):
    P = self.P
    xt = xt_pool.next()
    self.load_rows("sp", xt, stream, t, fourier, src, key_in)
    xn = xn_pool.next().begin()
    for half in range(2):
        po = pb.next()

        def mmo(e, po=po, half=half):
            for c in range(16):
                ins = e.matmul(po[:, :], yT[:, c, off:off + 128], wout[:, c, half * 512:(half + 1) * 512],
                               start=(c == 0), stop=(c == 15))
            return ins
        P.op("pe", mmo, reads=[yT, wout], writes=[po])
        hs = slice(half * 512, (half + 1) * 512)
        P.op("dve", lambda e, po=po, hs=hs: e.tensor_tensor(xn[:, hs], po[:, :], gb[:, hs], ALU.mult),
             reads=[po, gb], pw=[xn])
    P.op("pool", lambda e: e.tensor_tensor(xn[:], xn[:], xt[:], ALU.add), reads=[xt], writes=[xn])
    self.store_rows("sp", xn, stream, t, fourier, dst, key_out)


KB.out_stage = out_stage


def mlstm_layer(self, li, x_src, c_src, upd_ctx):
    P, A = self.P, self.A
    hT = self.hT
    N, T = self.N, self.T
    R = N // GW
    j = li // 2
    NTT = T // 128
    pb = Pool(self.pbank)
    S = self.scr.get(j)
    if S is None:
        S = {}
        S["OZX"] = self.scratch("OZX%d" % j, [3, NTT, 128, 16, 128], BF16)
        S["QT"] = self.scratch("QT%d" % j, [NTT, 128, 4, 4, 128], BF16)
        S["KT"] = self.scratch("KT%d" % j, [NTT, 128, 4, 4, 128], BF16)
        S["Kt"] = self.scratch("Kt%d" % j, [T, E], BF16)
        S["Vt"] = self.scratch("Vt%d" % j, [T, E], BF16)
        S["HF"] = self.scratch("HF%d" % j, [T, E], BF16)
        self.scr[j] = S
    m0 = A.mark()
    G = self.G
    bif = self.bif
    P.dma("sp", bif[:], self.m_bif[j].partition_broadcast(128), writes=[bif])
    mB = A.mark()
    cw = A.alloc("cw", [16, 9], F32)
    vecs = A.alloc("vecs", [3, 16], F32)
    P.dma("sp", cw[:], self.m_conv_w[j], writes=[cw])
    P.dma("sp", vecs[:], self.m_vecs[j], writes=[vecs])
    wif32 = A.alloc("wif32", [3, 16, 16], F32)
    wif = A.alloc("wif", [3, 16, 16], BF16)
    P.dma("sp", wif32[:], self.m_w_if[j], writes=[wif32])
    P.op("pool", lambda e: e.tensor_copy(wif[:], wif32[:]), reads=[wif32], writes=[wif])
    stage = Pool([A.alloc("mstg%d" % i, [1024], F32) for i in range(2)])
    wb_pool = Pool([A.alloc("mwb%d" % i, [8, 512], BF16) for i in range(2)])
    wq = A.alloc("wq", [4, DH], BF16)
    wk = A.alloc("wk", [4, DH], BF16)
    LP = 260 + (R + 2) * 68
    xmpad = A.alloc("xmpad", [LP], BF16)
    vTc = A.alloc("vTc", [T], BF16)
    xcv = A.alloc("xcv", [4, T], BF16)
    dg_pool = Pool([A.alloc("dg%d" % i, [9, 128], BF16) for i in range(2)])
    tl_pool = Pool([A.alloc("tl%d" % i, [512], BF16) for i in range(4)])
    t32_pool = Pool([A.alloc("t32_%d" % i, [512], F32) for i in range(2)])
    qt_pool = Pool([A.alloc("qt%d" % i, [4, 512], BF16) for i in range(4)])
    xl = xmpad.ap[:, 260:LP].rearrange("p (r c) -> p r c", c=68)
    P.op("pool", lambda e: e.memset(xmpad[:], 0.0), writes=[xmpad])
    ttiles = [(0, 256)] + [(CTX + i * 512, 512) for i in range(N // 512)]
    first_g = [True] * NTT
    ev = [0]

    def evac(out_ap, in_ap, reads, pw=(), writes=(), func=None, scale=None, bias=None):
        ev[0] += 1
        if func is not None or ev[0] % 2 == 0:
            kw = {}
            if scale is not None:
                kw["scale"] = scale
            if bias is not None:
                kw["bias"] = bias
            f = func if func is not None else AF.Copy
            return P.op("act", lambda e: e.activation(out_ap, in_ap, f, **kw), reads=reads, pw=pw, writes=writes)
        if scale is not None:
            return P.op("dve", lambda e: e.tensor_scalar(out_ap, in_ap, scale, None, ALU.mult), reads=reads, pw=pw, writes=writes)
        return P.op("dve", lambda e: e.tensor_copy(out_ap, in_ap), reads=reads, pw=pw, writes=writes)

    def gate_acc(pm, tt):
        if first_g[tt]:
            first_g[tt] = False
            P.op("dve", lambda e: e.tensor_tensor(G[:, tt, :], pm[:, 0:16], bif[:], ALU.add), reads=[pm, bif], pw=[G])
        else:
            P.op("dve", lambda e: e.tensor_tensor(G[:, tt, :], pm[:, 0:16], G[:, tt, :], ALU.add), reads=[pm], pw=[G])

    G.begin()
    import os
    stop = os.environ.get("MK_STOP", "")
    for hd in range(4):
        if stop.startswith("S") and hd > 0:
            return
        self.load_w(wq, self.m_w_q[j, hd], 4, DH, stage)
        self.load_w(wk, self.m_w_k[j, hd], 4, DH, stage)
        wxm = wb_pool.next()
        self.load_w(wxm, self.m_w_in[j, :, :, hd * 512:(hd + 1) * 512], 8, 512, stage)
        xcv.begin()
        if stop == "S0":
            return
        for cc in range(4):
            c = hd * 4 + cc
            xmpad.begin()
            vTc.begin()
            for (pos, w) in ttiles:
                pm = pb.next()

                def mm(e, pm=pm, cc=cc, pos=pos, w=w, wxm=wxm):
                    for kc in range(8):
                        ins = e.matmul(pm[:, 0:w], wxm[:, kc, cc * 128:(cc + 1) * 128], hT[:, kc, pos:pos + w],
                                       start=(kc == 0), stop=(kc == 7))
                    return ins
                P.op("pe", mm, reads=[wxm, hT], writes=[pm])
                if pos == 0:
                    ov = xmpad.ap[:, 2:258]
                    iv = pm[:, 0:256]
                else:
                    r0 = (pos - CTX) // 64
                    ov = xl[:, r0 + 1:r0 + 9, 2:66]
                    iv = pm.ap.rearrange("p (r c) -> p r c", c=64)
                P.op("act", lambda e, ov=ov, iv=iv: e.activation(ov, iv, AF.Copy), reads=[pm], pw=[xmpad])
                P.op("dve", lambda e, pm=pm, pos=pos, w=w: e.tensor_copy(vTc[:, pos:pos + w], pm[:, 0:w]), reads=[pm], pw=[vTc])
            if stop == "S1":
                return
            for tt in range(NTT):
                pm = pb.next()
                P.op("pe", lambda e, pm=pm, tt=tt, c=c: e.matmul(pm[:, 0:16], vTc[:, tt * 128:(tt + 1) * 128], wif[:, 2, c, :],
                                                                start=True, stop=True), reads=[vTc, wif], writes=[pm])
                gate_acc(pm, tt)
            if stop == "S2":
                return
            dg = dg_pool.next()

            def mkdg(e, dg=dg, c=c):
                for tap in range(9):
                    ins = e.tensor_scalar(dg[:, tap, :], self.ident[:], cw[:, c, tap:tap + 1], None, ALU.mult)
                return ins
            P.op("dve", mkdg, reads=[self.ident, cw], writes=[dg])
            for (pos, w) in ttiles:
                pm = pb.next()
                if pos == 0:
                    def mmc(e, pm=pm, dg=dg):
                        for k, dc in enumerate((-1, 0, 1)):
                            ins = e.matmul(pm[:, 0:256], dg[:, 3 + k, :], xmpad.ap[:, 2 + dc:258 + dc],
                                           start=(k == 0), stop=(k == 2))
                        return ins
                else:
                    r0 = (pos - CTX) // 64

                    def mmc(e, pm=pm, dg=dg, r0=r0):
                        n = 0
                        for dr in (-1, 0, 1):
                            for dc in (-1, 0, 1):
                                ins = e.matmul(pm[:, :], dg[:, 3 * (dr + 1) + (dc + 1), :],
                                               xl[:, r0 + 1 + dr:r0 + 9 + dr, 2 + dc:66 + dc],
                                               start=(n == 0), stop=(n == 8))
                                n += 1
                        return ins
                P.op("pe", mmc, reads=[dg, xmpad], writes=[pm])
                P.op("act", lambda e, pm=pm, cc=cc, pos=pos, w=w, c=c: e.activation(
                    xcv[:, cc, pos:pos + w], pm[:, 0:w], AF.Silu, bias=vecs[:, 0, c:c + 1]), reads=[pm, vecs], pw=[xcv])
                if stop == "S3":
                    continue
                tl = tl_pool.next()
                P.op("pool", lambda e, tl=tl, cc=cc, pos=pos, w=w, c=c: e.tensor_scalar(
                    tl[:, 0:w], xcv[:, cc, pos:pos + w], vecs[:, 2, c:c + 1], None, ALU.mult), reads=[xcv, vecs], writes=[tl])
                tt0 = pos // 128
                nt = w // 128
                P.dma("sp", S["OZX"][2, tt0:tt0 + nt, :, c, :].rearrange("t p k -> p t k"),
                      tl.ap[:, 0:w].rearrange("p (t k) -> p t k", k=128), reads=[tl],
                      writes=[self.dbuf(("OZX", j, 2, c, pos))])
        if stop in ("S3", "S4"):
            return
        for tt in range(NTT):
            pm = pb.next()

            def mmv(e, pm=pm, tt=tt, wxm=wxm):
                for kc in range(8):
                    ins = e.matmul(pm[:, :], hT[:, kc, tt * 128:(tt + 1) * 128], wxm[:, kc, :],
                                   start=(kc == 0), stop=(kc == 7))
                return ins
            P.op("pe", mmv, reads=[wxm, hT], writes=[pm])
            tl = tl_pool.next()
            evac(tl[:, :], pm[:, :], [pm], writes=[tl])
            P.dma("sp", S["Vt"][tt * 128:(tt + 1) * 128, hd * 512:(hd + 1) * 512], tl[:, :], reads=[tl],
                  writes=[self.dbuf(("Vt", j, tt, hd))])
        for kind in range(2):
            wz = wb_pool.next()
            col0 = E * (kind + 1) + hd * 512
            self.load_w(wz, self.m_w_in[j, :, :, col0:col0 + 512], 8, 512, stage)
            for cc in range(4):
                c = hd * 4 + cc
                for (pos, w) in ttiles:
                    pm = pb.next()

                    def mm(e, pm=pm, cc=cc, pos=pos, w=w, wz=wz):
                        for kc in range(8):
                            ins = e.matmul(pm[:, 0:w], wz[:, kc, cc * 128:(cc + 1) * 128], hT[:, kc, pos:pos + w],
                                           start=(kc == 0), stop=(kc == 7))
                        return ins
                    P.op("pe", mm, reads=[wz, hT], writes=[pm])
                    tl = tl_pool.next()
                    if kind == 0:
                        t32 = t32_pool.next()
                        P.op("act", lambda e, t32=t32, pm=pm, w=w: e.activation(t32[:, 0:w], pm[:, 0:w], AF.Sigmoid),
                             reads=[pm], writes=[t32])
                        P.op("pool", lambda e, tl=tl, t32=t32, w=w, c=c: e.tensor_scalar(
                            tl[:, 0:w], t32[:, 0:w], vecs[:, 1, c:c + 1], None, ALU.mult), reads=[t32, vecs], writes=[tl])
                    else:
                        P.op("act", lambda e, tl=tl, pm=pm, w=w: e.activation(tl[:, 0:w], pm[:, 0:w], AF.Silu),
                             reads=[pm], writes=[tl])
                    tt0 = pos // 128
                    nt = w // 128
                    P.dma("sp", S["OZX"][kind, tt0:tt0 + nt, :, c, :].rearrange("t p k -> p t k"),
                          tl.ap[:, 0:w].rearrange("p (t k) -> p t k", k=128), reads=[tl],
                          writes=[self.dbuf(("OZX", j, kind, c, pos))])
        for (pos, w) in ttiles:
            tt0 = pos // 128
            nt = w // 128
            qk = []
            for which, wmat, scl in ((0, wq, None), (1, wk, DH ** -0.5)):
                qt = qt_pool.next().begin()
                for ec in range(4):
                    pm = pb.next()

                    def mmq(e, pm=pm, ec=ec, pos=pos, w=w, wmat=wmat):
                        for dc in range(4):
                            ins = e.matmul(pm[:, 0:w], wmat[:, dc, ec * 128:(ec + 1) * 128], xcv[:, dc, pos:pos + w],
                                           start=(dc == 0), stop=(dc == 3))
                        return ins
                    P.op("pe", mmq, reads=[wmat, xcv], writes=[pm])
                    evac(qt[:, ec, 0:w], pm[:, 0:w], [pm], pw=[qt], scale=scl)
                dst = S["QT"] if which == 0 else S["KT"]
                for ec in range(4):
                    P.dma("sp", dst[tt0:tt0 + nt, :, hd, ec, :].rearrange("t p k -> p t k"),
                          qt.ap[:, ec, 0:w].rearrange("p (t k) -> p t k", k=128), reads=[qt],
                          writes=[self.dbuf(("QK", j, which, hd, ec, pos))])
                qk.append(qt)
            qt, kt = qk
            for sub in range(nt):
                tt = tt0 + sub
                sl = slice(sub * 128, (sub + 1) * 128)
                pm = pb.next()

                def mmk(e, pm=pm, sl=sl, pos=pos):
                    for dc in range(4):
                        ins = e.matmul(pm[:, :], xcv[:, dc, pos + sl.start:pos + sl.stop], wk[:, dc, :],
                                       start=(dc == 0), stop=(dc == 3))
                    return ins
                P.op("pe", mmk, reads=[wk, xcv], writes=[pm])
                tl = tl_pool.next()
                evac(tl[:, :], pm[:, :], [pm], writes=[tl], scale=DH ** -0.5)
                P.dma("sp", S["Kt"][tt * 128:(tt + 1) * 128, hd * 512:(hd + 1) * 512], tl[:, :], reads=[tl],
                      writes=[self.dbuf(("Kt", j, tt, hd))])
                pg = pb.next()

                def mmg(e, pg=pg, sl=sl, qt=qt, kt=kt, hd=hd):
                    n = 0
                    for src, buf in ((0, qt), (1, kt)):
                        for ec in range(4):
                            ins = e.matmul(pg[:, 0:16], buf[:, ec, sl], wif[:, src, hd * 4 + ec, :],
                                           start=(n == 0), stop=(n == 7))
                            n += 1
                    return ins
                P.op("pe", mmg, reads=[qt, kt, wif], writes=[pg])
                gate_acc(pg, tt)
    self.barrier()
    A.release(self.m_hT)
    import os
    self.stop = os.environ.get("MK_STOP", "")
    if self.stop == "B":
        return
    self.mlstm_scan(li, j, S, G, x_src, c_src, upd_ctx, pb)


KB.mlstm_layer = mlstm_layer


def mlstm_scan(self, li, j, S, G, x_src, c_src, upd_ctx, pb):
    P, A = self.P, self.A
    N, T = self.N, self.T
    NTT = T // 128
    NG = NTT * 4
    tri = A.alloc("tri", [3, 128], F32)
    mask = A.alloc("mask", [2, 128], F32)
    P.dma("sp", tri[:], self.c_tri[:, :, :], writes=[tri])
    P.dma("sp", mask[:], self.c_mask[:, :, :], writes=[mask])
    SP = A.alloc("SP", [2, NTT, 4], F32)
    TA = A.alloc("TA", [2, NTT, 4], F32)
    AA = A.alloc("AA", [2, NTT, 4], F32)
    WW = A.alloc("WW", [2, NTT, 4], F32)
    ENB = A.alloc("ENB", [2, NTT, 4], F32)
    EBL = A.alloc("EBL", [2, NTT, 4], F32)
    Gv = G.ap.rearrange("p t (r g) -> p r t g", r=2)
    for b_ in (SP, TA, AA, WW, ENB, EBL):
        b_.begin()
    for r in range(2):
        fpre = Gv[:, r, :, 4:8]
        liv = Gv[:, r, :, 0:4]
        P.op("act", lambda e, r=r, fpre=fpre: e.activation(SP[:, r, :, :], fpre, AF.Exp, scale=-1.0), reads=[G], pw=[SP])
        P.op("act", lambda e, r=r: e.activation(SP[:, r, :, :], SP[:, r, :, :], AF.Ln, bias=1.0), reads=[SP], pw=[SP])
        pbm = pb.next()
        pbl = pb.next()
        spf = SP[:, r, :, :].rearrange("p t h -> p (t h)")
        P.op("pe", lambda e, pbm=pbm, spf=spf, r=r: e.matmul(pbm[:, 0:NG], tri[:, r, :], spf, start=True, stop=True),
             reads=[tri, SP], writes=[pbm])
        P.op("pe", lambda e, pbl=pbl, spf=spf: e.matmul(pbl[:, 0:NG], tri[:, 2, :], spf, start=True, stop=True),
             reads=[tri, SP], writes=[pbl])
        bv = pbm[:, 0:NG].rearrange("p (t h) -> p t h", h=4)
        blv = pbl[:, 0:NG].rearrange("p (t h) -> p t h", h=4)
        P.op("dve", lambda e, r=r, liv=liv, bv=bv: e.tensor_tensor(TA[:, r, :, :], liv, bv, ALU.subtract), reads=[G, pbm], pw=[TA])
        P.op("act", lambda e, r=r: e.activation(AA[:, r, :, :], TA[:, r, :, :], AF.Exp), reads=[TA], pw=[AA])
        P.op("dve", lambda e, r=r, blv=blv: e.tensor_tensor(TA[:, r, :, :], TA[:, r, :, :], blv, ALU.add), reads=[pbl, AA], pw=[TA])
        P.op("act", lambda e, r=r: e.activation(WW[:, r, :, :], TA[:, r, :, :], AF.Exp), reads=[TA], pw=[WW])
        P.op("act", lambda e, r=r, bv=bv: e.activation(ENB[:, r, :, :], bv, AF.Exp, scale=-1.0), reads=[pbm], pw=[ENB])
        P.op("act", lambda e, r=r, blv=blv: e.activation(EBL[:, r, :, :], blv, AF.Exp), reads=[pbl], pw=[EBL])
    if "dbgG" in self.dbg:
        dG = self.outp("dbgG", [128, NTT, 16])
        P.dma("sp", dG[:, :, :], G[:], reads=[G])
        for nm, bf_ in (("dbgAA", AA), ("dbgWW", WW), ("dbgENB", ENB), ("dbgEBL", EBL), ("dbgSP", SP)):
            dd = self.outp(nm, [128, 2, NTT, 4])
            P.dma("sp", dd[:, :, :, :], bf_[:], reads=[bf_])
    if self.stop == "G":
        return
    mS = A.mark()
    keyx = "xs" if x_src is self.xs else "xin"
    keyc = "xc" if c_src is self.xcs else "cin"
    for r in range(2):
        A.release(mS)
        C32 = A.alloc("C32", [4, 4, 513], F32)
        Cbf = A.alloc("Cbf", [4, 4, 514], BF16)
        P.op("pool", lambda e: e.memset(C32[:], 0.0))
        tk0 = P.op("pool", lambda e: e.memset(Cbf[:], 0.0))
        nld = 3 if r == 0 else 2
        q_pool = Pool([A.alloc("qc%d" % i, [16, 128], BF16) for i in range(nld)])
        k_pool = Pool([A.alloc("kc%d" % i, [16, 128], BF16) for i in range(nld)])
        K_pool = Pool([A.alloc("Kc%d" % i, [E], BF16) for i in range(nld)])
        V_pool = Pool([A.alloc("Vc%d" % i, [E], BF16) for i in range(nld)])
        WT_pool = Pool([A.alloc("WT%d" % i, [128], BF16) for i in range(2)])
        Va_pool = Pool([A.alloc("Va%d" % i, [514], BF16) for i in range(2)])
        Vw_pool = Pool([A.alloc("Vw%d" % i, [514], BF16) for i in range(2)])
        rr_pool = Pool([A.alloc("rr%d" % i, [2], F32) for i in range(4)])
        hF_pool = Pool([A.alloc("hF%d" % i, [4, 512], BF16) for i in range(2 if r == 0 else 1)])
        if r == 1:
            h32 = A.alloc("h32", [4, 512], F32)
            hn = A.alloc("hn", [4, 512], BF16)
            stats = A.alloc("stats", [4, 6], F32)
            mv = A.alloc("mv", [4, 2], F32)
            rs = A.alloc("rs", [4], F32)
            t1 = A.alloc("t1", [16, 128], F32)
            yT = A.alloc("yTm", [16, 128], BF16)
            ozx = [A.alloc("ozx%d" % i, [16, 128], BF16) for i in range(3)]
            wout = A.alloc("mwout", [16, D], BF16)
            stage = Pool([A.alloc("sstg%d" % i, [512], F32) for i in range(2)])
            self.load_w(wout, self.m_w_out[j], 16, D, stage, engs=("pool",))
            xt_pool = Pool([A.alloc("mx%d" % i, [D], F32) for i in range(2)])
            xn_pool = Pool([A.alloc("mxn%d" % i, [D], F32) for i in range(1)])
        order = [0, 1] + list(range(2, NTT)) if r == 0 else [1, 0] + list(range(NTT - 1, 1, -1))
        ctok = {hd: tk0 for hd in range(4)}
        for tt in order:
            is_ctx = tt < 2
            emit = (not is_ctx) or upd_ctx
            Kc = K_pool.next()
            Vc = V_pool.next()
            P.dma("sp", Kc[:], S["Kt"][tt * 128:(tt + 1) * 128, :], writes=[Kc])
            P.dma("sp", Vc[:], S["Vt"][tt * 128:(tt + 1) * 128, :], writes=[Vc])
            if emit:
                qc = q_pool.next()
                kc_ = k_pool.next()
                P.dma("sp", qc[:], S["QT"][tt].rearrange("p h e k -> p (h e) k"), writes=[qc])
                P.dma("sp", kc_[:], S["KT"][tt].rearrange("p h e k -> p (h e) k"), writes=[kc_])
                hF = hF_pool.next()
                if r == 1:
                    P.dma("sp", hF[:], S["HF"][tt * 128:(tt + 1) * 128, :].rearrange("p (h v) -> p h v", h=4), writes=[hF])
                    h32.begin()
                else:
                    hF.begin()
            for hd in range(4):
                a_s = AA[:, r, tt, hd:hd + 1]
                w_s = WW[:, r, tt, hd:hd + 1]
                Vw = Vw_pool.next().begin()
                tok_mmn = None
                small = pb.next()
                ps = small
                pd = Buf(small.ap[:, 128:132])
                psn = Buf(small.ap[:, 132:136])
                P.op("pool", lambda e, Vw=Vw, Vc=Vc, hd=hd, w_s=w_s: e.tensor_scalar(
                    Vw[:, 0:512], Vc[:, hd * 512:(hd + 1) * 512], w_s, None, ALU.mult), reads=[Vc, WW], pw=[Vw])
                P.op("pool", lambda e, Vw=Vw, w_s=w_s: e.tensor_copy(Vw[:, 512:513], w_s), reads=[WW], pw=[Vw])
                if emit:
                    Va = Va_pool.next().begin()
                    P.op("pool", lambda e, Va=Va, Vc=Vc, hd=hd, a_s=a_s: e.tensor_scalar(
                        Va[:, 0:512], Vc[:, hd * 512:(hd + 1) * 512], a_s, None, ALU.mult), reads=[Vc, AA], pw=[Va])
                    P.op("pool", lambda e, Va=Va, a_s=a_s: e.tensor_copy(Va[:, 512:513], a_s), reads=[AA], pw=[Va])

                    def mms(e, ps=ps, qc=qc, kc_=kc_, hd=hd):
                        for ec in range(4):
                            ins = e.matmul(ps[:, 0:128], kc_[:, hd * 4 + ec, :], qc[:, hd * 4 + ec, :],
                                           start=(ec == 0), stop=(ec == 3))
                        return ins
                    P.op("pe", mms, reads=[qc, kc_], writes=[small])
                    WT = WT_pool.next()
                    P.op("dve", lambda e, WT=WT, ps=ps, r=r: e.tensor_tensor(WT[:], ps[:, 0:128], mask[:, r, :], ALU.mult),
                         reads=[small, mask], writes=[WT])
                    pn = pb.next()

                    def mmn(e, pn=pn, pd=pd, WT=WT, Va=Va, qc=qc, hd=hd):
                        e.matmul(pn[:, :], WT[:], Va[:, 0:512], start=True, stop=False)
                        for kc in range(4):
                            e.matmul(pn[:, :], qc[:, hd * 4 + kc, :], Cbf[:, hd, kc, 0:512], start=False, stop=(kc == 3))
                        e.matmul(pd[:, 0:1], WT[:], Va[:, 512:513], start=True, stop=False)
                        for kc in range(4):
                            ins = e.matmul(pd[:, 0:1], qc[:, hd * 4 + kc, :], Cbf[:, hd, kc, 512:513], start=False, stop=(kc == 3))
                        return ins
                    tok_mmn = P.op("pe", mmn, reads=[WT, Va, qc], writes=[pn, small], extra=[ctok.get(hd)])
                    rr = rr_pool.next()
                    P.op("act", lambda e, rr=rr, pd=pd: e.activation(rr[:, 0:1], pd[:, 0:1], AF.Abs), reads=[small], writes=[rr])
                    P.op("dve", lambda e, rr=rr, tt=tt, hd=hd, r=r: e.tensor_scalar(
                        rr[:, 0:1], rr[:, 0:1], ENB[:, r, tt, hd:hd + 1], None, ALU.max), reads=[rr, ENB], writes=[rr])
                    P.op("dve", lambda e, rr=rr: e.reciprocal(rr[:, 1:2], rr[:, 0:1]), reads=[rr], writes=[rr])
                    if r == 0:
                        P.op("act", lambda e, hF=hF, pn=pn, rr=rr, hd=hd: e.activation(hF[:, hd, :], pn[:, :], AF.Copy, scale=rr[:, 1:2]),
                             reads=[pn, rr], pw=[hF])
                    else:
                        P.op("dve", lambda e, hF=hF, pn=pn, rr=rr, hd=hd: e.scalar_tensor_tensor(
                            h32[:, hd, :], pn[:, :], rr[:, 1:2], hF[:, hd, :], ALU.mult, ALU.add), reads=[pn, rr, hF], pw=[h32])
                last = (tt == order[-1])
                if not last:
                    ebl = EBL[:, r, tt, hd:hd + 1]
                    pkv = [pb.next() for _ in range(4)]

                    def mmkv(e, pkv=pkv, psn=psn, Kc=Kc, Vw=Vw, hd=hd):
                        for kc in range(4):
                            e.matmul(pkv[kc][:, :], Kc[:, hd * 512 + kc * 128:hd * 512 + (kc + 1) * 128], Vw[:, 0:512],
                                     start=True, stop=True)
                        for kc in range(4):
                            ins = e.matmul(psn[:, kc:kc + 1], Kc[:, hd * 512 + kc * 128:hd * 512 + (kc + 1) * 128], Vw[:, 512:513],
                                           start=True, stop=True)
                        return ins
                    P.op("pe", mmkv, reads=[Kc, Vw], writes=pkv + [small])
                    toks = []
                    for kc in range(4):
                        tk = P.op("dve", lambda e, kc=kc, pkv=pkv, hd=hd, ebl=ebl: e.scalar_tensor_tensor(
                            C32[:, hd, kc, 0:512], C32[:, hd, kc, 0:512], ebl, pkv[kc][:, :], ALU.mult, ALU.add),
                            reads=[pkv[kc], EBL], extra=[ctok.get(hd)])
                        toks.append(tk)
                    tk = P.op("dve", lambda e, psn=psn, hd=hd, ebl=ebl: e.scalar_tensor_tensor(
                        C32[:, hd, :, 512], C32[:, hd, :, 512], ebl, psn[:, 0:4], ALU.mult, ALU.add),
                        reads=[small, EBL], extra=[ctok.get(hd)])
                    toks.append(tk)
                    ctok[hd] = P.op("act", lambda e, hd=hd: e.activation(Cbf[:, hd, :, 0:513], C32[:, hd, :, :], AF.Copy),
                                    extra=toks + [ctok.get(hd), tok_mmn])
            if emit and r == 0:
                P.dma("sp", S["HF"][tt * 128:(tt + 1) * 128, :].rearrange("p (h v) -> p h v", h=4), hF[:], reads=[hF],
                      writes=[self.dbuf(("HF", j, tt))])
            if emit and r == 1:
                for i in range(3):
                    P.dma("sp", ozx[i][:], S["OZX"][i, tt], writes=[ozx[i]])
                P.op("dve", lambda e: [e.bn_stats(stats[:, hd, :], h32[:, hd, :]) for hd in range(4)][-1], reads=[h32], writes=[stats])
                P.op("dve", lambda e: [e.bn_aggr(mv[:, hd, :], stats[:, hd, :]) for hd in range(4)][-1], reads=[stats], writes=[mv])
                P.op("act", lambda e: e.activation(rs[:], mv[:, :, 1], AF.Sqrt, bias=EPS), reads=[mv], writes=[rs])
                P.op("dve", lambda e: e.reciprocal(rs[:], rs[:]), reads=[rs], writes=[rs])
                hn.begin()
                for hd in range(4):
                    eng = "dve" if hd % 2 == 0 else "pool"
                    P.op(eng, lambda e, hd=hd: e.tensor_scalar(hn[:, hd, :], h32[:, hd, :], mv[:, hd, 0:1], rs[:, hd:hd + 1],
                                                               ALU.subtract, ALU.mult), reads=[h32, mv, rs], pw=[hn])
                t1.begin()
                for half in range(2):
                    ptb = pb.next()
                    ptv = ptb.ap.bitcast(BF16)

                    def tr(e, ptv=ptv, half=half):
                        for c8 in range(8):
                            c = half * 8 + c8
                            ins = e.transpose(ptv[:, c8 * 128:(c8 + 1) * 128], hn[:, c // 4, (c % 4) * 128:(c % 4 + 1) * 128], self.ident[:])
                        return ins
                    P.op("pe", tr, reads=[hn, self.ident], writes=[ptb])
                    pv = ptv[:, 0:1024].rearrange("p (c t) -> p c t", c=8)
                    P.op("dve", lambda e, pv=pv, half=half: e.tensor_tensor(t1[:, half * 8:(half + 1) * 8, :], pv,
                                                                             ozx[0][:, half * 8:(half + 1) * 8, :], ALU.mult),
                         reads=[ptb, ozx[0]], pw=[t1])
                P.op("pool", lambda e: e.tensor_tensor(t1[:], t1[:], ozx[2][:], ALU.add), reads=[ozx[2]], writes=[t1])
                P.op("pool", lambda e: e.tensor_tensor(yT[:], t1[:], ozx[1][:], ALU.mult), reads=[t1, ozx[1]], writes=[yT])
                if is_ctx:
                    self.out_stage(yT, 0, "ctx", tt, False, self.gc_bc, c_src, keyc, "xc", self.xcs, wout, xt_pool, xn_pool, pb)
                else:
                    self.out_stage(yT, 0, "lat", tt - 2, False, self.g_bc, x_src, keyx, "xs", self.xs, wout, xt_pool, xn_pool, pb)
        self.barrier()
        if self.stop == "P1":
            return


KB.mlstm_scan = mlstm_scan


_CACHE = {}
LAUNCH_GROUPS = ((0, 1, 2, 3),)


def _get_prog(N, layers, final):
    key = (N, layers, final)
    if key not in _CACHE:
        kb = KB(N=N, layers=layers, final=final, dbg=("xs", "xcs"))
        nc = kb.build()
        _CACHE[key] = (kb, nc)
    return _CACHE[key]


def kernel(**inputs):
    x = np.asarray(inputs["x"], dtype=np.float32)
    B, N, _ = x.shape
    sh = host_shared(inputs)
    cores = [host_core(inputs, b) for b in range(B)]
    out = None
    for gi, layers in enumerate(LAUNCH_GROUPS):
        final = (3 in layers)
        kb, nc = _get_prog(N, tuple(layers), final)
        in_maps = []
        for b in range(B):
            m = dict(sh)
            m.update(cores[b])
            in_maps.append({k: v for k, v in m.items() if k in kb.din})
        res = run_bass_kernel_spmd(nc, in_maps, core_ids=list(range(B)))
        for b in range(B):
            r = res.results[b]
            if final:
                continue
            cores[b]["x"] = np.asarray(r["xs"], dtype=np.float32)
            if any(l < 2 for l in layers):
                cores[b]["ctx"] = np.asarray(r["xcs"], dtype=np.float32)
        if final:
            out = np.stack([np.asarray(res.results[b]["out"], dtype=np.float32) for b in range(B)], axis=0)
    return out
```

```python
import math
from contextlib import ExitStack

import numpy as np
import ml_dtypes
import concourse.bass as bass
import concourse.mybir as mybir
from concourse.bass_utils import run_bass_kernel_spmd

F32 = mybir.dt.float32
BF16 = mybir.dt.bfloat16
AF = mybir.ActivationFunctionType
ALU = mybir.AluOpType
AX = mybir.AxisListType

D = 1024
E = 2048
NH = 4
DH = 512
CTX = 256
GW = 64
EPS = 1e-6
NDS = 12


class Buf:
    def __init__(self, ap, name=""):
        self.ap = ap
        self.name = name
        self.ws = []
        self.r = []
        self.prev = []
        self.excl = False

    def begin(self):
        self.prev = self.ws + self.r
        self.ws = []
        self.r = []
        return self

    def __getitem__(self, k):
        return self.ap[k]


class Prog:
    CE = ("pe", "dve", "act", "pool")
    DQ = ("sp", "act", "pool")

    def __init__(self, nc, es):
        self.nc = nc
        self.es = es
        self.q = {e: [] for e in ("pe", "dve", "act", "pool", "sp")}
        self.sems = {}
        self.cnt = {}
        for e in self.CE:
            self.sems[e] = es.enter_context(nc.semaphore("s_" + e))
            self.cnt[e] = 0
        self.dq_i = {}
        for qn in self.DQ:
            self.dq_i[qn] = 0
            for i in range(NDS):
                k = ("d", qn, i)
                self.sems[k] = es.enter_context(nc.semaphore("d_%s%d" % (qn, i)))
                self.cnt[k] = 0
        self.waited = {e: {} for e in self.q}
        self.n_ops = 0

    def _needed(self, eng, toks):
        out = []
        w = self.waited[eng]
        best = {}
        for t in toks:
            if t is None:
                continue
            k, v = t
            if k == eng and eng == "pe":
                continue
            if w.get(k, 0) >= v:
                continue
            if best.get(k, 0) < v:
                best[k] = v
        for k, v in best.items():
            w[k] = v
            out.append((k, v))
        return out

    def op(self, eng, fn, reads=(), writes=(), extra=(), pw=()):
        toks = list(extra)
        for b in pw:
            toks.extend(b.prev)
        for b in reads:
            toks.extend(b.ws)
            if b.excl:
                toks.extend(t for t in b.r if t[0] != eng)
        for b in writes:
            toks.extend(b.ws)
            toks.extend(b.r)
        waits = self._needed(eng, toks)
        self.cnt[eng] += 1
        tok = (eng, self.cnt[eng])
        self.q[eng].append((waits, fn, eng, 1))
        for b in reads:
            b.r.append(tok)
        for b in writes:
            b.ws = [tok]
            b.r = []
        for b in pw:
            b.ws.append(tok)
        self.n_ops += 1
        return tok

    def dma(self, qn, out_ap, in_ap, reads=(), writes=(), extra=(), also=(), **kw):
        i = self.dq_i[qn] % NDS
        self.dq_i[qn] += 1
        k = ("d", qn, i)
        toks = list(extra)
        if self.cnt[k] > 0:
            toks.append((k, self.cnt[k]))
        for b in reads:
            toks.extend(b.ws)
        for b in writes:
            toks.extend(b.ws)
            toks.extend(b.r)
        waits = self._needed(qn, toks)
        self.cnt[k] += 16
        tok = (k, self.cnt[k])

        def fn(e, out_ap=out_ap, in_ap=in_ap, kw=kw):
            return e.dma_start(out=out_ap, in_=in_ap, **kw)

        self.q[qn].append((waits, fn, k, 16))
        for b in reads:
            b.r.append(tok)
        for b in writes:
            b.ws = [tok]
            b.r = []
        for b in also:
            b.ws.append(tok)
        return tok

    def wait_all(self, eng, toks):
        waits = self._needed(eng, toks)
        self.q[eng].append((waits, None, None, 0))

    def cut(self):
        for name in self.q:
            self.q[name].append("CUT")

    def replay(self):
        nc = self.nc
        sems = self.sems
        segs = {}
        nseg = 0
        for name, items in self.q.items():
            cur = []
            lst = [cur]
            for it in items:
                if it == "CUT":
                    cur = []
                    lst.append(cur)
                else:
                    cur.append(it)
            segs[name] = lst
            nseg = max(nseg, len(lst))
        for si in range(nseg):
            if not any(len(segs[n][si]) for n in segs if si < len(segs[n])):
                continue
            with nc.Block() as block:
                def run(eng, name, si=si):
                    for waits, fn, sk, inc in segs[name][si]:
                        for k, v in waits:
                            eng.wait_ge(sems[k], v)
                        if fn is not None:
                            ins = fn(eng)
                            ins.then_inc(sems[sk], inc)

                @block.tensor
                def _(e):
                    run(e, "pe")

                @block.vector
                def _(e):
                    run(e, "dve")

                @block.scalar
                def _(e):
                    run(e, "act")

                @block.gpsimd
                def _(e):
                    run(e, "pool")

                @block.sync
                def _(e):
                    run(e, "sp")


class Pool:
    def __init__(self, bufs):
        self.bufs = bufs
        self.i = 0

    def next(self):
        b = self.bufs[self.i % len(self.bufs)]
        self.i += 1
        return b


class Arena:
    def __init__(self, ap):
        self.ap = ap
        self.top = 0
        self.W = ap.shape[1]
        self.peak = 0

    def alloc(self, name, free_shape, dt):
        n = 1
        for s in free_shape:
            n *= s
        esz = 4 if dt == F32 else 2
        words = (n * esz + 3) // 4
        words = (words + 7) // 8 * 8
        a = self.top
        self.top += words
        self.peak = max(self.peak, self.top)
        assert self.top <= self.W, "SBUF arena overflow at %s: %d > %d" % (name, self.top, self.W)
        v = self.ap[:, a:a + words]
        if dt != F32:
            v = v.bitcast(dt)
        v = v[:, 0:n]
        if len(free_shape) == 2:
            v = v.rearrange("p (a b) -> p a b", a=free_shape[0])
        elif len(free_shape) == 3:
            v = v.rearrange("p (a b c) -> p a b c", a=free_shape[0], b=free_shape[1])
        elif len(free_shape) == 4:
            v = v.rearrange("p (a b c d) -> p a b c d", a=free_shape[0], b=free_shape[1], c=free_shape[2])
        return Buf(v, name)

    def mark(self):
        return self.top

    def release(self, m):
        self.top = m


class K:
    pass


def fourier_tile_rows(dr, t):
    v = dr.rearrange("(n1 n2) d -> n2 n1 d", n2=GW)
    return [(0, 64, v[2 * t]), (64, 128, v[2 * t + 1])]


def natural_tile_rows(dr, t):
    return [(0, 128, dr[t * 128:(t + 1) * 128, :])]


def _prod(s):
    n = 1
    for v in s:
        n *= v
    return n


class KB:
    def __init__(self, N=4096, layers=(0, 1, 2, 3), final=True, dbg=()):
        self.N = N
        self.T = CTX + N
        self.layers = tuple(layers)
        self.final = final
        self.dbg = tuple(dbg)
        self.nc = bass.Bass("TRN2", target_bir_lowering=False)
        self.din = {}
        self.es = ExitStack()

    def inp(self, name, shape, dt=F32):
        t = self.nc.dram_tensor(name, list(shape), dt, kind="ExternalInput").ap()
        self.din[name] = t
        return t

    def outp(self, name, shape, dt=F32):
        return self.nc.dram_tensor(name, list(shape), dt, kind="ExternalOutput").ap()

    def scratch(self, name, shape, dt):
        kind = "ExternalOutput" if name in self.dbg else "Internal"
        return self.nc.dram_tensor(name, list(shape), dt, kind=kind).ap()

    def dbuf(self, key):
        b = self.dbufs.get(key)
        if b is None:
            b = Buf(None, str(key))
            self.dbufs[key] = b
        return b

    def barrier(self):
        P = self.P
        toks = [(k, v) for k, v in P.cnt.items() if v > 0]
        for e in ("pe", "dve", "act", "pool", "sp"):
            P.wait_all(e, toks)
        P.cut()

    def build(self):
        nc, es = self.nc, self.es
        N, T = self.N, self.T
        self.x_in = self.inp("x", [N, D])
        self.ctx_in = self.inp("ctx", [CTX, D])
        self.cvec = self.inp("cvec", [128, 8, 2])
        self.norm_g = self.inp("norm_g", [4, D])
        self.w_ada = self.inp("w_ada", [4, 128, 8, 3 * D])
        self.b_ada = self.inp("b_ada", [4, 3 * D])
        self.f_w_in = self.inp("f_w_in", [2, 128, 8, 2 * E])
        self.f_w_out = self.inp("f_w_out", [2, 128, 16, D])
        self.norm_f = self.inp("norm_f", [1, D])
        self.identb = self.inp("identb", [128, 128], BF16)
        self.c_cs = self.inp("c_cs", [128, 2, 512], BF16)
        self.c_a = self.inp("c_a", [128, 3, 128], BF16)
        self.c_bk = self.inp("c_bk", [128, 64, 64], BF16)
        self.c_d256 = self.inp("c_d256", [128, 2, 2, 256], BF16)
        self.declare_mlstm_inputs()
        self.out = self.outp("out", [N, D])
        self.xs = self.scratch("xs", [N, D], F32)
        self.xcs = self.scratch("xcs", [CTX, D], F32)
        self.dbufs = {}
        self.scr = {}
        with es:
            self.P = Prog(nc, es)
            sb_all = es.enter_context(nc.sbuf_tensor("sb_all", [128, 50 * 1024], F32))
            self.A = Arena(sb_all)
            self.psum = es.enter_context(nc.psum_tensor("ps_all", [128, 8 * 512], F32))
            self.pbank = [Buf(self.psum[:, b * 512:(b + 1) * 512], "bank%d" % b) for b in range(8)]
            for b_ in self.pbank:
                b_.excl = True
            A = self.A
            self.ident = A.alloc("ident", [128], BF16)
            self.P.dma("sp", self.ident[:], self.identb[:, :], writes=[self.ident])
            self.g_bc = A.alloc("g_bc", [D], F32)
            self.gc_bc = A.alloc("gc_bc", [D], F32)
            x_src, c_src = self.x_in, self.ctx_in
            for li in self.layers:
                upd_ctx = li < 2
                need_ctx = upd_ctx or (li % 2 == 0)
                fourier = (li % 2 == 1)
                mL = A.mark()
                if fourier:
                    self.fTc = A.alloc("fTc", [16, 256], F32)
                else:
                    self.G = A.alloc("G", [T // 128, 16], F32)
                    self.bif = A.alloc("bif", [16], F32)
                self.m_hT = A.mark()
                self.hT = A.alloc("hT", [8, T], BF16)
                m = A.mark()
                self.prep(li, x_src, c_src, need_ctx, fourier)
                self.barrier()
                A.release(m)
                if fourier:
                    self.fourier_layer(li, x_src, c_src, upd_ctx)
                else:
                    self.mlstm_layer(li, x_src, c_src, upd_ctx)
                self.barrier()
                A.release(mL)
                x_src = self.xs
                if upd_ctx:
                    c_src = self.xcs
            if self.final:
                self.final_norm(x_src)
            self.barrier()
            self.P.replay()
        return nc

    def declare_mlstm_inputs(self):
        pass

    def mlstm_layer(self, li, x_src, c_src, upd_ctx):
        raise NotImplementedError

    def tiles(self, fourier, need_ctx):
        out = []
        if need_ctx:
            for t in range(CTX // 128):
                out.append(("ctx", t, t * 128))
        for t in range(self.N // 128):
            out.append(("lat", t, CTX + t * 128))
        return out

    def tile_rows(self, stream, t, fourier, dr):
        if stream == "lat" and fourier:
            return fourier_tile_rows(dr, t)
        return natural_tile_rows(dr, t)

    def load_rows(self, q, buf, stream, t, fourier, dr, key):
        P = self.P
        toks = []
        db = self.dbuf((key, stream, t))
        first = True
        for (p0, p1, ap) in self.tile_rows(stream, t, fourier, dr):
            if first:
                tok = P.dma(q, buf.ap[p0:p1], ap, reads=[db], writes=[buf])
                first = False
            else:
                tok = P.dma(q, buf.ap[p0:p1], ap, reads=[db], also=[buf])
            toks.append(tok)
        return toks

    def store_rows(self, q, buf, stream, t, fourier, dr, key, extra=()):
        P = self.P
        db = self.dbuf((key, stream, t))
        toks = []
        for (p0, p1, ap) in self.tile_rows(stream, t, fourier, dr):
            toks.append(P.dma(q, ap, buf.ap[p0:p1], reads=[buf], writes=[db], extra=extra))
        return toks

    def prep(self, li, x_src, c_src, need_ctx, fourier):
        P, A = self.P, self.A
        cv = A.alloc("cv", [8, 2], F32)
        sv = A.alloc("sv", [8, 2], F32)
        ones = A.alloc("ones", [128], F32)
        sbc = A.alloc("sbc", [2, 8, 128], F32)
        mod = [A.alloc("mod%d" % s, [3 * D], F32) for s in range(2)]
        bbc = A.alloc("bbc", [3 * D], F32)
        gn = A.alloc("gn", [D], F32)
        abc = [A.alloc("abc%d" % s, [D], F32) for s in range(2)]
        wst = Pool([A.alloc("wst%d" % i, [8, 256], F32) for i in range(2)])
        P.dma("sp", cv[:], self.cvec[:, :, :], writes=[cv])
        P.dma("sp", bbc[:], self.b_ada[li].partition_broadcast(128), writes=[bbc])
        P.dma("sp", gn[:], self.norm_g[li].partition_broadcast(128), writes=[gn])
        P.op("act", lambda e: e.activation(sv[:], cv[:], AF.Silu), reads=[cv], writes=[sv])
        P.op("pool", lambda e: e.memset(ones[:], 1.0), writes=[ones])
        nstream = 2 if need_ctx else 1

        def mk_sbc(e):
            for s in range(nstream):
                for kc in range(8):
                    ins = e.tensor_scalar(sbc[:, s, kc, :], ones[:], sv[:, kc, s:s + 1], None, ALU.mult)
            return ins
        P.op("dve", mk_sbc, reads=[ones, sv], writes=[sbc])
        pb = Pool([self.pbank[0], self.pbank[1]])
        for blk in range(12):
            w = wst.next()
            P.dma("sp", w[:], self.w_ada[li, :, :, blk * 256:(blk + 1) * 256], writes=[w])
            for s in range(nstream):
                pm = pb.next()

                def mm(e, s=s, w=w, pm=pm):
                    for kc in range(8):
                        ins = e.matmul(pm[:, 0:256], sbc[:, s, kc, :], w[:, kc, :], start=(kc == 0), stop=(kc == 7))
                    return ins
                P.op("pe", mm, reads=[sbc, w], writes=[pm])
                sl = slice(blk * 256, (blk + 1) * 256)
                P.op("dve", lambda e, s=s, pm=pm, sl=sl: e.tensor_tensor(mod[s][:, sl], pm[:, 0:256], bbc[:, sl], ALU.add),
                     reads=[pm, bbc], writes=[mod[s]])
        for s in range(nstream):
            P.op("dve", lambda e, s=s: e.scalar_tensor_tensor(abc[s][:], mod[s][:, D:2 * D], 1.0, gn[:], ALU.add, ALU.mult),
                 reads=[mod[s], gn], writes=[abc[s]])
        P.op("pool", lambda e: e.tensor_copy(self.g_bc[:], mod[0][:, 2 * D:3 * D]), reads=[mod[0]], writes=[self.g_bc])
        if need_ctx:
            P.op("pool", lambda e: e.tensor_copy(self.gc_bc[:], mod[1][:, 2 * D:3 * D]), reads=[mod[1]], writes=[self.gc_bc])
        xt_pool = Pool([A.alloc("xt%d" % i, [D], F32) for i in range(3)])
        junk = A.alloc("junk", [D], F32)
        t1_pool = Pool([A.alloc("t1_%d" % i, [D], F32) for i in range(2)])
        xn_pool = Pool([A.alloc("xn%d" % i, [D], BF16) for i in range(2)])
        st_pool = Pool([A.alloc("st%d" % i, [2], F32) for i in range(4)])
        pt_pool = Pool([self.pbank[2], self.pbank[3]])
        key = "xc" if c_src is self.xcs else "cin"
        keyx = "xs" if x_src is self.xs else "xin"
        self.hT.begin()
        for ti, (stream, t, pos0) in enumerate(self.tiles(fourier, need_ctx)):
            s = 1 if stream == "ctx" else 0
            dr = c_src if stream == "ctx" else x_src
            xt = xt_pool.next()
            self.load_rows("sp", xt, stream, t, fourier, dr, key if stream == "ctx" else keyx)
            st = st_pool.next()
            P.op("act", lambda e, xt=xt, st=st: e.activation(junk[:], xt[:], AF.Square, accum_out=st[:, 0:1]),
                 reads=[xt], writes=[junk, st])
            P.op("act", lambda e, st=st: e.activation(st[:, 1:2], st[:, 0:1], AF.Sqrt, bias=EPS, scale=1.0 / D),
                 reads=[st], writes=[st])
            P.op("dve", lambda e, st=st: e.reciprocal(st[:, 1:2], st[:, 1:2]), reads=[st], writes=[st])
            t1 = t1_pool.next()
            P.op("dve", lambda e, xt=xt, st=st, t1=t1, s=s: e.scalar_tensor_tensor(
                t1[:], xt[:], st[:, 1:2], abc[s][:], ALU.mult, ALU.mult), reads=[xt, st, abc[s]], writes=[t1])
            xn = xn_pool.next()
            P.op("pool", lambda e, t1=t1, xn=xn, s=s: e.tensor_tensor(xn[:], t1[:], mod[s][:, 0:D], ALU.add),
                 reads=[t1, mod[s]], writes=[xn])
            pt = pt_pool.next()
            ptv = pt.ap.bitcast(BF16)

            def tr(e, xn=xn, ptv=ptv):
                for c in range(8):
                    ins = e.transpose(ptv[:, c * 128:(c + 1) * 128], xn[:, c * 128:(c + 1) * 128], self.ident[:])
                return ins
            P.op("pe", tr, reads=[xn, self.ident], writes=[pt])
            eng = "act" if ti % 2 == 0 else "dve"
            hv = self.hT[:, :, pos0:pos0 + 128]
            pv = ptv[:, 0:1024].rearrange("p (c t) -> p c t", c=8)
            if eng == "act":
                P.op("act", lambda e, hv=hv, pv=pv: e.activation(hv, pv, AF.Copy), reads=[pt], pw=[self.hT])
            else:
                P.op("dve", lambda e, hv=hv, pv=pv: e.tensor_copy(hv, pv), reads=[pt], pw=[self.hT])

    def final_norm(self, x_src):
        P, A = self.P, self.A
        m = A.mark()
        nf = A.alloc("nf", [D], F32)
        P.dma("sp", nf[:], self.norm_f[0].partition_broadcast(128), writes=[nf])
        xt_pool = Pool([A.alloc("fxt%d" % i, [D], F32) for i in range(3)])
        yo_pool = Pool([A.alloc("fyo%d" % i, [D], F32) for i in range(3)])
        junk = A.alloc("fjunk", [D], F32)
        st_pool = Pool([A.alloc("fst%d" % i, [2], F32) for i in range(4)])
        keyx = "xs" if x_src is self.xs else "xin"
        last = []
        for t in range(self.N // 128):
            xt = xt_pool.next()
            self.load_rows("sp", xt, "lat", t, False, x_src, keyx)
            st = st_pool.next()
            P.op("act", lambda e, xt=xt, st=st: e.activation(junk[:], xt[:], AF.Square, accum_out=st[:, 0:1]),
                 reads=[xt], writes=[junk, st])
            P.op("act", lambda e, st=st: e.activation(st[:, 1:2], st[:, 0:1], AF.Sqrt, bias=EPS, scale=1.0 / D),
                 reads=[st], writes=[st])
            P.op("dve", lambda e, st=st: e.reciprocal(st[:, 1:2], st[:, 1:2]), reads=[st], writes=[st])
            yo = yo_pool.next()
            P.op("dve", lambda e, xt=xt, st=st, yo=yo: e.scalar_tensor_tensor(
                yo[:], xt[:], st[:, 1:2], nf[:], ALU.mult, ALU.mult), reads=[xt, st, nf], writes=[yo])
            last += self.store_rows("sp", yo, "lat", t, False, self.out, "out")
        A.release(m)


def _load_w(self, dst_view, src_ap, kc, ncols, stage_pool, engs=("pool",)):
    P = self.P
    piece = max(1, stage_pool.bufs[0].ap.shape[1] // kc)
    c0 = 0
    i = 0
    while c0 < ncols:
        w = min(piece, ncols - c0)
        st = stage_pool.next()
        sv = st.ap[:, 0:kc * w].rearrange("p (k n) -> p k n", k=kc)
        P.dma("sp", sv, src_ap[:, :, c0:c0 + w], writes=[st])
        eng = engs[i % len(engs)]
        dv = dst_view.ap[:, :, c0:c0 + w] if isinstance(dst_view, Buf) else dst_view[:, :, c0:c0 + w]
        if eng == "act":
            P.op("act", lambda e, dv=dv, sv=sv: e.activation(dv, sv, AF.Copy), reads=[st], pw=[self._lw_buf])
        else:
            P.op(eng, lambda e, dv=dv, sv=sv: e.tensor_copy(dv, sv), reads=[st], pw=[self._lw_buf])
        c0 += w
        i += 1


def load_w(self, dst_buf, src_ap, kc, ncols, stage_pool, engs=("pool",), view=None):
    self._lw_buf = dst_buf
    dst_buf.begin()
    _load_w(self, dst_buf if view is None else view, src_ap, kc, ncols, stage_pool, engs)


KB.load_w = load_w


def fourier_layer(self, li, x_src, c_src, upd_ctx):
    P, A = self.P, self.A
    hT = self.hT
    N, T = self.N, self.T
    j = li // 2
    NT = N // 128
    Y = self.scratch("Y%d" % li, [64, 2, 64, E], BF16)
    ZS = self.scratch("ZS%d" % li, [16, 128, T], BF16)
    pb = Pool(self.pbank)
    fTc = self.fTc
    m0 = A.mark()
    cs = A.alloc("cs", [2, 512], BF16)
    ca = A.alloc("ca", [3, 128], BF16)
    d256 = A.alloc("d256", [2, 2, 256], BF16)
    P.dma("sp", cs[:], self.c_cs[:, :, :], writes=[cs])
    P.dma("sp", ca[:], self.c_a[:, :, :], writes=[ca])
    P.dma("sp", d256[:], self.c_d256[:, :, :, :], writes=[d256])
    stage = Pool([A.alloc("wstg%d" % i, [2048], F32) for i in range(2)])
    wb_pool = Pool([A.alloc("wb%d" % i, [8, 512], BF16) for i in range(2)])
    uT = A.alloc("uT", [4, T], BF16)
    UT_pool = Pool([A.alloc("UT%d" % i, [2, 2, 256], BF16) for i in range(3)])
    UTc = A.alloc("UTc", [2, 2, 2, 256], BF16) if upd_ctx else None
    Ysb_pool = Pool([A.alloc("Ysb%d" % i, [2, 512], BF16) for i in range(3)])
    zsb_pool = Pool([A.alloc("zsb%d" % i, [4, 512], BF16) for i in range(2)])
    ttiles = []
    if upd_ctx:
        ttiles.append((0, 256))
    for i in range(N // 512):
        ttiles.append((CTX + i * 512, 512))
    ptiles = []
    if upd_ctx:
        ptiles += [("ctx", 0, 0), ("ctx", 1, 128)]
    ptiles += [("lat", t, CTX + t * 128) for t in range(NT)]
    ev = 0
    if upd_ctx:
        fTc.begin()
    for fb in range(4):
        wb = wb_pool.next()
        self.load_w(wb, self.f_w_in[j, :, :, fb * 512:(fb + 1) * 512], 8, 512, stage)
        uT.begin()
        for (pos, w) in ttiles:
            for cc in range(4):
                pm = pb.next()

                def mm(e, pm=pm, wb=wb, cc=cc, pos=pos, w=w):
                    for kc in range(8):
                        ins = e.matmul(pm[:, 0:w], wb[:, kc, cc * 128:(cc + 1) * 128], hT[:, kc, pos:pos + w],
                                       start=(kc == 0), stop=(kc == 7))
                    return ins
                P.op("pe", mm, reads=[wb, hT], writes=[pm])
                ev += 1
                if ev % 2 == 0:
                    P.op("act", lambda e, pm=pm, cc=cc, pos=pos, w=w: e.activation(uT[:, cc, pos:pos + w], pm[:, 0:w], AF.Copy),
                         reads=[pm], pw=[uT])
                else:
                    P.op("dve", lambda e, pm=pm, cc=cc, pos=pos, w=w: e.tensor_copy(uT[:, cc, pos:pos + w], pm[:, 0:w]),
                         reads=[pm], pw=[uT])
        if upd_ctx:
            UTc.begin()
        for (stream, t, pos) in ptiles:
            UT = UT_pool.next().begin() if stream == "lat" else None
            for g in range(2):
                pm = pb.next()

                def mm2(e, pm=pm, g=g, pos=pos):
                    for cc in range(2):
                        ins = e.matmul(pm[:, :], uT[:, 2 * g + cc, pos:pos + 128], cs[:, cc, :],
                                       start=(cc == 0), stop=(cc == 1))
                    return ins
                P.op("pe", mm2, reads=[uT, cs], writes=[pm])
                if stream == "lat":
                    ov = UT[:, :, g, :]
                    ob = UT
                else:
                    ov = UTc[:, t, :, g, :]
                    ob = UTc
                pv = pm.ap.rearrange("p (a b) -> p a b", a=2)
                ev += 1
                if ev % 2 == 0:
                    P.op("act", lambda e, ov=ov, pv=pv: e.activation(ov, pv, AF.Copy), reads=[pm], pw=[ob])
                else:
                    P.op("dve", lambda e, ov=ov, pv=pv: e.tensor_copy(ov, pv), reads=[pm], pw=[ob])
            if stream == "lat":
                p_r = pb.next()
                p_i = pb.next()
                uc = UT[:, 0, :, :].rearrange("p g f -> p (g f)")
                us = UT[:, 1, :, :].rearrange("p g f -> p (g f)")

                def mmA(e, p_r=p_r, p_i=p_i, uc=uc, us=us):
                    e.matmul(p_r[:, :], ca[:, 0, :], uc, start=True, stop=False)
                    e.matmul(p_r[:, :], ca[:, 1, :], us, start=False, stop=True)
                    e.matmul(p_i[:, :], ca[:, 1, :], uc, start=True, stop=False)
                    return e.matmul(p_i[:, :], ca[:, 2, :], us, start=False, stop=True)
                P.op("pe", mmA, reads=[UT, ca], writes=[p_r, p_i])
                Ysb = Ysb_pool.next().begin()
                P.op("act", lambda e, Ysb=Ysb, p_r=p_r: e.activation(Ysb[:, 0, :], p_r[:, :], AF.Copy), reads=[p_r], pw=[Ysb])
                P.op("dve", lambda e, Ysb=Ysb, p_i=p_i: e.tensor_copy(Ysb[:, 1, :], p_i[:, :]), reads=[p_i], pw=[Ysb])
                for jj in range(2):
                    P.dma("sp", Y[:, :, 2 * t + jj, fb * 512:(fb + 1) * 512], Ysb.ap[jj * 64:(jj + 1) * 64, :, :],
                          reads=[Ysb], writes=[self.dbuf(("Y", t, fb, jj))])
        if upd_ctx:
            for g in range(2):
                for half in range(2):
                    pm = pb.next()

                    def mmc(e, pm=pm, g=g, half=half):
                        n = 0
                        for tile in range(2):
                            for cs_ in range(2):
                                ins = e.matmul(pm[:, 0:256], UTc[:, tile, cs_, g, half * 128:(half + 1) * 128],
                                               d256[:, tile, cs_, :], start=(n == 0), stop=(n == 3))
                                n += 1
                        return ins
                    P.op("pe", mmc, reads=[UTc, d256], writes=[pm])
                    c = fb * 4 + g * 2 + half
                    P.op("dve", lambda e, pm=pm, c=c: e.tensor_copy(fTc[:, c, :], pm[:, 0:256]), reads=[pm], pw=[fTc])
        wz = wb_pool.next()
        self.load_w(wz, self.f_w_in[j, :, :, E + fb * 512:E + (fb + 1) * 512], 8, 512, stage)
        for (pos, w) in ttiles:
            zsb = zsb_pool.next().begin()
            for cc in range(4):
                pm = pb.next()

                def mmz(e, pm=pm, wz=wz, cc=cc, pos=pos, w=w):
                    for kc in range(8):
                        ins = e.matmul(pm[:, 0:w], wz[:, kc, cc * 128:(cc + 1) * 128], hT[:, kc, pos:pos + w],
                                       start=(kc == 0), stop=(kc == 7))
                    return ins
                P.op("pe", mmz, reads=[wz, hT], writes=[pm])
                P.op("act", lambda e, zsb=zsb, pm=pm, cc=cc, w=w: e.activation(zsb[:, cc, 0:w], pm[:, 0:w], AF.Silu),
                     reads=[pm], pw=[zsb])
            P.dma("sp", ZS[fb * 4:(fb + 1) * 4, :, pos:pos + w].rearrange("c p t -> p c t"), zsb.ap[:, :, 0:w],
                  reads=[zsb], writes=[self.dbuf(("ZS", fb, pos))])
    self.barrier()
    A.release(self.m_hT)
    wout = A.alloc("wout", [16, D], BF16)
    stage = Pool([A.alloc("wstg%d" % i, [1024], F32) for i in range(2)])
    self.load_w(wout, self.f_w_out[j], 16, D, stage, engs=("pool", "dve"))
    bk = A.alloc("bk", [64, 64], BF16)
    P.dma("sp", bk[:], self.c_bk[:, :, :], writes=[bk])
    zs_pool = Pool([A.alloc("zs%d" % i, [16, 512], BF16) for i in range(2)])
    Yk_pool = Pool([A.alloc("Yk%d" % i, [E], BF16) for i in range(9)])
    yT_pool = Pool([A.alloc("yT%d" % i, [16, 512], BF16) for i in range(2)])
    xt_pool = Pool([A.alloc("fx%d" % i, [D], F32) for i in range(2)])
    xn_pool = Pool([A.alloc("fxn%d" % i, [D], F32) for i in range(2)])
    keyx = "xs" if x_src is self.xs else "xin"

    def out_stage(yT, width_off, stream, t, gb, src, key_in, key_out, dst):
        xt = xt_pool.next()
        self.load_rows("sp", xt, stream, t, stream == "lat", src, key_in)
        xn = xn_pool.next().begin()
        for half in range(2):
            po = pb.next()

            def mmo(e, po=po, half=half):
                for c in range(16):
                    ins = e.matmul(po[:, :], yT[:, c, width_off:width_off + 128], wout[:, c, half * 512:(half + 1) * 512],
                                   start=(c == 0), stop=(c == 15))
                return ins
            P.op("pe", mmo, reads=[yT, wout], writes=[po])
            hs = slice(half * 512, (half + 1) * 512)
            P.op("dve", lambda e, po=po, hs=hs: e.tensor_tensor(xn[:, hs], po[:, :], gb[:, hs], ALU.mult),
                 reads=[po, gb], pw=[xn])
        P.op("pool", lambda e: e.tensor_tensor(xn[:], xn[:], xt[:], ALU.add), reads=[xt], writes=[xn])
        self.store_rows("sp", xn, stream, t, stream == "lat", dst, key_out)

    for st in range(N // 512):
        pos = CTX + st * 512
        zs = zs_pool.next()
        P.dma("sp", zs[:], ZS[:, :, pos:pos + 512].rearrange("c p t -> p c t"), writes=[zs])
        Yks = []
        for kk in range(8):
            k1 = st * 8 + kk
            Yk = Yk_pool.next()
            P.dma("sp", Yk[:], Y[k1].rearrange("r n f -> (r n) f"), writes=[Yk])
            Yks.append(Yk)
        yT = yT_pool.next().begin()
        for c in range(16):
            pf = pb.next()

            def mmf(e, pf=pf, c=c, Yks=Yks, st=st):
                for kk in range(8):
                    ins = e.matmul(pf[:, kk * 64:(kk + 1) * 64], Yks[kk][:, c * 128:(c + 1) * 128], bk[:, st * 8 + kk, :],
                                   start=True, stop=True)
                return ins
            P.op("pe", mmf, reads=Yks + [bk], writes=[pf])
            P.op("dve", lambda e, pf=pf, c=c, yT=yT, zs=zs: e.tensor_tensor(yT[:, c, :], pf[:, :], zs[:, c, :], ALU.mult),
                 reads=[pf, zs], pw=[yT])
        for i in range(4):
            out_stage(yT, i * 128, "lat", st * 4 + i, self.g_bc, x_src, keyx, "xs", self.xs)
    if upd_ctx:
        zs = zs_pool.next()
        P.dma("sp", zs.ap[:, :, 0:256], ZS[:, :, 0:256].rearrange("c p t -> p c t"), writes=[zs])
        yT = yT_pool.next()
        P.op("dve", lambda e: e.tensor_tensor(yT.ap[:, :, 0:256], fTc[:, :, :], zs.ap[:, :, 0:256], ALU.mult),
             reads=[fTc, zs], writes=[yT])
        keyc = "xc" if c_src is self.xcs else "cin"
        for t in range(2):
            out_stage(yT, t * 128, "ctx", t, self.gc_bc, c_src, keyc, "xc", self.xcs)


KB.fourier_layer = fourier_layer


def _bf(a):
    return np.ascontiguousarray(a.astype(np.float32)).astype(ml_dtypes.bfloat16)


def make_consts():
    c = {}
    c["identb"] = _bf(np.eye(128))
    p = np.arange(128)
    cc = np.arange(2)
    ch = (cc[None, :] * 128 + p[:, None])
    k = np.arange(256)
    ang = 2 * np.pi * ((ch[:, :, None] * k[None, None, :]) % 256) / 256.0
    c["c_cs"] = _bf(np.concatenate([np.cos(ang), np.sin(ang)], axis=-1) / 16.0)
    n1 = np.arange(64)
    a64 = 2 * np.pi * ((n1[:, None] * n1[None, :]) % 64) / 64.0
    C, S = np.cos(a64), np.sin(a64)
    Z = np.zeros((64, 64))
    bd = lambda M: np.block([[M, Z], [Z, M]])
    c["c_a"] = _bf(np.stack([bd(C), bd(-S), bd(-C)], axis=1))
    n2 = np.arange(64)[:, None, None]
    k1 = np.arange(64)[None, :, None]
    k2 = np.arange(64)[None, None, :]
    num = (n2 * k2 * 64 + n2 * k1) % 4096
    th = 2 * np.pi * num / 4096.0
    c["c_bk"] = _bf(np.concatenate([np.cos(th), np.sin(th)], axis=0) / 64.0)
    n = (np.arange(2)[None, :] * 128 + p[:, None])
    a256 = 2 * np.pi * ((n[:, :, None] * k[None, None, :]) % 256) / 256.0
    c["c_d256"] = _bf(np.stack([np.cos(a256), -np.sin(a256)], axis=2) / 16.0)
    return c


def _pk(w, kc):
    n = w.shape[-1]
    return np.ascontiguousarray(w.reshape(kc, 128, n).transpose(1, 0, 2))


def host_shared(inp):
    f = lambda a: np.asarray(a, dtype=np.float32)
    sh = {}
    sh["norm_g"] = f(inp["norm_g"])
    sh["w_ada"] = np.stack([_pk(f(inp["w_ada"][l]), 8) for l in range(4)])
    sh["b_ada"] = f(inp["b_ada"])
    sh["f_w_in"] = np.stack([_pk(f(inp["f_w_in"][l]), 8) for l in range(2)])
    sh["f_w_out"] = np.stack([_pk(f(inp["f_w_out"][l]), 16) for l in range(2)])
    sh["norm_f"] = f(inp["norm_f"]).reshape(1, D)
    sh.update(make_consts())
    sh.update(host_shared_mlstm(inp))
    return sh


def host_shared_mlstm(inp):
    return {}


def host_core(inp, b):
    f = lambda a: np.asarray(a, dtype=np.float32)
    d = {}
    d["x"] = np.ascontiguousarray(f(inp["x"][b]))
    d["ctx"] = np.ascontiguousarray(f(inp["ctx"][b]))
    cv = np.stack([f(inp["c"][b]), f(inp["c_ctx"])], axis=-1)
    d["cvec"] = np.ascontiguousarray(cv.reshape(8, 128, 2).transpose(1, 0, 2))
    return d


def declare_mlstm_inputs(self):
    self.m_w_in = self.inp("m_w_in", [2, 128, 8, 3 * E])
    self.m_conv_w = self.inp("m_conv_w", [2, 128, 16, 9])
    self.m_vecs = self.inp("m_vecs", [2, 128, 3, 16])
    self.m_w_q = self.inp("m_w_q", [2, 4, 128, 4, DH])
    self.m_w_k = self.inp("m_w_k", [2, 4, 128, 4, DH])
    self.m_w_if = self.inp("m_w_if", [2, 128, 3, 16, 16])
    self.m_bif = self.inp("m_bif", [2, 16])
    self.m_w_out = self.inp("m_w_out", [2, 128, 16, D])
    self.c_tri = self.inp("c_tri", [128, 3, 128])
    self.c_mask = self.inp("c_mask", [128, 2, 128])


KB.declare_mlstm_inputs = declare_mlstm_inputs


def host_shared_mlstm(inp):
    f = lambda a: np.asarray(a, dtype=np.float32)
    sh = {}
    sh["m_w_in"] = np.stack([_pk(f(inp["m_w_in"][l]), 8) for l in range(2)])
    cw = f(inp["m_conv_w"]).reshape(2, 9, E)
    sh["m_conv_w"] = np.ascontiguousarray(cw.reshape(2, 9, 16, 128).transpose(0, 3, 2, 1))
    vec = np.stack([f(inp["m_conv_b"]), f(inp["m_ln_w"]), f(inp["m_skip"])], axis=1)
    sh["m_vecs"] = np.ascontiguousarray(vec.reshape(2, 3, 16, 128).transpose(0, 3, 1, 2))
    sh["m_w_q"] = np.stack([np.stack([_pk(f(inp["m_w_q"][l][h]), 4) for h in range(4)]) for l in range(2)])
    sh["m_w_k"] = np.stack([np.stack([_pk(f(inp["m_w_k"][l][h]), 4) for h in range(4)]) for l in range(2)])
    wif = f(inp["m_w_if"])
    wif = wif.reshape(2, 2, 3, 16, 128, 8).transpose(0, 4, 2, 3, 1, 5)
    sh["m_w_if"] = np.ascontiguousarray(wif.reshape(2, 128, 3, 16, 16))
    bif = np.concatenate([f(inp["m_b_i"]), f(inp["m_b_f"])], axis=-1)
    sh["m_bif"] = np.ascontiguousarray(bif.reshape(2, 16))
    sh["m_w_out"] = np.stack([_pk(f(inp["m_w_out"][l]), 16) for l in range(2)])
    s_ = np.arange(128)[:, None]
    j_ = np.arange(128)[None, :]
    mf = (s_ <= j_).astype(np.float32)
    mb = (s_ >= j_).astype(np.float32)
    sh["c_tri"] = np.ascontiguousarray(np.stack([-mf, -mb, -np.ones((128, 128), np.float32)], axis=1))
    sh["c_mask"] = np.ascontiguousarray(np.stack([mf, mb], axis=1))
    return sh


def out_stage(self, yT, off, stream, t, fourier, gb, src, key_in, key_out, dst, wout, xt_pool, xn_pool, pb):
    P = self.P
    xt = xt_pool.next()
    self.load_rows("sp", xt, stream, t, fourier, src, key_in)
    xn = xn_pool.next().begin()
    for half in range(2):
        po = pb.next()

        def mmo(e, po=po, half=half):
            for c in range(16):
                ins = e.matmul(po[:, :], yT[:, c, off:off + 128], wout[:, c, half * 512:(half + 1) * 512],
                               start=(c == 0), stop=(c == 15))
            return ins
        P.op("pe", mmo, reads=[yT, wout], writes=[po])
        hs = slice(half * 512, (half + 1) * 512)
        P.op("dve", lambda e, po=po, hs=hs: e.tensor_tensor(xn[:, hs], po[:, :], gb[:, hs], ALU.mult),
             reads=[po, gb], pw=[xn])
    P.op("pool", lambda e: e.tensor_tensor(xn[:], xn[:], xt[:], ALU.add), reads=[xt], writes=[xn])
    self.store_rows("sp", xn, stream, t, fourier, dst, key_out)


KB.out_stage = out_stage


def mlstm_layer(self, li, x_src, c_src, upd_ctx):
    P, A = self.P, self.A
    hT = self.hT
    N, T = self.N, self.T
    R = N // GW
    j = li // 2
    NTT = T // 128
    pb = Pool(self.pbank)
    S = self.scr.get(j)
    if S is None:
        S = {}
        S["OZX"] = self.scratch("OZX%d" % j, [3, NTT, 128, 16, 128], BF16)
        S["QT"] = self.scratch("QT%d" % j, [NTT, 128, 4, 4, 128], BF16)
        S["KT"] = self.scratch("KT%d" % j, [NTT, 128, 4, 4, 128], BF16)
        S["Kt"] = self.scratch("Kt%d" % j, [T, E], BF16)
        S["Vt"] = self.scratch("Vt%d" % j, [T, E], BF16)
        S["HF"] = self.scratch("HF%d" % j, [T, E], BF16)
        self.scr[j] = S
    m0 = A.mark()
    G = self.G
    bif = self.bif
    P.dma("sp", bif[:], self.m_bif[j].partition_broadcast(128), writes=[bif])
    mB = A.mark()
    cw = A.alloc("cw", [16, 9], F32)
    vecs = A.alloc("vecs", [3, 16], F32)
    P.dma("sp", cw[:], self.m_conv_w[j], writes=[cw])
    P.dma("sp", vecs[:], self.m_vecs[j], writes=[vecs])
    wif32 = A.alloc("wif32", [3, 16, 16], F32)
    wif = A.alloc("wif", [3, 16, 16], BF16)
    P.dma("sp", wif32[:], self.m_w_if[j], writes=[wif32])
    P.op("pool", lambda e: e.tensor_copy(wif[:], wif32[:]), reads=[wif32], writes=[wif])
    stage = Pool([A.alloc("mstg%d" % i, [1024], F32) for i in range(2)])
    wb_pool = Pool([A.alloc("mwb%d" % i, [8, 512], BF16) for i in range(2)])
    wq = A.alloc("wq", [4, DH], BF16)
    wk = A.alloc("wk", [4, DH], BF16)
    LP = 260 + (R + 2) * 68
    xmpad = A.alloc("xmpad", [LP], BF16)
    vTc = A.alloc("vTc", [T], BF16)
    xcv = A.alloc("xcv", [4, T], BF16)
    dg_pool = Pool([A.alloc("dg%d" % i, [9, 128], BF16) for i in range(2)])
    tl_pool = Pool([A.alloc("tl%d" % i, [512], BF16) for i in range(4)])
    t32_pool = Pool([A.alloc("t32_%d" % i, [512], F32) for i in range(2)])
    qt_pool = Pool([A.alloc("qt%d" % i, [4, 512], BF16) for i in range(4)])
    xl = xmpad.ap[:, 260:LP].rearrange("p (r c) -> p r c", c=68)
    P.op("pool", lambda e: e.memset(xmpad[:], 0.0), writes=[xmpad])
    ttiles = [(0, 256)] + [(CTX + i * 512, 512) for i in range(N // 512)]
    first_g = [True] * NTT
    ev = [0]

    def evac(out_ap, in_ap, reads, pw=(), writes=(), func=None, scale=None, bias=None):
        ev[0] += 1
        if func is not None or ev[0] % 2 == 0:
            kw = {}
            if scale is not None:
                kw["scale"] = scale
            if bias is not None:
                kw["bias"] = bias
            f = func if func is not None else AF.Copy
            return P.op("act", lambda e: e.activation(out_ap, in_ap, f, **kw), reads=reads, pw=pw, writes=writes)
        if scale is not None:
            return P.op("dve", lambda e: e.tensor_scalar(out_ap, in_ap, scale, None, ALU.mult), reads=reads, pw=pw, writes=writes)
        return P.op("dve", lambda e: e.tensor_copy(out_ap, in_ap), reads=reads, pw=pw, writes=writes)

    def gate_acc(pm, tt):
        if first_g[tt]:
            first_g[tt] = False
            P.op("dve", lambda e: e.tensor_tensor(G[:, tt, :], pm[:, 0:16], bif[:], ALU.add), reads=[pm, bif], pw=[G])
        else:
            P.op("dve", lambda e: e.tensor_tensor(G[:, tt, :], pm[:, 0:16], G[:, tt, :], ALU.add), reads=[pm], pw=[G])

    G.begin()
    import os
    stop = os.environ.get("MK_STOP", "")
    for hd in range(4):
        if stop.startswith("S") and hd > 0:
            return
        self.load_w(wq, self.m_w_q[j, hd], 4, DH, stage)
        self.load_w(wk, self.m_w_k[j, hd], 4, DH, stage)
        wxm = wb_pool.next()
        self.load_w(wxm, self.m_w_in[j, :, :, hd * 512:(hd + 1) * 512], 8, 512, stage)
        xcv.begin()
        if stop == "S0":
            return
        for cc in range(4):
            c = hd * 4 + cc
            xmpad.begin()
            vTc.begin()
            for (pos, w) in ttiles:
                pm = pb.next()

                def mm(e, pm=pm, cc=cc, pos=pos, w=w, wxm=wxm):
                    for kc in range(8):
                        ins = e.matmul(pm[:, 0:w], wxm[:, kc, cc * 128:(cc + 1) * 128], hT[:, kc, pos:pos + w],
                                       start=(kc == 0), stop=(kc == 7))
                    return ins
                P.op("pe", mm, reads=[wxm, hT], writes=[pm])
                if pos == 0:
                    ov = xmpad.ap[:, 2:258]
                    iv = pm[:, 0:256]
                else:
                    r0 = (pos - CTX) // 64
                    ov = xl[:, r0 + 1:r0 + 9, 2:66]
                    iv = pm.ap.rearrange("p (r c) -> p r c", c=64)
                P.op("act", lambda e, ov=ov, iv=iv: e.activation(ov, iv, AF.Copy), reads=[pm], pw=[xmpad])
                P.op("dve", lambda e, pm=pm, pos=pos, w=w: e.tensor_copy(vTc[:, pos:pos + w], pm[:, 0:w]), reads=[pm], pw=[vTc])
            if stop == "S1":
                return
            for tt in range(NTT):
                pm = pb.next()
                P.op("pe", lambda e, pm=pm, tt=tt, c=c: e.matmul(pm[:, 0:16], vTc[:, tt * 128:(tt + 1) * 128], wif[:, 2, c, :],
                                                                start=True, stop=True), reads=[vTc, wif], writes=[pm])
                gate_acc(pm, tt)
            if stop == "S2":
                return
            dg = dg_pool.next()

            def mkdg(e, dg=dg, c=c):
                for tap in range(9):
                    ins = e.tensor_scalar(dg[:, tap, :], self.ident[:], cw[:, c, tap:tap + 1], None, ALU.mult)
                return ins
            P.op("dve", mkdg, reads=[self.ident, cw], writes=[dg])
            for (pos, w) in ttiles:
                pm = pb.next()
                if pos == 0:
                    def mmc(e, pm=pm, dg=dg):
                        for k, dc in enumerate((-1, 0, 1)):
                            ins = e.matmul(pm[:, 0:256], dg[:, 3 + k, :], xmpad.ap[:, 2 + dc:258 + dc],
                                           start=(k == 0), stop=(k == 2))
                        return ins
                else:
                    r0 = (pos - CTX) // 64

                    def mmc(e, pm=pm, dg=dg, r0=r0):
                        n = 0
                        for dr in (-1, 0, 1):
                            for dc in (-1, 0, 1):
                                ins = e.matmul(pm[:, :], dg[:, 3 * (dr + 1) + (dc + 1), :],
                                               xl[:, r0 + 1 + dr:r0 + 9 + dr, 2 + dc:66 + dc],
                                               start=(n == 0), stop=(n == 8))
                                n += 1
                        return ins
                P.op("pe", mmc, reads=[dg, xmpad], writes=[pm])
                P.op("act", lambda e, pm=pm, cc=cc, pos=pos, w=w, c=c: e.activation(
                    xcv[:, cc, pos:pos + w], pm[:, 0:w], AF.Silu, bias=vecs[:, 0, c:c + 1]), reads=[pm, vecs], pw=[xcv])
                if stop == "S3":
                    continue
                tl = tl_pool.next()
                P.op("pool", lambda e, tl=tl, cc=cc, pos=pos, w=w, c=c: e.tensor_scalar(
                    tl[:, 0:w], xcv[:, cc, pos:pos + w], vecs[:, 2, c:c + 1], 1.0, ALU.mult, ALU.mult), reads=[xcv, vecs], writes=[tl])
                tt0 = pos // 128
                nt = w // 128
                P.dma("sp", S["OZX"][2, tt0:tt0 + nt, :, c, :].rearrange("t p k -> p t k"),
                      tl.ap[:, 0:w].rearrange("p (t k) -> p t k", k=128), reads=[tl],
                      writes=[self.dbuf(("OZX", j, 2, c, pos))])
        if stop in ("S3", "S4"):
            return
        for tt in range(NTT):
            pm = pb.next()

            def mmv(e, pm=pm, tt=tt, wxm=wxm):
                for kc in range(8):
                    ins = e.matmul(pm[:, :], hT[:, kc, tt * 128:(tt + 1) * 128], wxm[:, kc, :],
                                   start=(kc == 0), stop=(kc == 7))
                return ins
            P.op("pe", mmv, reads=[wxm, hT], writes=[pm])
            tl = tl_pool.next()
            evac(tl[:, :], pm[:, :], [pm], writes=[tl])
            P.dma("sp", S["Vt"][tt * 128:(tt + 1) * 128, hd * 512:(hd + 1) * 512], tl[:, :], reads=[tl],
                  writes=[self.dbuf(("Vt", j, tt, hd))])
        for kind in range(2):
            wz = wb_pool.next()
            col0 = E * (kind + 1) + hd * 512
            self.load_w(wz, self.m_w_in[j, :, :, col0:col0 + 512], 8, 512, stage)
            for cc in range(4):
                c = hd * 4 + cc
                for (pos, w) in ttiles:
                    pm = pb.next()

                    def mm(e, pm=pm, cc=cc, pos=pos, w=w, wz=wz):
                        for kc in range(8):
                            ins = e.matmul(pm[:, 0:w], wz[:, kc, cc * 128:(cc + 1) * 128], hT[:, kc, pos:pos + w],
                                           start=(kc == 0), stop=(kc == 7))
                        return ins
                    P.op("pe", mm, reads=[wz, hT], writes=[pm])
                    tl = tl_pool.next()
                    if kind == 0:
                        t32 = t32_pool.next()
                        P.op("act", lambda e, t32=t32, pm=pm, w=w: e.activation(t32[:, 0:w], pm[:, 0:w], AF.Sigmoid),
                             reads=[pm], writes=[t32])
                        P.op("pool", lambda e, tl=tl, t32=t32, w=w, c=c: e.tensor_scalar(
                            tl[:, 0:w], t32[:, 0:w], vecs[:, 1, c:c + 1], 1.0, ALU.mult, ALU.mult), reads=[t32, vecs], writes=[tl])
                    else:
                        P.op("act", lambda e, tl=tl, pm=pm, w=w: e.activation(tl[:, 0:w], pm[:, 0:w], AF.Silu),
                             reads=[pm], writes=[tl])
                    tt0 = pos // 128
                    nt = w // 128
                    P.dma("sp", S["OZX"][kind, tt0:tt0 + nt, :, c, :].rearrange("t p k -> p t k"),
                          tl.ap[:, 0:w].rearrange("p (t k) -> p t k", k=128), reads=[tl],
                          writes=[self.dbuf(("OZX", j, kind, c, pos))])
        for (pos, w) in ttiles:
            tt0 = pos // 128
            nt = w // 128
            qk = []
            for which, wmat, scl in ((0, wq, None), (1, wk, DH ** -0.5)):
                qt = qt_pool.next().begin()
                for ec in range(4):
                    pm = pb.next()

                    def mmq(e, pm=pm, ec=ec, pos=pos, w=w, wmat=wmat):
                        for dc in range(4):
                            ins = e.matmul(pm[:, 0:w], wmat[:, dc, ec * 128:(ec + 1) * 128], xcv[:, dc, pos:pos + w],
                                           start=(dc == 0), stop=(dc == 3))
                        return ins
                    P.op("pe", mmq, reads=[wmat, xcv], writes=[pm])
                    evac(qt[:, ec, 0:w], pm[:, 0:w], [pm], pw=[qt], scale=scl)
                dst = S["QT"] if which == 0 else S["KT"]
                for ec in range(4):
                    P.dma("sp", dst[tt0:tt0 + nt, :, hd, ec, :].rearrange("t p k -> p t k"),
                          qt.ap[:, ec, 0:w].rearrange("p (t k) -> p t k", k=128), reads=[qt],
                          writes=[self.dbuf(("QK", j, which, hd, ec, pos))])
                qk.append(qt)
            qt, kt = qk
            for sub in range(nt):
                tt = tt0 + sub
                sl = slice(sub * 128, (sub + 1) * 128)
                pm = pb.next()

                def mmk(e, pm=pm, sl=sl, pos=pos):
                    for dc in range(4):
                        ins = e.matmul(pm[:, :], xcv[:, dc, pos + sl.start:pos + sl.stop], wk[:, dc, :],
                                       start=(dc == 0), stop=(dc == 3))
                    return ins
                P.op("pe", mmk, reads=[wk, xcv], writes=[pm])
                tl = tl_pool.next()
                evac(tl[:, :], pm[:, :], [pm], writes=[tl], scale=DH ** -0.5)
                P.dma("sp", S["Kt"][tt * 128:(tt + 1) * 128, hd * 512:(hd + 1) * 512], tl[:, :], reads=[tl],
                      writes=[self.dbuf(("Kt", j, tt, hd))])
                pg = pb.next()

                def mmg(e, pg=pg, sl=sl, qt=qt, kt=kt, hd=hd):
                    n = 0
                    for src, buf in ((0, qt), (1, kt)):
                        for ec in range(4):
                            ins = e.matmul(pg[:, 0:16], buf[:, ec, sl], wif[:, src, hd * 4 + ec, :],
                                           start=(n == 0), stop=(n == 7))
                            n += 1
                    return ins
                P.op("pe", mmg, reads=[qt, kt, wif], writes=[pg])
                gate_acc(pg, tt)
    self.barrier()
    A.release(self.m_hT)
    import os
    self.stop = os.environ.get("MK_STOP", "")
    if self.stop == "B":
        return
    self.mlstm_scan(li, j, S, G, x_src, c_src, upd_ctx, pb)


KB.mlstm_layer = mlstm_layer


def mlstm_scan(self, li, j, S, G, x_src, c_src, upd_ctx, pb):
    P, A = self.P, self.A
    N, T = self.N, self.T
    NTT = T // 128
    NG = NTT * 4
    tri = A.alloc("tri", [3, 128], F32)
    mask = A.alloc("mask", [2, 128], F32)
    P.dma("sp", tri[:], self.c_tri[:, :, :], writes=[tri])
    P.dma("sp", mask[:], self.c_mask[:, :, :], writes=[mask])
    SP = A.alloc("SP", [2, NTT, 4], F32)
    TA = A.alloc("TA", [2, NTT, 4], F32)
    AA = A.alloc("AA", [2, NTT, 4], F32)
    WW = A.alloc("WW", [2, NTT, 4], F32)
    ENB = A.alloc("ENB", [2, NTT, 4], F32)
    EBL = A.alloc("EBL", [2, NTT, 4], F32)
    Gv = G.ap.rearrange("p t (r g) -> p r t g", r=2)
    for b_ in (SP, TA, AA, WW, ENB, EBL):
        b_.begin()
    for r in range(2):
        fpre = Gv[:, r, :, 4:8]
        liv = Gv[:, r, :, 0:4]
        P.op("act", lambda e, r=r, fpre=fpre: e.activation(SP[:, r, :, :], fpre, AF.Exp, scale=-1.0), reads=[G], pw=[SP])
        P.op("act", lambda e, r=r: e.activation(SP[:, r, :, :], SP[:, r, :, :], AF.Ln, bias=1.0), reads=[SP], pw=[SP])
        pbm = pb.next()
        pbl = pb.next()
        spf = SP[:, r, :, :].rearrange("p t h -> p (t h)")
        P.op("pe", lambda e, pbm=pbm, spf=spf, r=r: e.matmul(pbm[:, 0:NG], tri[:, r, :], spf, start=True, stop=True),
             reads=[tri, SP], writes=[pbm])
        P.op("pe", lambda e, pbl=pbl, spf=spf: e.matmul(pbl[:, 0:NG], tri[:, 2, :], spf, start=True, stop=True),
             reads=[tri, SP], writes=[pbl])
        bv = pbm[:, 0:NG].rearrange("p (t h) -> p t h", h=4)
        blv = pbl[:, 0:NG].rearrange("p (t h) -> p t h", h=4)
        P.op("dve", lambda e, r=r, liv=liv, bv=bv: e.tensor_tensor(TA[:, r, :, :], liv, bv, ALU.subtract), reads=[G, pbm], pw=[TA])
        P.op("act", lambda e, r=r: e.activation(AA[:, r, :, :], TA[:, r, :, :], AF.Exp), reads=[TA], pw=[AA])
        P.op("dve", lambda e, r=r, blv=blv: e.tensor_tensor(TA[:, r, :, :], TA[:, r, :, :], blv, ALU.add), reads=[pbl, AA], pw=[TA])
        P.op("act", lambda e, r=r: e.activation(WW[:, r, :, :], TA[:, r, :, :], AF.Exp), reads=[TA], pw=[WW])
        P.op("act", lambda e, r=r, bv=bv: e.activation(ENB[:, r, :, :], bv, AF.Exp, scale=-1.0), reads=[pbm], pw=[ENB])
        P.op("act", lambda e, r=r, blv=blv: e.activation(EBL[:, r, :, :], blv, AF.Exp), reads=[pbl], pw=[EBL])
    if "dbgG" in self.dbg:
        dG = self.outp("dbgG", [128, NTT, 16])
        P.dma("sp", dG[:, :, :], G[:], reads=[G])
        for nm, bf_ in (("dbgAA", AA), ("dbgWW", WW), ("dbgENB", ENB), ("dbgEBL", EBL), ("dbgSP", SP)):
            dd = self.outp(nm, [128, 2, NTT, 4])
            P.dma("sp", dd[:, :, :, :], bf_[:], reads=[bf_])
    if self.stop == "G":
        return
    mS = A.mark()
    keyx = "xs" if x_src is self.xs else "xin"
    keyc = "xc" if c_src is self.xcs else "cin"
    for r in range(2):
        A.release(mS)
        C32 = A.alloc("C32", [4, 4, 513], F32)
        Cbf = A.alloc("Cbf", [4, 4, 514], BF16)
        P.op("pool", lambda e: e.memset(C32[:], 0.0))
        tk0 = P.op("pool", lambda e: e.memset(Cbf[:], 0.0))
        nld = 3 if r == 0 else 2
        q_pool = Pool([A.alloc("qc%d" % i, [16, 128], BF16) for i in range(nld)])
        k_pool = Pool([A.alloc("kc%d" % i, [16, 128], BF16) for i in range(nld)])
        K_pool = Pool([A.alloc("Kc%d" % i, [E], BF16) for i in range(nld)])
        V_pool = Pool([A.alloc("Vc%d" % i, [E], BF16) for i in range(nld)])
        WT_pool = Pool([A.alloc("WT%d" % i, [128], BF16) for i in range(2)])
        Va_pool = Pool([A.alloc("Va%d" % i, [514], BF16) for i in range(2)])
        Vw_pool = Pool([A.alloc("Vw%d" % i, [514], BF16) for i in range(2)])
        rr_pool = Pool([A.alloc("rr%d" % i, [2], F32) for i in range(4)])
        hF_pool = Pool([A.alloc("hF%d" % i, [4, 512], BF16) for i in range(2 if r == 0 else 1)])
        if r == 1:
            h32 = A.alloc("h32", [4, 512], F32)
            hn = A.alloc("hn", [4, 512], BF16)
            stats = A.alloc("stats", [4, 6], F32)
            mv = A.alloc("mv", [4, 2], F32)
            rs = A.alloc("rs", [4], F32)
            t1 = A.alloc("t1", [16, 128], F32)
            yT = A.alloc("yTm", [16, 128], BF16)
            ozx = [A.alloc("ozx%d" % i, [16, 128], BF16) for i in range(3)]
            wout = A.alloc("mwout", [16, D], BF16)
            stage = Pool([A.alloc("sstg%d" % i, [512], F32) for i in range(2)])
            self.load_w(wout, self.m_w_out[j], 16, D, stage, engs=("pool",))
            xt_pool = Pool([A.alloc("mx%d" % i, [D], F32) for i in range(2)])
            xn_pool = Pool([A.alloc("mxn%d" % i, [D], F32) for i in range(1)])
        order = [0, 1] + list(range(2, NTT)) if r == 0 else [1, 0] + list(range(NTT - 1, 1, -1))
        ctok = {hd: tk0 for hd in range(4)}
        for tt in order:
            is_ctx = tt < 2
            emit = (not is_ctx) or upd_ctx
            Kc = K_pool.next()
            Vc = V_pool.next()
            P.dma("sp", Kc[:], S["Kt"][tt * 128:(tt + 1) * 128, :], writes=[Kc])
            P.dma("sp", Vc[:], S["Vt"][tt * 128:(tt + 1) * 128, :], writes=[Vc])
            if emit:
                qc = q_pool.next()
                kc_ = k_pool.next()
                P.dma("sp", qc[:], S["QT"][tt].rearrange("p h e k -> p (h e) k"), writes=[qc])
                P.dma("sp", kc_[:], S["KT"][tt].rearrange("p h e k -> p (h e) k"), writes=[kc_])
                hF = hF_pool.next()
                if r == 1:
                    P.dma("sp", hF[:], S["HF"][tt * 128:(tt + 1) * 128, :].rearrange("p (h v) -> p h v", h=4), writes=[hF])
                    h32.begin()
                else:
                    hF.begin()
            for hd in range(4):
                a_s = AA[:, r, tt, hd:hd + 1]
                w_s = WW[:, r, tt, hd:hd + 1]
                Vw = Vw_pool.next().begin()
                tok_mmn = None
                small = pb.next()
                ps = small
                pd = Buf(small.ap[:, 128:132])
                psn = Buf(small.ap[:, 132:136])
                P.op("pool", lambda e, Vw=Vw, Vc=Vc, hd=hd, w_s=w_s: e.tensor_scalar(
                    Vw[:, 0:512], Vc[:, hd * 512:(hd + 1) * 512], w_s, 1.0, ALU.mult, ALU.mult), reads=[Vc, WW], pw=[Vw])
                P.op("pool", lambda e, Vw=Vw, w_s=w_s: e.tensor_copy(Vw[:, 512:513], w_s), reads=[WW], pw=[Vw])
                if emit:
                    Va = Va_pool.next().begin()
                    P.op("act", lambda e, Va=Va, Vc=Vc, hd=hd, a_s=a_s: e.activation(
                        Va[:, 0:512], Vc[:, hd * 512:(hd + 1) * 512], AF.Copy, scale=a_s), reads=[Vc, AA], pw=[Va])
                    P.op("act", lambda e, Va=Va, a_s=a_s: e.activation(Va[:, 512:513], a_s, AF.Copy), reads=[AA], pw=[Va])

                    def mms(e, ps=ps, qc=qc, kc_=kc_, hd=hd):
                        for ec in range(4):
                            ins = e.matmul(ps[:, 0:128], kc_[:, hd * 4 + ec, :], qc[:, hd * 4 + ec, :],
                                           start=(ec == 0), stop=(ec == 3))
                        return ins
                    P.op("pe", mms, reads=[qc, kc_], writes=[small])
                    WT = WT_pool.next()
                    P.op("dve", lambda e, WT=WT, ps=ps, r=r: e.tensor_tensor(WT[:], ps[:, 0:128], mask[:, r, :], ALU.mult),
                         reads=[small, mask], writes=[WT])
                    pn = pb.next()

                    def mmn(e, pn=pn, pd=pd, WT=WT, Va=Va, qc=qc, hd=hd):
                        e.matmul(pn[:, :], WT[:], Va[:, 0:512], start=True, stop=False)
                        for kc in range(4):
                            e.matmul(pn[:, :], qc[:, hd * 4 + kc, :], Cbf[:, hd, kc, 0:512], start=False, stop=(kc == 3))
                        e.matmul(pd[:, 0:1], WT[:], Va[:, 512:513], start=True, stop=False)
                        for kc in range(4):
                            ins = e.matmul(pd[:, 0:1], qc[:, hd * 4 + kc, :], Cbf[:, hd, kc, 512:513], start=False, stop=(kc == 3))
                        return ins
                    tok_mmn = P.op("pe", mmn, reads=[WT, Va, qc], writes=[pn, small], extra=[ctok.get(hd)])
                    rr = rr_pool.next()
                    P.op("act", lambda e, rr=rr, pd=pd: e.activation(rr[:, 0:1], pd[:, 0:1], AF.Abs), reads=[small], writes=[rr])
                    P.op("dve", lambda e, rr=rr, tt=tt, hd=hd, r=r: e.tensor_scalar(
                        rr[:, 0:1], rr[:, 0:1], ENB[:, r, tt, hd:hd + 1], None, ALU.max), reads=[rr, ENB], writes=[rr])
                    P.op("dve", lambda e, rr=rr: e.reciprocal(rr[:, 1:2], rr[:, 0:1]), reads=[rr], writes=[rr])
                    if r == 0:
                        P.op("act", lambda e, hF=hF, pn=pn, rr=rr, hd=hd: e.activation(hF[:, hd, :], pn[:, :], AF.Copy, scale=rr[:, 1:2]),
                             reads=[pn, rr], pw=[hF])
                    else:
                        P.op("dve", lambda e, hF=hF, pn=pn, rr=rr, hd=hd: e.scalar_tensor_tensor(
                            h32[:, hd, :], pn[:, :], rr[:, 1:2], hF[:, hd, :], ALU.mult, ALU.add), reads=[pn, rr, hF], pw=[h32])
                last = (tt == order[-1])
                if not last:
                    ebl = EBL[:, r, tt, hd:hd + 1]
                    pkv = [pb.next() for _ in range(4)]

                    def mmkv(e, pkv=pkv, psn=psn, Kc=Kc, Vw=Vw, hd=hd):
                        for kc in range(4):
                            e.matmul(pkv[kc][:, :], Kc[:, hd * 512 + kc * 128:hd * 512 + (kc + 1) * 128], Vw[:, 0:512],
                                     start=True, stop=True)
                        for kc in range(4):
                            ins = e.matmul(psn[:, kc:kc + 1], Kc[:, hd * 512 + kc * 128:hd * 512 + (kc + 1) * 128], Vw[:, 512:513],
                                           start=True, stop=True)
                        return ins
                    P.op("pe", mmkv, reads=[Kc, Vw], writes=pkv + [small])
                    toks = []
                    for kc in range(4):
                        tk = P.op("dve", lambda e, kc=kc, pkv=pkv, hd=hd, ebl=ebl: e.scalar_tensor_tensor(
                            C32[:, hd, kc, 0:512], C32[:, hd, kc, 0:512], ebl, pkv[kc][:, :], ALU.mult, ALU.add),
                            reads=[pkv[kc], EBL], extra=[ctok.get(hd)])
                        toks.append(tk)
                    tk = P.op("dve", lambda e, psn=psn, hd=hd, ebl=ebl: e.scalar_tensor_tensor(
                        C32[:, hd, :, 512], C32[:, hd, :, 512], ebl, psn[:, 0:4], ALU.mult, ALU.add),
                        reads=[small, EBL], extra=[ctok.get(hd)])
                    toks.append(tk)
                    ctok[hd] = P.op("act", lambda e, hd=hd: e.activation(Cbf[:, hd, :, 0:513], C32[:, hd, :, :], AF.Copy),
                                    extra=toks + [ctok.get(hd), tok_mmn])
            if emit and r == 0:
                P.dma("sp", S["HF"][tt * 128:(tt + 1) * 128, :].rearrange("p (h v) -> p h v", h=4), hF[:], reads=[hF],
                      writes=[self.dbuf(("HF", j, tt))])
            if emit and r == 1:
                for i in range(3):
                    P.dma("sp", ozx[i][:], S["OZX"][i, tt], writes=[ozx[i]])
                P.op("dve", lambda e: [e.bn_stats(stats[:, hd, :], h32[:, hd, :]) for hd in range(4)][-1], reads=[h32], writes=[stats])
                P.op("dve", lambda e: [e.bn_aggr(mv[:, hd, :], stats[:, hd, :]) for hd in range(4)][-1], reads=[stats], writes=[mv])
                P.op("act", lambda e: e.activation(rs[:], mv[:, :, 1], AF.Sqrt, bias=EPS), reads=[mv], writes=[rs])
                P.op("dve", lambda e: e.reciprocal(rs[:], rs[:]), reads=[rs], writes=[rs])
                hn.begin()
                for hd in range(4):
                    eng = "dve"
                    P.op(eng, lambda e, hd=hd: e.tensor_scalar(hn[:, hd, :], h32[:, hd, :], mv[:, hd, 0:1], rs[:, hd:hd + 1],
                                                               ALU.subtract, ALU.mult), reads=[h32, mv, rs], pw=[hn])
                t1.begin()
                for half in range(2):
                    ptb = pb.next()
                    ptv = ptb.ap.bitcast(BF16)

                    def tr(e, ptv=ptv, half=half):
                        for c8 in range(8):
                            c = half * 8 + c8
                            ins = e.transpose(ptv[:, c8 * 128:(c8 + 1) * 128], hn[:, c // 4, (c % 4) * 128:(c % 4 + 1) * 128], self.ident[:])
                        return ins
                    P.op("pe", tr, reads=[hn, self.ident], writes=[ptb])
                    pv = ptv[:, 0:1024].rearrange("p (c t) -> p c t", c=8)
                    P.op("dve", lambda e, pv=pv, half=half: e.tensor_tensor(t1[:, half * 8:(half + 1) * 8, :], pv,
                                                                             ozx[0][:, half * 8:(half + 1) * 8, :], ALU.mult),
                         reads=[ptb, ozx[0]], pw=[t1])
                P.op("pool", lambda e: e.tensor_tensor(t1[:], t1[:], ozx[2][:], ALU.add), reads=[ozx[2]], writes=[t1])
                P.op("pool", lambda e: e.tensor_tensor(yT[:], t1[:], ozx[1][:], ALU.mult), reads=[t1, ozx[1]], writes=[yT])
                if is_ctx:
                    self.out_stage(yT, 0, "ctx", tt, False, self.gc_bc, c_src, keyc, "xc", self.xcs, wout, xt_pool, xn_pool, pb)
                else:
                    self.out_stage(yT, 0, "lat", tt - 2, False, self.g_bc, x_src, keyx, "xs", self.xs, wout, xt_pool, xn_pool, pb)
        self.barrier()
        if self.stop == "P1":
            return


KB.mlstm_scan = mlstm_scan


_CACHE = {}
LAUNCH_GROUPS = ((0, 1, 2, 3),)


def _get_prog(N, layers, final):
    key = (N, layers, final)
    if key not in _CACHE:
        kb = KB(N=N, layers=layers, final=final, dbg=("xs", "xcs"))
        nc = kb.build()
        _CACHE[key] = (kb, nc)
    return _CACHE[key]


def kernel(**inputs):
    x = np.asarray(inputs["x"], dtype=np.float32)
    B, N, _ = x.shape
    sh = host_shared(inputs)
    cores = [host_core(inputs, b) for b in range(B)]
    out = None
    for gi, layers in enumerate(LAUNCH_GROUPS):
        final = (3 in layers)
        kb, nc = _get_prog(N, tuple(layers), final)
        in_maps = []
        for b in range(B):
            m = dict(sh)
            m.update(cores[b])
            in_maps.append({k: v for k, v in m.items() if k in kb.din})
        res = run_bass_kernel_spmd(nc, in_maps, core_ids=list(range(B)))
        for b in range(B):
            r = res.results[b]
            if final:
                continue
            cores[b]["x"] = np.asarray(r["xs"], dtype=np.float32)
            if any(l < 2 for l in layers):
                cores[b]["ctx"] = np.asarray(r["xcs"], dtype=np.float32)
        if final:
            out = np.stack([np.asarray(res.results[b]["out"], dtype=np.float32) for b in range(B)], axis=0)
    return out
```

```python
import math
from contextlib import ExitStack

import numpy as np
import ml_dtypes
import concourse.bass as bass
import concourse.mybir as mybir
from concourse.bass_utils import run_bass_kernel_spmd

F32 = mybir.dt.float32
BF16 = mybir.dt.bfloat16
AF = mybir.ActivationFunctionType
ALU = mybir.AluOpType
AX = mybir.AxisListType

D = 1024
E = 2048
NH = 4
DH = 512
CTX = 256
GW = 64
EPS = 1e-6
NDS = 12


class Buf:
    def __init__(self, ap, name=""):
        self.ap = ap
        self.name = name
        self.ws = []
        self.r = []
        self.prev = []
        self.excl = False

    def begin(self):
        self.prev = self.ws + self.r
        self.ws = []
        self.r = []
        return self

    def __getitem__(self, k):
        return self.ap[k]


class Prog:
    CE = ("pe", "dve", "act", "pool")
    DQ = ("sp", "act", "pool")

    def __init__(self, nc, es):
        self.nc = nc
        self.es = es
        self.q = {e: [] for e in ("pe", "dve", "act", "pool", "sp")}
        self.sems = {}
        self.cnt = {}
        for e in self.CE:
            self.sems[e] = es.enter_context(nc.semaphore("s_" + e))
            self.cnt[e] = 0
        self.dq_i = {}
        for qn in self.DQ:
            self.dq_i[qn] = 0
            for i in range(NDS):
                k = ("d", qn, i)
                self.sems[k] = es.enter_context(nc.semaphore("d_%s%d" % (qn, i)))
                self.cnt[k] = 0
        self.waited = {e: {} for e in self.q}
        self.n_ops = 0

    def _needed(self, eng, toks):
        out = []
        w = self.waited[eng]
        best = {}
        for t in toks:
            if t is None:
                continue
            k, v = t
            if k == eng and eng == "pe":
                continue
            if w.get(k, 0) >= v:
                continue
            if best.get(k, 0) < v:
                best[k] = v
        for k, v in best.items():
            w[k] = v
            out.append((k, v))
        return out

    def op(self, eng, fn, reads=(), writes=(), extra=(), pw=()):
        toks = list(extra)
        for b in pw:
            toks.extend(b.prev)
        for b in reads:
            toks.extend(b.ws)
            if b.excl:
                toks.extend(t for t in b.r if t[0] != eng)
        for b in writes:
            toks.extend(b.ws)
            toks.extend(b.r)
        waits = self._needed(eng, toks)
        self.cnt[eng] += 1
        tok = (eng, self.cnt[eng])
        self.q[eng].append((waits, fn, eng, 1))
        for b in reads:
            b.r.append(tok)
        for b in writes:
            b.ws = [tok]
            b.r = []
        for b in pw:
            b.ws.append(tok)
        self.n_ops += 1
        return tok

    def dma(self, qn, out_ap, in_ap, reads=(), writes=(), extra=(), also=(), **kw):
        i = self.dq_i[qn] % NDS
        self.dq_i[qn] += 1
        k = ("d", qn, i)
        toks = list(extra)
        if self.cnt[k] > 0:
            toks.append((k, self.cnt[k]))
        for b in reads:
            toks.extend(b.ws)
        for b in writes:
            toks.extend(b.ws)
            toks.extend(b.r)
        waits = self._needed(qn, toks)
        self.cnt[k] += 16
        tok = (k, self.cnt[k])

        def fn(e, out_ap=out_ap, in_ap=in_ap, kw=kw):
            return e.dma_start(out=out_ap, in_=in_ap, **kw)

        self.q[qn].append((waits, fn, k, 16))
        for b in reads:
            b.r.append(tok)
        for b in writes:
            b.ws = [tok]
            b.r = []
        for b in also:
            b.ws.append(tok)
        return tok

    def wait_all(self, eng, toks):
        waits = self._needed(eng, toks)
        self.q[eng].append((waits, None, None, 0))

    def cut(self):
        for name in self.q:
            self.q[name].append("CUT")

    def replay(self):
        nc = self.nc
        sems = self.sems
        segs = {}
        nseg = 0
        for name, items in self.q.items():
            cur = []
            lst = [cur]
            for it in items:
                if it == "CUT":
                    cur = []
                    lst.append(cur)
                else:
                    cur.append(it)
            segs[name] = lst
            nseg = max(nseg, len(lst))
        for si in range(nseg):
            if not any(len(segs[n][si]) for n in segs if si < len(segs[n])):
                continue
            with nc.Block() as block:
                def run(eng, name, si=si):
                    for waits, fn, sk, inc in segs[name][si]:
                        for k, v in waits:
                            eng.wait_ge(sems[k], v)
                        if fn is not None:
                            ins = fn(eng)
                            ins.then_inc(sems[sk], inc)

                @block.tensor
                def _(e):
                    run(e, "pe")

                @block.vector
                def _(e):
                    run(e, "dve")

                @block.scalar
                def _(e):
                    run(e, "act")

                @block.gpsimd
                def _(e):
                    run(e, "pool")

                @block.sync
                def _(e):
                    run(e, "sp")


class Pool:
    def __init__(self, bufs):
        self.bufs = bufs
        self.i = 0

    def next(self):
        b = self.bufs[self.i % len(self.bufs)]
        self.i += 1
        return b


class Arena:
    def __init__(self, ap):
        self.ap = ap
        self.top = 0
        self.W = ap.shape[1]
        self.peak = 0

    def alloc(self, name, free_shape, dt):
        n = 1
        for s in free_shape:
            n *= s
        esz = 4 if dt == F32 else 2
        words = (n * esz + 3) // 4
        words = (words + 7) // 8 * 8
        a = self.top
        self.top += words
        self.peak = max(self.peak, self.top)
        assert self.top <= self.W, "SBUF arena overflow at %s: %d > %d" % (name, self.top, self.W)
        v = self.ap[:, a:a + words]
        if dt != F32:
            v = v.bitcast(dt)
        v = v[:, 0:n]
        if len(free_shape) == 2:
            v = v.rearrange("p (a b) -> p a b", a=free_shape[0])
        elif len(free_shape) == 3:
            v = v.rearrange("p (a b c) -> p a b c", a=free_shape[0], b=free_shape[1])
        elif len(free_shape) == 4:
            v = v.rearrange("p (a b c d) -> p a b c d", a=free_shape[0], b=free_shape[1], c=free_shape[2])
        return Buf(v, name)

    def mark(self):
        return self.top

    def release(self, m):
        self.top = m


class K:
    pass


def fourier_tile_rows(dr, t):
    v = dr.rearrange("(n1 n2) d -> n2 n1 d", n2=GW)
    return [(0, 64, v[2 * t]), (64, 128, v[2 * t + 1])]


def natural_tile_rows(dr, t):
    return [(0, 128, dr[t * 128:(t + 1) * 128, :])]


def _prod(s):
    n = 1
    for v in s:
        n *= v
    return n


class KB:
    def __init__(self, N=4096, layers=(0, 1, 2, 3), final=True, dbg=()):
        self.N = N
        self.T = CTX + N
        self.layers = tuple(layers)
        self.final = final
        self.dbg = tuple(dbg)
        self.nc = bass.Bass("TRN2", target_bir_lowering=False)
        self.din = {}
        self.es = ExitStack()

    def inp(self, name, shape, dt=F32):
        t = self.nc.dram_tensor(name, list(shape), dt, kind="ExternalInput").ap()
        self.din[name] = t
        return t

    def outp(self, name, shape, dt=F32):
        return self.nc.dram_tensor(name, list(shape), dt, kind="ExternalOutput").ap()

    def scratch(self, name, shape, dt):
        kind = "ExternalOutput" if name in self.dbg else "Internal"
        return self.nc.dram_tensor(name, list(shape), dt, kind=kind).ap()

    def dbuf(self, key):
        b = self.dbufs.get(key)
        if b is None:
            b = Buf(None, str(key))
            self.dbufs[key] = b
        return b

    def barrier(self):
        P = self.P
        toks = [(k, v) for k, v in P.cnt.items() if v > 0]
        for e in ("pe", "dve", "act", "pool", "sp"):
            P.wait_all(e, toks)
        P.cut()

    def build(self):
        nc, es = self.nc, self.es
        N, T = self.N, self.T
        self.x_in = self.inp("x", [N, D])
        self.ctx_in = self.inp("ctx", [CTX, D])
        self.cvec = self.inp("cvec", [128, 8, 2])
        self.norm_g = self.inp("norm_g", [4, D])
        self.w_ada = self.inp("w_ada", [4, 128, 8, 3 * D])
        self.b_ada = self.inp("b_ada", [4, 3 * D])
        self.f_w_in = self.inp("f_w_in", [2, 128, 8, 2 * E])
        self.f_w_out = self.inp("f_w_out", [2, 128, 16, D])
        self.norm_f = self.inp("norm_f", [1, D])
        self.identb = self.inp("identb", [128, 128], BF16)
        self.c_cs = self.inp("c_cs", [128, 2, 512], BF16)
        self.c_a = self.inp("c_a", [128, 3, 128], BF16)
        self.c_bk = self.inp("c_bk", [128, 64, 64], BF16)
        self.c_d256 = self.inp("c_d256", [128, 2, 2, 256], BF16)
        self.declare_mlstm_inputs()
        self.out = self.outp("out", [N, D])
        self.xs = self.scratch("xs", [N, D], F32)
        self.xcs = self.scratch("xcs", [CTX, D], F32)
        self.dbufs = {}
        self.scr = {}
        with es:
            self.P = Prog(nc, es)
            sb_all = es.enter_context(nc.sbuf_tensor("sb_all", [128, 50 * 1024], F32))
            self.A = Arena(sb_all)
            self.psum = es.enter_context(nc.psum_tensor("ps_all", [128, 8 * 512], F32))
            self.pbank = [Buf(self.psum[:, b * 512:(b + 1) * 512], "bank%d" % b) for b in range(8)]
            for b_ in self.pbank:
                b_.excl = True
            A = self.A
            self.ident = A.alloc("ident", [128], BF16)
            self.P.dma("sp", self.ident[:], self.identb[:, :], writes=[self.ident])
            self.g_bc = A.alloc("g_bc", [D], F32)
            self.gc_bc = A.alloc("gc_bc", [D], F32)
            x_src, c_src = self.x_in, self.ctx_in
            for li in self.layers:
                upd_ctx = li < 2
                need_ctx = upd_ctx or (li % 2 == 0)
                fourier = (li % 2 == 1)
                mL = A.mark()
                if fourier:
                    self.fTc = A.alloc("fTc", [16, 256], F32)
                else:
                    self.G = A.alloc("G", [T // 128, 16], F32)
                    self.bif = A.alloc("bif", [16], F32)
                self.m_hT = A.mark()
                self.hT = A.alloc("hT", [8, T], BF16)
                m = A.mark()
                self.prep(li, x_src, c_src, need_ctx, fourier)
                self.barrier()
                A.release(m)
                if fourier:
                    self.fourier_layer(li, x_src, c_src, upd_ctx)
                else:
                    self.mlstm_layer(li, x_src, c_src, upd_ctx)
                self.barrier()
                A.release(mL)
                x_src = self.xs
                if upd_ctx:
                    c_src = self.xcs
            if self.final:
                self.final_norm(x_src)
            self.barrier()
            self.P.replay()
        return nc

    def declare_mlstm_inputs(self):
        pass

    def mlstm_layer(self, li, x_src, c_src, upd_ctx):
        raise NotImplementedError

    def tiles(self, fourier, need_ctx):
        out = []
        if need_ctx:
            for t in range(CTX // 128):
                out.append(("ctx", t, t * 128))
        for t in range(self.N // 128):
            out.append(("lat", t, CTX + t * 128))
        return out

    def tile_rows(self, stream, t, fourier, dr):
        if stream == "lat" and fourier:
            return fourier_tile_rows(dr, t)
        return natural_tile_rows(dr, t)

    def load_rows(self, q, buf, stream, t, fourier, dr, key):
        P = self.P
        toks = []
        db = self.dbuf((key, stream, t))
        first = True
        for (p0, p1, ap) in self.tile_rows(stream, t, fourier, dr):
            if first:
                tok = P.dma(q, buf.ap[p0:p1], ap, reads=[db], writes=[buf])
                first = False
            else:
                tok = P.dma(q, buf.ap[p0:p1], ap, reads=[db], also=[buf])
            toks.append(tok)
        return toks

    def store_rows(self, q, buf, stream, t, fourier, dr, key, extra=()):
        P = self.P
        db = self.dbuf((key, stream, t))
        toks = []
        for (p0, p1, ap) in self.tile_rows(stream, t, fourier, dr):
            toks.append(P.dma(q, ap, buf.ap[p0:p1], reads=[buf], writes=[db], extra=extra))
        return toks

    def prep(self, li, x_src, c_src, need_ctx, fourier):
        P, A = self.P, self.A
        cv = A.alloc("cv", [8, 2], F32)
        sv = A.alloc("sv", [8, 2], F32)
        ones = A.alloc("ones", [128], F32)
        sbc = A.alloc("sbc", [2, 8, 128], F32)
        mod = [A.alloc("mod%d" % s, [3 * D], F32) for s in range(2)]
        bbc = A.alloc("bbc", [3 * D], F32)
        gn = A.alloc("gn", [D], F32)
        abc = [A.alloc("abc%d" % s, [D], F32) for s in range(2)]
        wst = Pool([A.alloc("wst%d" % i, [8, 256], F32) for i in range(2)])
        P.dma("sp", cv[:], self.cvec[:, :, :], writes=[cv])
        P.dma("sp", bbc[:], self.b_ada[li].partition_broadcast(128), writes=[bbc])
        P.dma("sp", gn[:], self.norm_g[li].partition_broadcast(128), writes=[gn])
        P.op("act", lambda e: e.activation(sv[:], cv[:], AF.Silu), reads=[cv], writes=[sv])
        P.op("pool", lambda e: e.memset(ones[:], 1.0), writes=[ones])
        nstream = 2 if need_ctx else 1

        def mk_sbc(e):
            for s in range(nstream):
                for kc in range(8):
                    ins = e.tensor_scalar(sbc[:, s, kc, :], ones[:], sv[:, kc, s:s + 1], None, ALU.mult)
            return ins
        P.op("dve", mk_sbc, reads=[ones, sv], writes=[sbc])
        pb = Pool([self.pbank[0], self.pbank[1]])
        for blk in range(12):
            w = wst.next()
            P.dma("sp", w[:], self.w_ada[li, :, :, blk * 256:(blk + 1) * 256], writes=[w])
            for s in range(nstream):
                pm = pb.next()

                def mm(e, s=s, w=w, pm=pm):
                    for kc in range(8):
                        ins = e.matmul(pm[:, 0:256], sbc[:, s, kc, :], w[:, kc, :], start=(kc == 0), stop=(kc == 7))
                    return ins
                P.op("pe", mm, reads=[sbc, w], writes=[pm])
                sl = slice(blk * 256, (blk + 1) * 256)
                P.op("dve", lambda e, s=s, pm=pm, sl=sl: e.tensor_tensor(mod[s][:, sl], pm[:, 0:256], bbc[:, sl], ALU.add),
                     reads=[pm, bbc], writes=[mod[s]])
        for s in range(nstream):
            P.op("dve", lambda e, s=s: e.scalar_tensor_tensor(abc[s][:], mod[s][:, D:2 * D], 1.0, gn[:], ALU.add, ALU.mult),
                 reads=[mod[s], gn], writes=[abc[s]])
        P.op("pool", lambda e: e.tensor_copy(self.g_bc[:], mod[0][:, 2 * D:3 * D]), reads=[mod[0]], writes=[self.g_bc])
        if need_ctx:
            P.op("pool", lambda e: e.tensor_copy(self.gc_bc[:], mod[1][:, 2 * D:3 * D]), reads=[mod[1]], writes=[self.gc_bc])
        xt_pool = Pool([A.alloc("xt%d" % i, [D], F32) for i in range(3)])
        junk = A.alloc("junk", [D], F32)
        t1_pool = Pool([A.alloc("t1_%d" % i, [D], F32) for i in range(2)])
        xn_pool = Pool([A.alloc("xn%d" % i, [D], BF16) for i in range(2)])
        st_pool = Pool([A.alloc("st%d" % i, [2], F32) for i in range(4)])
        pt_pool = Pool([self.pbank[2], self.pbank[3]])
        key = "xc" if c_src is self.xcs else "cin"
        keyx = "xs" if x_src is self.xs else "xin"
        self.hT.begin()
        for ti, (stream, t, pos0) in enumerate(self.tiles(fourier, need_ctx)):
            s = 1 if stream == "ctx" else 0
            dr = c_src if stream == "ctx" else x_src
            xt = xt_pool.next()
            self.load_rows("sp", xt, stream, t, fourier, dr, key if stream == "ctx" else keyx)
            st = st_pool.next()
            P.op("act", lambda e, xt=xt, st=st: e.activation(junk[:], xt[:], AF.Square, accum_out=st[:, 0:1]),
                 reads=[xt], writes=[junk, st])
            P.op("act", lambda e, st=st: e.activation(st[:, 1:2], st[:, 0:1], AF.Sqrt, bias=EPS, scale=1.0 / D),
                 reads=[st], writes=[st])
            P.op("dve", lambda e, st=st: e.reciprocal(st[:, 1:2], st[:, 1:2]), reads=[st], writes=[st])
            t1 = t1_pool.next()
            P.op("dve", lambda e, xt=xt, st=st, t1=t1, s=s: e.scalar_tensor_tensor(
                t1[:], xt[:], st[:, 1:2], abc[s][:], ALU.mult, ALU.mult), reads=[xt, st, abc[s]], writes=[t1])
            xn = xn_pool.next()
            P.op("pool", lambda e, t1=t1, xn=xn, s=s: e.tensor_tensor(xn[:], t1[:], mod[s][:, 0:D], ALU.add),
                 reads=[t1, mod[s]], writes=[xn])
            pt = pt_pool.next()
            ptv = pt.ap.bitcast(BF16)

            def tr(e, xn=xn, ptv=ptv):
                for c in range(8):
                    ins = e.transpose(ptv[:, c * 128:(c + 1) * 128], xn[:, c * 128:(c + 1) * 128], self.ident[:])
                return ins
            P.op("pe", tr, reads=[xn, self.ident], writes=[pt])
            eng = "act" if ti % 2 == 0 else "dve"
            hv = self.hT[:, :, pos0:pos0 + 128]
            pv = ptv[:, 0:1024].rearrange("p (c t) -> p c t", c=8)
            if eng == "act":
                P.op("act", lambda e, hv=hv, pv=pv: e.activation(hv, pv, AF.Copy), reads=[pt], pw=[self.hT])
            else:
                P.op("dve", lambda e, hv=hv, pv=pv: e.tensor_copy(hv, pv), reads=[pt], pw=[self.hT])

    def final_norm(self, x_src):
        P, A = self.P, self.A
        m = A.mark()
        nf = A.alloc("nf", [D], F32)
        P.dma("sp", nf[:], self.norm_f[0].partition_broadcast(128), writes=[nf])
        xt_pool = Pool([A.alloc("fxt%d" % i, [D], F32) for i in range(3)])
        yo_pool = Pool([A.alloc("fyo%d" % i, [D], F32) for i in range(3)])
        junk = A.alloc("fjunk", [D], F32)
        st_pool = Pool([A.alloc("fst%d" % i, [2], F32) for i in range(4)])
        keyx = "xs" if x_src is self.xs else "xin"
        last = []
        for t in range(self.N // 128):
            xt = xt_pool.next()
            self.load_rows("sp", xt, "lat", t, False, x_src, keyx)
            st = st_pool.next()
            P.op("act", lambda e, xt=xt, st=st: e.activation(junk[:], xt[:], AF.Square, accum_out=st[:, 0:1]),
                 reads=[xt], writes=[junk, st])
            P.op("act", lambda e, st=st: e.activation(st[:, 1:2], st[:, 0:1], AF.Sqrt, bias=EPS, scale=1.0 / D),
                 reads=[st], writes=[st])
            P.op("dve", lambda e, st=st: e.reciprocal(st[:, 1:2], st[:, 1:2]), reads=[st], writes=[st])
            yo = yo_pool.next()
            P.op("dve", lambda e, xt=xt, st=st, yo=yo: e.scalar_tensor_tensor(
                yo[:], xt[:], st[:, 1:2], nf[:], ALU.mult, ALU.mult), reads=[xt, st, nf], writes=[yo])
            last += self.store_rows("sp", yo, "lat", t, False, self.out, "out")
        A.release(m)


def _load_w(self, dst_view, src_ap, kc, ncols, stage_pool, engs=("pool",)):
    P = self.P
    piece = max(1, stage_pool.bufs[0].ap.shape[1] // kc)
    c0 = 0
    i = 0
    while c0 < ncols:
        w = min(piece, ncols - c0)
        st = stage_pool.next()
        sv = st.ap[:, 0:kc * w].rearrange("p (k n) -> p k n", k=kc)
        P.dma("sp", sv, src_ap[:, :, c0:c0 + w], writes=[st])
        eng = engs[i % len(engs)]
        dv = dst_view.ap[:, :, c0:c0 + w] if isinstance(dst_view, Buf) else dst_view[:, :, c0:c0 + w]
        if eng == "act":
            P.op("act", lambda e, dv=dv, sv=sv: e.activation(dv, sv, AF.Copy), reads=[st], pw=[self._lw_buf])
        else:
            P.op(eng, lambda e, dv=dv, sv=sv: e.tensor_copy(dv, sv), reads=[st], pw=[self._lw_buf])
        c0 += w
        i += 1


def load_w(self, dst_buf, src_ap, kc, ncols, stage_pool, engs=("pool",), view=None):
    self._lw_buf = dst_buf
    dst_buf.begin()
    _load_w(self, dst_buf if view is None else view, src_ap, kc, ncols, stage_pool, engs)


KB.load_w = load_w


def fourier_layer(self, li, x_src, c_src, upd_ctx):
    P, A = self.P, self.A
    hT = self.hT
    N, T = self.N, self.T
    j = li // 2
    NT = N // 128
    Y = self.scratch("Y%d" % li, [64, 2, 64, E], BF16)
    ZS = self.scratch("ZS%d" % li, [16, 128, T], BF16)
    pb = Pool(self.pbank)
    fTc = self.fTc
    m0 = A.mark()
    cs = A.alloc("cs", [2, 512], BF16)
    ca = A.alloc("ca", [3, 128], BF16)
    d256 = A.alloc("d256", [2, 2, 256], BF16)
    P.dma("sp", cs[:], self.c_cs[:, :, :], writes=[cs])
    P.dma("sp", ca[:], self.c_a[:, :, :], writes=[ca])
    P.dma("sp", d256[:], self.c_d256[:, :, :, :], writes=[d256])
    stage = Pool([A.alloc("wstg%d" % i, [2048], F32) for i in range(2)])
    wb_pool = Pool([A.alloc("wb%d" % i, [8, 512], BF16) for i in range(2)])
    uT = A.alloc("uT", [4, T], BF16)
    UT_pool = Pool([A.alloc("UT%d" % i, [2, 2, 256], BF16) for i in range(3)])
    UTc = A.alloc("UTc", [2, 2, 2, 256], BF16) if upd_ctx else None
    Ysb_pool = Pool([A.alloc("Ysb%d" % i, [2, 512], BF16) for i in range(3)])
    zsb_pool = Pool([A.alloc("zsb%d" % i, [4, 512], BF16) for i in range(2)])
    ttiles = []
    if upd_ctx:
        ttiles.append((0, 256))
    for i in range(N // 512):
        ttiles.append((CTX + i * 512, 512))
    ptiles = []
    if upd_ctx:
        ptiles += [("ctx", 0, 0), ("ctx", 1, 128)]
    ptiles += [("lat", t, CTX + t * 128) for t in range(NT)]
    ev = 0
    if upd_ctx:
        fTc.begin()
    for fb in range(4):
        wb = wb_pool.next()
        self.load_w(wb, self.f_w_in[j, :, :, fb * 512:(fb + 1) * 512], 8, 512, stage)
        uT.begin()
        for (pos, w) in ttiles:
            for cc in range(4):
                pm = pb.next()

                def mm(e, pm=pm, wb=wb, cc=cc, pos=pos, w=w):
                    for kc in range(8):
                        ins = e.matmul(pm[:, 0:w], wb[:, kc, cc * 128:(cc + 1) * 128], hT[:, kc, pos:pos + w],
                                       start=(kc == 0), stop=(kc == 7))
                    return ins
                P.op("pe", mm, reads=[wb, hT], writes=[pm])
                ev += 1
                if ev % 2 == 0:
                    P.op("act", lambda e, pm=pm, cc=cc, pos=pos, w=w: e.activation(uT[:, cc, pos:pos + w], pm[:, 0:w], AF.Copy),
                         reads=[pm], pw=[uT])
                else:
                    P.op("dve", lambda e, pm=pm, cc=cc, pos=pos, w=w: e.tensor_copy(uT[:, cc, pos:pos + w], pm[:, 0:w]),
                         reads=[pm], pw=[uT])
        if upd_ctx:
            UTc.begin()
        for (stream, t, pos) in ptiles:
            UT = UT_pool.next().begin() if stream == "lat" else None
            for g in range(2):
                pm = pb.next()

                def mm2(e, pm=pm, g=g, pos=pos):
                    for cc in range(2):
                        ins = e.matmul(pm[:, :], uT[:, 2 * g + cc, pos:pos + 128], cs[:, cc, :],
                                       start=(cc == 0), stop=(cc == 1))
                    return ins
                P.op("pe", mm2, reads=[uT, cs], writes=[pm])
                if stream == "lat":
                    ov = UT[:, :, g, :]
                    ob = UT
                else:
                    ov = UTc[:, t, :, g, :]
                    ob = UTc
                pv = pm.ap.rearrange("p (a b) -> p a b", a=2)
                ev += 1
                if ev % 2 == 0:
                    P.op("act", lambda e, ov=ov, pv=pv: e.activation(ov, pv, AF.Copy), reads=[pm], pw=[ob])
                else:
                    P.op("dve", lambda e, ov=ov, pv=pv: e.tensor_copy(ov, pv), reads=[pm], pw=[ob])
            if stream == "lat":
                p_r = pb.next()
                p_i = pb.next()
                uc = UT[:, 0, :, :].rearrange("p g f -> p (g f)")
                us = UT[:, 1, :, :].rearrange("p g f -> p (g f)")

                def mmA(e, p_r=p_r, p_i=p_i, uc=uc, us=us):
                    e.matmul(p_r[:, :], ca[:, 0, :], uc, start=True, stop=False)
                    e.matmul(p_r[:, :], ca[:, 1, :], us, start=False, stop=True)
                    e.matmul(p_i[:, :], ca[:, 1, :], uc, start=True, stop=False)
                    return e.matmul(p_i[:, :], ca[:, 2, :], us, start=False, stop=True)
                P.op("pe", mmA, reads=[UT, ca], writes=[p_r, p_i])
                Ysb = Ysb_pool.next().begin()
                P.op("act", lambda e, Ysb=Ysb, p_r=p_r: e.activation(Ysb[:, 0, :], p_r[:, :], AF.Copy), reads=[p_r], pw=[Ysb])
                P.op("dve", lambda e, Ysb=Ysb, p_i=p_i: e.tensor_copy(Ysb[:, 1, :], p_i[:, :]), reads=[p_i], pw=[Ysb])
                for jj in range(2):
                    P.dma("sp", Y[:, :, 2 * t + jj, fb * 512:(fb + 1) * 512], Ysb.ap[jj * 64:(jj + 1) * 64, :, :],
                          reads=[Ysb], writes=[self.dbuf(("Y", t, fb, jj))])
        if upd_ctx:
            for g in range(2):
                for half in range(2):
                    pm = pb.next()

                    def mmc(e, pm=pm, g=g, half=half):
                        n = 0
                        for tile in range(2):
                            for cs_ in range(2):
                                ins = e.matmul(pm[:, 0:256], UTc[:, tile, cs_, g, half * 128:(half + 1) * 128],
                                               d256[:, tile, cs_, :], start=(n == 0), stop=(n == 3))
                                n += 1
                        return ins
                    P.op("pe", mmc, reads=[UTc, d256], writes=[pm])
                    c = fb * 4 + g * 2 + half
                    P.op("dve", lambda e, pm=pm, c=c: e.tensor_copy(fTc[:, c, :], pm[:, 0:256]), reads=[pm], pw=[fTc])
        wz = wb_pool.next()
        self.load_w(wz, self.f_w_in[j, :, :, E + fb * 512:E + (fb + 1) * 512], 8, 512, stage)
        for (pos, w) in ttiles:
            zsb = zsb_pool.next().begin()
            for cc in range(4):
                pm = pb.next()

                def mmz(e, pm=pm, wz=wz, cc=cc, pos=pos, w=w):
                    for kc in range(8):
                        ins = e.matmul(pm[:, 0:w], wz[:, kc, cc * 128:(cc + 1) * 128], hT[:, kc, pos:pos + w],
                                       start=(kc == 0), stop=(kc == 7))
                    return ins
                P.op("pe", mmz, reads=[wz, hT], writes=[pm])
                P.op("act", lambda e, zsb=zsb, pm=pm, cc=cc, w=w: e.activation(zsb[:, cc, 0:w], pm[:, 0:w], AF.Silu),
                     reads=[pm], pw=[zsb])
            P.dma("sp", ZS[fb * 4:(fb + 1) * 4, :, pos:pos + w].rearrange("c p t -> p c t"), zsb.ap[:, :, 0:w],
                  reads=[zsb], writes=[self.dbuf(("ZS", fb, pos))])
    self.barrier()
    A.release(self.m_hT)
    wout = A.alloc("wout", [16, D], BF16)
    stage = Pool([A.alloc("wstg%d" % i, [1024], F32) for i in range(2)])
    self.load_w(wout, self.f_w_out[j], 16, D, stage, engs=("pool", "dve"))
    bk = A.alloc("bk", [64, 64], BF16)
    P.dma("sp", bk[:], self.c_bk[:, :, :], writes=[bk])
    zs_pool = Pool([A.alloc("zs%d" % i, [16, 512], BF16) for i in range(2)])
    Yk_pool = Pool([A.alloc("Yk%d" % i, [E], BF16) for i in range(9)])
    yT_pool = Pool([A.alloc("yT%d" % i, [16, 512], BF16) for i in range(2)])
    xt_pool = Pool([A.alloc("fx%d" % i, [D], F32) for i in range(2)])
    xn_pool = Pool([A.alloc("fxn%d" % i, [D], F32) for i in range(2)])
    keyx = "xs" if x_src is self.xs else "xin"

    def out_stage(yT, width_off, stream, t, gb, src, key_in, key_out, dst):
        xt = xt_pool.next()
        self.load_rows("sp", xt, stream, t, stream == "lat", src, key_in)
        xn = xn_pool.next().begin()
        for half in range(2):
            po = pb.next()

            def mmo(e, po=po, half=half):
                for c in range(16):
                    ins = e.matmul(po[:, :], yT[:, c, width_off:width_off + 128], wout[:, c, half * 512:(half + 1) * 512],
                                   start=(c == 0), stop=(c == 15))
                return ins
            P.op("pe", mmo, reads=[yT, wout], writes=[po])
            hs = slice(half * 512, (half + 1) * 512)
            P.op("dve", lambda e, po=po, hs=hs: e.tensor_tensor(xn[:, hs], po[:, :], gb[:, hs], ALU.mult),
                 reads=[po, gb], pw=[xn])
        P.op("pool", lambda e: e.tensor_tensor(xn[:], xn[:], xt[:], ALU.add), reads=[xt], writes=[xn])
        self.store_rows("sp", xn, stream, t, stream == "lat", dst, key_out)

    for st in range(N // 512):
        pos = CTX + st * 512
        zs = zs_pool.next()
        P.dma("sp", zs[:], ZS[:, :, pos:pos + 512].rearrange("c p t -> p c t"), writes=[zs])
        Yks = []
        for kk in range(8):
            k1 = st * 8 + kk
            Yk = Yk_pool.next()
            P.dma("sp", Yk[:], Y[k1].rearrange("r n f -> (r n) f"), writes=[Yk])
            Yks.append(Yk)
        yT = yT_pool.next().begin()
        for c in range(16):
            pf = pb.next()

            def mmf(e, pf=pf, c=c, Yks=Yks, st=st):
                for kk in range(8):
                    ins = e.matmul(pf[:, kk * 64:(kk + 1) * 64], Yks[kk][:, c * 128:(c + 1) * 128], bk[:, st * 8 + kk, :],
                                   start=True, stop=True)
                return ins
            P.op("pe", mmf, reads=Yks + [bk], writes=[pf])
            P.op("dve", lambda e, pf=pf, c=c, yT=yT, zs=zs: e.tensor_tensor(yT[:, c, :], pf[:, :], zs[:, c, :], ALU.mult),
                 reads=[pf, zs], pw=[yT])
        for i in range(4):
            out_stage(yT, i * 128, "lat", st * 4 + i, self.g_bc, x_src, keyx, "xs", self.xs)
    if upd_ctx:
        zs = zs_pool.next()
        P.dma("sp", zs.ap[:, :, 0:256], ZS[:, :, 0:256].rearrange("c p t -> p c t"), writes=[zs])
        yT = yT_pool.next()
        P.op("dve", lambda e: e.tensor_tensor(yT.ap[:, :, 0:256], fTc[:, :, :], zs.ap[:, :, 0:256], ALU.mult),
             reads=[fTc, zs], writes=[yT])
        keyc = "xc" if c_src is self.xcs else "cin"
        for t in range(2):
            out_stage(yT, t * 128, "ctx", t, self.gc_bc, c_src, keyc, "xc", self.xcs)


KB.fourier_layer = fourier_layer


def _bf(a):
    return np.ascontiguousarray(a.astype(np.float32)).astype(ml_dtypes.bfloat16)


def make_consts():
    c = {}
    c["identb"] = _bf(np.eye(128))
    p = np.arange(128)
    cc = np.arange(2)
    ch = (cc[None, :] * 128 + p[:, None])
    k = np.arange(256)
    ang = 2 * np.pi * ((ch[:, :, None] * k[None, None, :]) % 256) / 256.0
    c["c_cs"] = _bf(np.concatenate([np.cos(ang), np.sin(ang)], axis=-1) / 16.0)
    n1 = np.arange(64)
    a64 = 2 * np.pi * ((n1[:, None] * n1[None, :]) % 64) / 64.0
    C, S = np.cos(a64), np.sin(a64)
    Z = np.zeros((64, 64))
    bd = lambda M: np.block([[M, Z], [Z, M]])
    c["c_a"] = _bf(np.stack([bd(C), bd(-S), bd(-C)], axis=1))
    n2 = np.arange(64)[:, None, None]
    k1 = np.arange(64)[None, :, None]
    k2 = np.arange(64)[None, None, :]
    num = (n2 * k2 * 64 + n2 * k1) % 4096
    th = 2 * np.pi * num / 4096.0
    c["c_bk"] = _bf(np.concatenate([np.cos(th), np.sin(th)], axis=0) / 64.0)
    n = (np.arange(2)[None, :] * 128 + p[:, None])
    a256 = 2 * np.pi * ((n[:, :, None] * k[None, None, :]) % 256) / 256.0
    c["c_d256"] = _bf(np.stack([np.cos(a256), -np.sin(a256)], axis=2) / 16.0)
    return c


def _pk(w, kc):
    n = w.shape[-1]
    return np.ascontiguousarray(w.reshape(kc, 128, n).transpose(1, 0, 2))


def host_shared(inp):
    f = lambda a: np.asarray(a, dtype=np.float32)
    sh = {}
    sh["norm_g"] = f(inp["norm_g"])
    sh["w_ada"] = np.stack([_pk(f(inp["w_ada"][l]), 8) for l in range(4)])
    sh["b_ada"] = f(inp["b_ada"])
    sh["f_w_in"] = np.stack([_pk(f(inp["f_w_in"][l]), 8) for l in range(2)])
    sh["f_w_out"] = np.stack([_pk(f(inp["f_w_out"][l]), 16) for l in range(2)])
    sh["norm_f"] = f(inp["norm_f"]).reshape(1, D)
    sh.update(make_consts())
    sh.update(host_shared_mlstm(inp))
    return sh


def host_shared_mlstm(inp):
    return {}


def host_core(inp, b):
    f = lambda a: np.asarray(a, dtype=np.float32)
    d = {}
    d["x"] = np.ascontiguousarray(f(inp["x"][b]))
    d["ctx"] = np.ascontiguousarray(f(inp["ctx"][b]))
    cv = np.stack([f(inp["c"][b]), f(inp["c_ctx"])], axis=-1)
    d["cvec"] = np.ascontiguousarray(cv.reshape(8, 128, 2).transpose(1, 0, 2))
    return d


def declare_mlstm_inputs(self):
    self.m_w_in = self.inp("m_w_in", [2, 128, 8, 3 * E])
    self.m_conv_w = self.inp("m_conv_w", [2, 128, 16, 9])
    self.m_vecs = self.inp("m_vecs", [2, 128, 3, 16])
    self.m_w_q = self.inp("m_w_q", [2, 4, 128, 4, DH])
    self.m_w_k = self.inp("m_w_k", [2, 4, 128, 4, DH])
    self.m_w_if = self.inp("m_w_if", [2, 128, 3, 16, 16])
    self.m_bif = self.inp("m_bif", [2, 16])
    self.m_w_out = self.inp("m_w_out", [2, 128, 16, D])
    self.c_tri = self.inp("c_tri", [128, 3, 128])
    self.c_mask = self.inp("c_mask", [128, 2, 128])


KB.declare_mlstm_inputs = declare_mlstm_inputs


def host_shared_mlstm(inp):
    f = lambda a: np.asarray(a, dtype=np.float32)
    sh = {}
    sh["m_w_in"] = np.stack([_pk(f(inp["m_w_in"][l]), 8) for l in range(2)])
    cw = f(inp["m_conv_w"]).reshape(2, 9, E)
    sh["m_conv_w"] = np.ascontiguousarray(cw.reshape(2, 9, 16, 128).transpose(0, 3, 2, 1))
    vec = np.stack([f(inp["m_conv_b"]), f(inp["m_ln_w"]), f(inp["m_skip"])], axis=1)
    sh["m_vecs"] = np.ascontiguousarray(vec.reshape(2, 3, 16, 128).transpose(0, 3, 1, 2))
    sh["m_w_q"] = np.stack([np.stack([_pk(f(inp["m_w_q"][l][h]), 4) for h in range(4)]) for l in range(2)])
    sh["m_w_k"] = np.stack([np.stack([_pk(f(inp["m_w_k"][l][h]), 4) for h in range(4)]) for l in range(2)])
    wif = f(inp["m_w_if"])
    wif = wif.reshape(2, 2, 3, 16, 128, 8).transpose(0, 4, 2, 3, 1, 5)
    sh["m_w_if"] = np.ascontiguousarray(wif.reshape(2, 128, 3, 16, 16))
    bif = np.concatenate([f(inp["m_b_i"]), f(inp["m_b_f"])], axis=-1)
    sh["m_bif"] = np.ascontiguousarray(bif.reshape(2, 16))
    sh["m_w_out"] = np.stack([_pk(f(inp["m_w_out"][l]), 16) for l in range(2)])
    s_ = np.arange(128)[:, None]
    j_ = np.arange(128)[None, :]
    mf = (s_ <= j_).astype(np.float32)
    mb = (s_ >= j_).astype(np.float32)
    sh["c_tri"] = np.ascontiguousarray(np.stack([-mf, -mb, -np.ones((128, 128), np.float32)], axis=1))
    sh["c_mask"] = np.ascontiguousarray(np.stack([mf, mb], axis=1))
    return sh


def out_stage(self, yT, off, stream, t, fourier, gb, src, key_in, key_out, dst, wout, xt_pool, xn_pool, pb):
    P = self.P
    xt = xt_pool.next()
    self.load_rows("sp", xt, stream, t, fourier, src, key_in)
    xn = xn_pool.next().begin()
    for half in range(2):
        po = pb.next()

        def mmo(e, po=po, half=half):
            for c in range(16):
                ins = e.matmul(po[:, :], yT[:, c, off:off + 128], wout[:, c, half * 512:(half + 1) * 512],
                               start=(c == 0), stop=(c == 15))
            return ins
        P.op("pe", mmo, reads=[yT, wout], writes=[po])
        hs = slice(half * 512, (half + 1) * 512)
        P.op("dve", lambda e, po=po, hs=hs: e.tensor_tensor(xn[:, hs], po[:, :], gb[:, hs], ALU.mult),
             reads=[po, gb], pw=[xn])
    P.op("pool", lambda e: e.tensor_tensor(xn[:], xn[:], xt[:], ALU.add), reads=[xt], writes=[xn])
    self.store_rows("sp", xn, stream, t, fourier, dst, key_out)


KB.out_stage = out_stage


def mlstm_layer(self, li, x_src, c_src, upd_ctx):
    P, A = self.P, self.A
    hT = self.hT
    N, T = self.N, self.T
    R = N // GW
    j = li // 2
    NTT = T // 128
    pb = Pool(self.pbank)
    S = self.scr.get(j)
    if S is None:
        S = {}
        S["OZX"] = self.scratch("OZX%d" % j, [3, NTT, 128, 16, 128], BF16)
        S["QT"] = self.scratch("QT%d" % j, [NTT, 128, 4, 4, 128], BF16)
        S["KT"] = self.scratch("KT%d" % j, [NTT, 128, 4, 4, 128], BF16)
        S["Kt"] = self.scratch("Kt%d" % j, [T, E], BF16)
        S["Vt"] = self.scratch("Vt%d" % j, [T, E], BF16)
        S["HF"] = self.scratch("HF%d" % j, [T, E], BF16)
        self.scr[j] = S
    m0 = A.mark()
    G = self.G
    bif = self.bif
    P.dma("sp", bif[:], self.m_bif[j].partition_broadcast(128), writes=[bif])
    mB = A.mark()
    cw = A.alloc("cw", [16, 9], F32)
    vecs = A.alloc("vecs", [3, 16], F32)
    P.dma("sp", cw[:], self.m_conv_w[j], writes=[cw])
    P.dma("sp", vecs[:], self.m_vecs[j], writes=[vecs])
    wif32 = A.alloc("wif32", [3, 16, 16], F32)
    wif = A.alloc("wif", [3, 16, 16], BF16)
    P.dma("sp", wif32[:], self.m_w_if[j], writes=[wif32])
    P.op("pool", lambda e: e.tensor_copy(wif[:], wif32[:]), reads=[wif32], writes=[wif])
    stage = Pool([A.alloc("mstg%d" % i, [1024], F32) for i in range(2)])
    wb_pool = Pool([A.alloc("mwb%d" % i, [8, 512], BF16) for i in range(2)])
    wq = A.alloc("wq", [4, DH], BF16)
    wk = A.alloc("wk", [4, DH], BF16)
    LP = 260 + (R + 2) * 68
    xmpad = A.alloc("xmpad", [LP], BF16)
    vTc = A.alloc("vTc", [T], BF16)
    xcv = A.alloc("xcv", [4, T], BF16)
    dg_pool = Pool([A.alloc("dg%d" % i, [9, 128], BF16) for i in range(2)])
    tl_pool = Pool([A.alloc("tl%d" % i, [512], BF16) for i in range(4)])
    t32_pool = Pool([A.alloc("t32_%d" % i, [512], F32) for i in range(2)])
    qt_pool = Pool([A.alloc("qt%d" % i, [4, 512], BF16) for i in range(4)])
    xl = xmpad.ap[:, 260:LP].rearrange("p (r c) -> p r c", c=68)
    P.op("pool", lambda e: e.memset(xmpad[:], 0.0), writes=[xmpad])
    ttiles = [(0, 256)] + [(CTX + i * 512, 512) for i in range(N // 512)]
    first_g = [True] * NTT
    ev = [0]

    def evac(out_ap, in_ap, reads, pw=(), writes=(), func=None, scale=None, bias=None):
        ev[0] += 1
        if func is not None or ev[0] % 2 == 0:
            kw = {}
            if scale is not None:
                kw["scale"] = scale
            if bias is not None:
                kw["bias"] = bias
            f = func if func is not None else AF.Copy
            return P.op("act", lambda e: e.activation(out_ap, in_ap, f, **kw), reads=reads, pw=pw, writes=writes)
        if scale is not None:
            return P.op("dve", lambda e: e.tensor_scalar(out_ap, in_ap, scale, None, ALU.mult), reads=reads, pw=pw, writes=writes)
        return P.op("dve", lambda e: e.tensor_copy(out_ap, in_ap), reads=reads, pw=pw, writes=writes)

    def gate_acc(pm, tt):
        if first_g[tt]:
            first_g[tt] = False
            P.op("dve", lambda e: e.tensor_tensor(G[:, tt, :], pm[:, 0:16], bif[:], ALU.add), reads=[pm, bif], pw=[G])
        else:
            P.op("dve", lambda e: e.tensor_tensor(G[:, tt, :], pm[:, 0:16], G[:, tt, :], ALU.add), reads=[pm], pw=[G])

    G.begin()
    import os
    stop = os.environ.get("MK_STOP", "")
    for hd in range(4):
        if stop.startswith("S") and hd > 0:
            return
        self.load_w(wq, self.m_w_q[j, hd], 4, DH, stage)
        self.load_w(wk, self.m_w_k[j, hd], 4, DH, stage)
        wxm = wb_pool.next()
        self.load_w(wxm, self.m_w_in[j, :, :, hd * 512:(hd + 1) * 512], 8, 512, stage)
        xcv.begin()
        if stop == "S0":
            return
        for cc in range(4):
            c = hd * 4 + cc
            xmpad.begin()
            vTc.begin()
            for (pos, w) in ttiles:
                pm = pb.next()

                def mm(e, pm=pm, cc=cc, pos=pos, w=w, wxm=wxm):
                    for kc in range(8):
                        ins = e.matmul(pm[:, 0:w], wxm[:, kc, cc * 128:(cc + 1) * 128], hT[:, kc, pos:pos + w],
                                       start=(kc == 0), stop=(kc == 7))
                    return ins
                P.op("pe", mm, reads=[wxm, hT], writes=[pm])
                if pos == 0:
                    ov = xmpad.ap[:, 2:258]
                    iv = pm[:, 0:256]
                else:
                    r0 = (pos - CTX) // 64
                    ov = xl[:, r0 + 1:r0 + 9, 2:66]
                    iv = pm.ap.rearrange("p (r c) -> p r c", c=64)
                P.op("act", lambda e, ov=ov, iv=iv: e.activation(ov, iv, AF.Copy), reads=[pm], pw=[xmpad])
                P.op("dve", lambda e, pm=pm, pos=pos, w=w: e.tensor_copy(vTc[:, pos:pos + w], pm[:, 0:w]), reads=[pm], pw=[vTc])
            if stop == "S1":
                return
            for tt in range(NTT):
                pm = pb.next()
                P.op("pe", lambda e, pm=pm, tt=tt, c=c: e.matmul(pm[:, 0:16], vTc[:, tt * 128:(tt + 1) * 128], wif[:, 2, c, :],
                                                                start=True, stop=True), reads=[vTc, wif], writes=[pm])
                gate_acc(pm, tt)
            if stop == "S2":
                return
            dg = dg_pool.next()

            def mkdg(e, dg=dg, c=c):
                for tap in range(9):
                    ins = e.tensor_scalar(dg[:, tap, :], self.ident[:], cw[:, c, tap:tap + 1], None, ALU.mult)
                return ins
            P.op("dve", mkdg, reads=[self.ident, cw], writes=[dg])
            for (pos, w) in ttiles:
                pm = pb.next()
                if pos == 0:
                    def mmc(e, pm=pm, dg=dg):
                        for k, dc in enumerate((-1, 0, 1)):
                            ins = e.matmul(pm[:, 0:256], dg[:, 3 + k, :], xmpad.ap[:, 2 + dc:258 + dc],
                                           start=(k == 0), stop=(k == 2))
                        return ins
                else:
                    r0 = (pos - CTX) // 64

                    def mmc(e, pm=pm, dg=dg, r0=r0):
                        n = 0
                        for dr in (-1, 0, 1):
                            for dc in (-1, 0, 1):
                                ins = e.matmul(pm[:, :], dg[:, 3 * (dr + 1) + (dc + 1), :],
                                               xl[:, r0 + 1 + dr:r0 + 9 + dr, 2 + dc:66 + dc],
                                               start=(n == 0), stop=(n == 8))
                                n += 1
                        return ins
                P.op("pe", mmc, reads=[dg, xmpad], writes=[pm])
                P.op("act", lambda e, pm=pm, cc=cc, pos=pos, w=w, c=c: e.activation(
                    xcv[:, cc, pos:pos + w], pm[:, 0:w], AF.Silu, bias=vecs[:, 0, c:c + 1]), reads=[pm, vecs], pw=[xcv])
                if stop == "S3":
                    continue
                tl = tl_pool.next()
                P.op("dve", lambda e, tl=tl, cc=cc, pos=pos, w=w, c=c: e.tensor_scalar(
                    tl[:, 0:w], xcv[:, cc, pos:pos + w], vecs[:, 2, c:c + 1], None, ALU.mult), reads=[xcv, vecs], writes=[tl])
                tt0 = pos // 128
                nt = w // 128
                P.dma("sp", S["OZX"][2, tt0:tt0 + nt, :, c, :].rearrange("t p k -> p t k"),
                      tl.ap[:, 0:w].rearrange("p (t k) -> p t k", k=128), reads=[tl],
                      writes=[self.dbuf(("OZX", j, 2, c, pos))])
        if stop in ("S3", "S4"):
            return
        for tt in range(NTT):
            pm = pb.next()

            def mmv(e, pm=pm, tt=tt, wxm=wxm):
                for kc in range(8):
                    ins = e.matmul(pm[:, :], hT[:, kc, tt * 128:(tt + 1) * 128], wxm[:, kc, :],
                                   start=(kc == 0), stop=(kc == 7))
                return ins
            P.op("pe", mmv, reads=[wxm, hT], writes=[pm])
            tl = tl_pool.next()
            evac(tl[:, :], pm[:, :], [pm], writes=[tl])
            P.dma("sp", S["Vt"][tt * 128:(tt + 1) * 128, hd * 512:(hd + 1) * 512], tl[:, :], reads=[tl],
                  writes=[self.dbuf(("Vt", j, tt, hd))])
        for kind in range(2):
            wz = wb_pool.next()
            col0 = E * (kind + 1) + hd * 512
            self.load_w(wz, self.m_w_in[j, :, :, col0:col0 + 512], 8, 512, stage)
            for cc in range(4):
                c = hd * 4 + cc
                for (pos, w) in ttiles:
                    pm = pb.next()

                    def mm(e, pm=pm, cc=cc, pos=pos, w=w, wz=wz):
                        for kc in range(8):
                            ins = e.matmul(pm[:, 0:w], wz[:, kc, cc * 128:(cc + 1) * 128], hT[:, kc, pos:pos + w],
                                           start=(kc == 0), stop=(kc == 7))
                        return ins
                    P.op("pe", mm, reads=[wz, hT], writes=[pm])
                    tl = tl_pool.next()
                    if kind == 0:
                        t32 = t32_pool.next()
                        P.op("act", lambda e, t32=t32, pm=pm, w=w: e.activation(t32[:, 0:w], pm[:, 0:w], AF.Sigmoid),
                             reads=[pm], writes=[t32])
                        P.op("dve", lambda e, tl=tl, t32=t32, w=w, c=c: e.tensor_scalar(
                            tl[:, 0:w], t32[:, 0:w], vecs[:, 1, c:c + 1], None, ALU.mult), reads=[t32, vecs], writes=[tl])
                    else:
                        P.op("act", lambda e, tl=tl, pm=pm, w=w: e.activation(tl[:, 0:w], pm[:, 0:w], AF.Silu),
                             reads=[pm], writes=[tl])
                    tt0 = pos // 128
                    nt = w // 128
                    P.dma("sp", S["OZX"][kind, tt0:tt0 + nt, :, c, :].rearrange("t p k -> p t k"),
                          tl.ap[:, 0:w].rearrange("p (t k) -> p t k", k=128), reads=[tl],
                          writes=[self.dbuf(("OZX", j, kind, c, pos))])
        for (pos, w) in ttiles:
            tt0 = pos // 128
            nt = w // 128
            qk = []
            for which, wmat, scl in ((0, wq, None), (1, wk, DH ** -0.5)):
                qt = qt_pool.next().begin()
                for ec in range(4):
                    pm = pb.next()

                    def mmq(e, pm=pm, ec=ec, pos=pos, w=w, wmat=wmat):
                        for dc in range(4):
                            ins = e.matmul(pm[:, 0:w], wmat[:, dc, ec * 128:(ec + 1) * 128], xcv[:, dc, pos:pos + w],
                                           start=(dc == 0), stop=(dc == 3))
                        return ins
                    P.op("pe", mmq, reads=[wmat, xcv], writes=[pm])
                    evac(qt[:, ec, 0:w], pm[:, 0:w], [pm], pw=[qt], scale=scl)
                dst = S["QT"] if which == 0 else S["KT"]
                for ec in range(4):
                    P.dma("sp", dst[tt0:tt0 + nt, :, hd, ec, :].rearrange("t p k -> p t k"),
                          qt.ap[:, ec, 0:w].rearrange("p (t k) -> p t k", k=128), reads=[qt],
                          writes=[self.dbuf(("QK", j, which, hd, ec, pos))])
                qk.append(qt)
            qt, kt = qk
            for sub in range(nt):
                tt = tt0 + sub
                sl = slice(sub * 128, (sub + 1) * 128)
                pm = pb.next()

                def mmk(e, pm=pm, sl=sl, pos=pos):
                    for dc in range(4):
                        ins = e.matmul(pm[:, :], xcv[:, dc, pos + sl.start:pos + sl.stop], wk[:, dc, :],
                                       start=(dc == 0), stop=(dc == 3))
                    return ins
                P.op("pe", mmk, reads=[wk, xcv], writes=[pm])
                tl = tl_pool.next()
                evac(tl[:, :], pm[:, :], [pm], writes=[tl], scale=DH ** -0.5)
                P.dma("sp", S["Kt"][tt * 128:(tt + 1) * 128, hd * 512:(hd + 1) * 512], tl[:, :], reads=[tl],
                      writes=[self.dbuf(("Kt", j, tt, hd))])
                pg = pb.next()

                def mmg(e, pg=pg, sl=sl, qt=qt, kt=kt, hd=hd):
                    n = 0
                    for src, buf in ((0, qt), (1, kt)):
                        for ec in range(4):
                            ins = e.matmul(pg[:, 0:16], buf[:, ec, sl], wif[:, src, hd * 4 + ec, :],
                                           start=(n == 0), stop=(n == 7))
                            n += 1
                    return ins
                P.op("pe", mmg, reads=[qt, kt, wif], writes=[pg])
                gate_acc(pg, tt)
    self.barrier()
    A.release(self.m_hT)
    import os
    self.stop = os.environ.get("MK_STOP", "")
    if self.stop == "B":
        return
    self.mlstm_scan(li, j, S, G, x_src, c_src, upd_ctx, pb)


KB.mlstm_layer = mlstm_layer


def mlstm_scan(self, li, j, S, G, x_src, c_src, upd_ctx, pb):
    P, A = self.P, self.A
    N, T = self.N, self.T
    NTT = T // 128
    NG = NTT * 4
    tri = A.alloc("tri", [3, 128], F32)
    mask = A.alloc("mask", [2, 128], F32)
    P.dma("sp", tri[:], self.c_tri[:, :, :], writes=[tri])
    P.dma("sp", mask[:], self.c_mask[:, :, :], writes=[mask])
    SP = A.alloc("SP", [2, NTT, 4], F32)
    TA = A.alloc("TA", [2, NTT, 4], F32)
    AA = A.alloc("AA", [2, NTT, 4], F32)
    WW = A.alloc("WW", [2, NTT, 4], F32)
    ENB = A.alloc("ENB", [2, NTT, 4], F32)
    EBL = A.alloc("EBL", [2, NTT, 4], F32)
    Gv = G.ap.rearrange("p t (r g) -> p r t g", r=2)
    for b_ in (SP, TA, AA, WW, ENB, EBL):
        b_.begin()
    for r in range(2):
        fpre = Gv[:, r, :, 4:8]
        liv = Gv[:, r, :, 0:4]
        P.op("act", lambda e, r=r, fpre=fpre: e.activation(SP[:, r, :, :], fpre, AF.Exp, scale=-1.0), reads=[G], pw=[SP])
        P.op("act", lambda e, r=r: e.activation(SP[:, r, :, :], SP[:, r, :, :], AF.Ln, bias=1.0), reads=[SP], pw=[SP])
        pbm = pb.next()
        pbl = pb.next()
        spf = SP[:, r, :, :].rearrange("p t h -> p (t h)")
        P.op("pe", lambda e, pbm=pbm, spf=spf, r=r: e.matmul(pbm[:, 0:NG], tri[:, r, :], spf, start=True, stop=True),
             reads=[tri, SP], writes=[pbm])
        P.op("pe", lambda e, pbl=pbl, spf=spf: e.matmul(pbl[:, 0:NG], tri[:, 2, :], spf, start=True, stop=True),
             reads=[tri, SP], writes=[pbl])
        bv = pbm[:, 0:NG].rearrange("p (t h) -> p t h", h=4)
        blv = pbl[:, 0:NG].rearrange("p (t h) -> p t h", h=4)
        P.op("dve", lambda e, r=r, liv=liv, bv=bv: e.tensor_tensor(TA[:, r, :, :], liv, bv, ALU.subtract), reads=[G, pbm], pw=[TA])
        P.op("act", lambda e, r=r: e.activation(AA[:, r, :, :], TA[:, r, :, :], AF.Exp), reads=[TA], pw=[AA])
        P.op("dve", lambda e, r=r, blv=blv: e.tensor_tensor(TA[:, r, :, :], TA[:, r, :, :], blv, ALU.add), reads=[pbl, AA], pw=[TA])
        P.op("act", lambda e, r=r: e.activation(WW[:, r, :, :], TA[:, r, :, :], AF.Exp), reads=[TA], pw=[WW])
        P.op("act", lambda e, r=r, bv=bv: e.activation(ENB[:, r, :, :], bv, AF.Exp, scale=-1.0), reads=[pbm], pw=[ENB])
        P.op("act", lambda e, r=r, blv=blv: e.activation(EBL[:, r, :, :], blv, AF.Exp), reads=[pbl], pw=[EBL])
    if "dbgG" in self.dbg:
        dG = self.outp("dbgG", [128, NTT, 16])
        P.dma("sp", dG[:, :, :], G[:], reads=[G])
        for nm, bf_ in (("dbgAA", AA), ("dbgWW", WW), ("dbgENB", ENB), ("dbgEBL", EBL), ("dbgSP", SP)):
            dd = self.outp(nm, [128, 2, NTT, 4])
            P.dma("sp", dd[:, :, :, :], bf_[:], reads=[bf_])
    if self.stop == "G":
        return
    mS = A.mark()
    keyx = "xs" if x_src is self.xs else "xin"
    keyc = "xc" if c_src is self.xcs else "cin"
    for r in range(2):
        A.release(mS)
        C32 = A.alloc("C32", [4, 4, 513], F32)
        Cbf = A.alloc("Cbf", [4, 4, 514], BF16)
        P.op("pool", lambda e: e.memset(C32[:], 0.0))
        tk0 = P.op("pool", lambda e: e.memset(Cbf[:], 0.0))
        nld = 3 if r == 0 else 2
        q_pool = Pool([A.alloc("qc%d" % i, [16, 128], BF16) for i in range(nld)])
        k_pool = Pool([A.alloc("kc%d" % i, [16, 128], BF16) for i in range(nld)])
        K_pool = Pool([A.alloc("Kc%d" % i, [E], BF16) for i in range(nld)])
        V_pool = Pool([A.alloc("Vc%d" % i, [E], BF16) for i in range(nld)])
        WT_pool = Pool([A.alloc("WT%d" % i, [128], BF16) for i in range(2)])
        Va_pool = Pool([A.alloc("Va%d" % i, [514], BF16) for i in range(2)])
        Vw_pool = Pool([A.alloc("Vw%d" % i, [514], BF16) for i in range(2)])
        rr_pool = Pool([A.alloc("rr%d" % i, [2], F32) for i in range(4)])
        hF_pool = Pool([A.alloc("hF%d" % i, [4, 512], BF16) for i in range(2 if r == 0 else 1)])
        if r == 1:
            h32 = A.alloc("h32", [4, 512], F32)
            hn = A.alloc("hn", [4, 512], BF16)
            stats = A.alloc("stats", [4, 6], F32)
            mv = A.alloc("mv", [4, 2], F32)
            rs = A.alloc("rs", [4], F32)
            t1 = A.alloc("t1", [16, 128], F32)
            yT = A.alloc("yTm", [16, 128], BF16)
            ozx = [A.alloc("ozx%d" % i, [16, 128], BF16) for i in range(3)]
            wout = A.alloc("mwout", [16, D], BF16)
            stage = Pool([A.alloc("sstg%d" % i, [512], F32) for i in range(2)])
            self.load_w(wout, self.m_w_out[j], 16, D, stage, engs=("pool",))
            xt_pool = Pool([A.alloc("mx%d" % i, [D], F32) for i in range(2)])
            xn_pool = Pool([A.alloc("mxn%d" % i, [D], F32) for i in range(1)])
        order = [0, 1] + list(range(2, NTT)) if r == 0 else [1, 0] + list(range(NTT - 1, 1, -1))
        ctok = {hd: tk0 for hd in range(4)}
        CH = {}

        def chunk_loads(tt):
            is_ctx = tt < 2
            emit = (not is_ctx) or upd_ctx
            ch = {"emit": emit, "is_ctx": is_ctx}
            Kc = K_pool.next()
            Vc = V_pool.next()
            P.dma("sp", Kc[:], S["Kt"][tt * 128:(tt + 1) * 128, :], writes=[Kc])
            P.dma("sp", Vc[:], S["Vt"][tt * 128:(tt + 1) * 128, :], writes=[Vc])
            ch["Kc"], ch["Vc"] = Kc, Vc
            if emit:
                qc = q_pool.next()
                kc_ = k_pool.next()
                P.dma("sp", qc[:], S["QT"][tt].rearrange("p h e k -> p (h e) k"), writes=[qc])
                P.dma("sp", kc_[:], S["KT"][tt].rearrange("p h e k -> p (h e) k"), writes=[kc_])
                ch["qc"], ch["kc"] = qc, kc_
            CH[tt] = ch

        def stage_A(tt, hd, r=r):
            if hd == 0:
                chunk_loads(tt)
            ch = CH[tt]
            Vc = ch["Vc"]
            st = {}
            a_s = AA[:, r, tt, hd:hd + 1]
            w_s = WW[:, r, tt, hd:hd + 1]
            Vw = Vw_pool.next().begin()
            st["Vw"] = Vw
            st["small"] = small = pb.next()
            P.op("dve", lambda e: e.tensor_scalar(Vw[:, 0:512], Vc[:, hd * 512:(hd + 1) * 512], w_s, None, ALU.mult),
                 reads=[Vc, WW], pw=[Vw])
            P.op("pool", lambda e: e.tensor_copy(Vw[:, 512:513], w_s), reads=[WW], pw=[Vw])
            if ch["emit"]:
                qc, kc_ = ch["qc"], ch["kc"]
                Va = Va_pool.next().begin()
                st["Va"] = Va
                P.op("act", lambda e: e.activation(Va[:, 0:512], Vc[:, hd * 512:(hd + 1) * 512], AF.Copy, scale=a_s),
                     reads=[Vc, AA], pw=[Va])
                P.op("act", lambda e: e.activation(Va[:, 512:513], a_s, AF.Copy), reads=[AA], pw=[Va])

                def mms(e):
                    for ec in range(4):
                        ins = e.matmul(small[:, 0:128], kc_[:, hd * 4 + ec, :], qc[:, hd * 4 + ec, :],
                                       start=(ec == 0), stop=(ec == 3))
                    return ins
                P.op("pe", mms, reads=[qc, kc_], writes=[small])
                WT = WT_pool.next()
                st["WT"] = WT
                P.op("dve", lambda e: e.tensor_tensor(WT[:], small[:, 0:128], mask[:, r, :], ALU.mult),
                     reads=[small, mask], writes=[WT])
            return st

        def stage_B(tt, hd, st, r=r):
            ch = CH[tt]
            st["tok_mmn"] = None
            if not ch["emit"]:
                return
            if hd == 0:
                hF = hF_pool.next()
                ch["hF"] = hF
                if r == 1:
                    P.dma("sp", hF[:], S["HF"][tt * 128:(tt + 1) * 128, :].rearrange("p (h v) -> p h v", h=4), writes=[hF])
                    h32.begin()
                else:
                    hF.begin()
            hF = ch["hF"]
            qc = ch["qc"]
            small, WT, Va = st["small"], st["WT"], st["Va"]
            pd = small.ap[:, 128:132]
            pn = pb.next()

            def mmn(e):
                e.matmul(pn[:, :], WT[:], Va[:, 0:512], start=True, stop=False)
                for kc in range(4):
                    e.matmul(pn[:, :], qc[:, hd * 4 + kc, :], Cbf[:, hd, kc, 0:512], start=False, stop=(kc == 3))
                e.matmul(pd[:, 0:1], WT[:], Va[:, 512:513], start=True, stop=False)
                for kc in range(4):
                    ins = e.matmul(pd[:, 0:1], qc[:, hd * 4 + kc, :], Cbf[:, hd, kc, 512:513], start=False, stop=(kc == 3))
                return ins
            st["tok_mmn"] = P.op("pe", mmn, reads=[WT, Va, qc], writes=[pn, small], extra=[ctok.get(hd)])
            rr = rr_pool.next()
            P.op("act", lambda e: e.activation(rr[:, 0:1], pd[:, 0:1], AF.Abs), reads=[small], writes=[rr])
            P.op("dve", lambda e: e.tensor_scalar(rr[:, 0:1], rr[:, 0:1], ENB[:, r, tt, hd:hd + 1], None, ALU.max),
                 reads=[rr, ENB], writes=[rr])
            P.op("dve", lambda e: e.reciprocal(rr[:, 1:2], rr[:, 0:1]), reads=[rr], writes=[rr])
            if r == 0:
                P.op("act", lambda e: e.activation(hF[:, hd, :], pn[:, :], AF.Copy, scale=rr[:, 1:2]),
                     reads=[pn, rr], pw=[hF])
            else:
                P.op("dve", lambda e: e.scalar_tensor_tensor(h32[:, hd, :], pn[:, :], rr[:, 1:2], hF[:, hd, :], ALU.mult, ALU.add),
                     reads=[pn, rr, hF], pw=[h32])

        def stage_C(tt, hd, st, r=r):
            ch = CH[tt]
            Kc = ch["Kc"]
            small, Vw = st["small"], st["Vw"]
            psn = small.ap[:, 132:136]
            if tt != order[-1]:
                ebl = EBL[:, r, tt, hd:hd + 1]
                toks = []
                for kc in range(4):
                    pk = pb.next()
                    P.op("pe", lambda e, pk=pk, kc=kc: e.matmul(pk[:, :], Kc[:, hd * 512 + kc * 128:hd * 512 + (kc + 1) * 128],
                                                                Vw[:, 0:512], start=True, stop=True), reads=[Kc, Vw], writes=[pk])
                    tk = P.op("dve", lambda e, pk=pk, kc=kc: e.scalar_tensor_tensor(
                        C32[:, hd, kc, 0:512], C32[:, hd, kc, 0:512], ebl, pk[:, :], ALU.mult, ALU.add),
                        reads=[pk, EBL], extra=[ctok.get(hd)])
                    toks.append(tk)

                def mmsn(e):
                    for kc in range(4):
                        ins = e.matmul(psn[:, kc:kc + 1], Kc[:, hd * 512 + kc * 128:hd * 512 + (kc + 1) * 128], Vw[:, 512:513],
                                       start=True, stop=True)
                    return ins
                P.op("pe", mmsn, reads=[Kc, Vw], writes=[small])
                tk = P.op("dve", lambda e: e.scalar_tensor_tensor(
                    C32[:, hd, :, 512], C32[:, hd, :, 512], ebl, psn[:, 0:4], ALU.mult, ALU.add),
                    reads=[small, EBL], extra=[ctok.get(hd)])
                toks.append(tk)
                ctok[hd] = P.op("act", lambda e: e.activation(Cbf[:, hd, :, 0:513], C32[:, hd, :, :], AF.Copy),
                                extra=toks + [ctok.get(hd), st["tok_mmn"]])
            if hd == 3:
                chunk_end(tt)

        def chunk_end(tt, r=r):
            ch = CH[tt]
            if not ch["emit"]:
                return
            hF = ch["hF"]
            is_ctx = ch["is_ctx"]
            if r == 0:
                P.dma("sp", S["HF"][tt * 128:(tt + 1) * 128, :].rearrange("p (h v) -> p h v", h=4), hF[:], reads=[hF],
                      writes=[self.dbuf(("HF", j, tt))])
                return
            for i in range(3):
                P.dma("sp", ozx[i][:], S["OZX"][i, tt], writes=[ozx[i]])
            P.op("dve", lambda e: [e.bn_stats(stats[:, hd, :], h32[:, hd, :]) for hd in range(4)][-1], reads=[h32], writes=[stats])
            P.op("dve", lambda e: [e.bn_aggr(mv[:, hd, :], stats[:, hd, :]) for hd in range(4)][-1], reads=[stats], writes=[mv])
            P.op("act", lambda e: e.activation(rs[:], mv[:, :, 1], AF.Sqrt, bias=EPS), reads=[mv], writes=[rs])
            P.op("dve", lambda e: e.reciprocal(rs[:], rs[:]), reads=[rs], writes=[rs])
            hn.begin()
            for hd in range(4):
                P.op("dve", lambda e, hd=hd: e.tensor_scalar(hn[:, hd, :], h32[:, hd, :], mv[:, hd, 0:1], rs[:, hd:hd + 1],
                                                             ALU.subtract, ALU.mult), reads=[h32, mv, rs], pw=[hn])
            t1.begin()
            for half in range(2):
                ptb = pb.next()
                ptv = ptb.ap.bitcast(BF16)

                def tr(e, ptv=ptv, half=half):
                    for c8 in range(8):
                        c = half * 8 + c8
                        ins = e.transpose(ptv[:, c8 * 128:(c8 + 1) * 128], hn[:, c // 4, (c % 4) * 128:(c % 4 + 1) * 128], self.ident[:])
                    return ins
                P.op("pe", tr, reads=[hn, self.ident], writes=[ptb])
                pv = ptv[:, 0:1024].rearrange("p (c t) -> p c t", c=8)
                P.op("dve", lambda e, pv=pv, half=half: e.tensor_tensor(t1[:, half * 8:(half + 1) * 8, :], pv,
                                                                         ozx[0][:, half * 8:(half + 1) * 8, :], ALU.mult),
                     reads=[ptb, ozx[0]], pw=[t1])
            P.op("pool", lambda e: e.tensor_tensor(t1[:], t1[:], ozx[2][:], ALU.add), reads=[ozx[2]], writes=[t1])
            P.op("pool", lambda e: e.tensor_tensor(yT[:], t1[:], ozx[1][:], ALU.mult), reads=[t1, ozx[1]], writes=[yT])
            if is_ctx:
                self.out_stage(yT, 0, "ctx", tt, False, self.gc_bc, c_src, keyc, "xc", self.xcs, wout, xt_pool, xn_pool, pb)
            else:
                self.out_stage(yT, 0, "lat", tt - 2, False, self.g_bc, x_src, keyx, "xs", self.xs, wout, xt_pool, xn_pool, pb)

        items = [(tt, hd) for tt in order for hd in range(4)]
        prev = None
        for it in items:
            st = stage_A(*it)
            if prev is not None:
                stage_B(prev[0], prev[1], prev[2])
                stage_C(prev[0], prev[1], prev[2])
            prev = (it[0], it[1], st)
        stage_B(prev[0], prev[1], prev[2])
        stage_C(prev[0], prev[1], prev[2])
        self.barrier()
        if self.stop == "P1":
            return


KB.mlstm_scan = mlstm_scan


_CACHE = {}
LAUNCH_GROUPS = ((0, 1, 2, 3),)


def _get_prog(N, layers, final):
    key = (N, layers, final)
    if key not in _CACHE:
        kb = KB(N=N, layers=layers, final=final, dbg=("xs", "xcs"))
        nc = kb.build()
        _CACHE[key] = (kb, nc)
    return _CACHE[key]


def kernel(**inputs):
    x = np.asarray(inputs["x"], dtype=np.float32)
    B, N, _ = x.shape
    sh = host_shared(inputs)
    cores = [host_core(inputs, b) for b in range(B)]
    out = None
    for gi, layers in enumerate(LAUNCH_GROUPS):
        final = (3 in layers)
        kb, nc = _get_prog(N, tuple(layers), final)
        in_maps = []
        for b in range(B):
            m = dict(sh)
            m.update(cores[b])
            in_maps.append({k: v for k, v in m.items() if k in kb.din})
        res = run_bass_kernel_spmd(nc, in_maps, core_ids=list(range(B)))
        for b in range(B):
            r = res.results[b]
            if final:
                continue
            cores[b]["x"] = np.asarray(r["xs"], dtype=np.float32)
            if any(l < 2 for l in layers):
                cores[b]["ctx"] = np.asarray(r["xcs"], dtype=np.float32)
        if final:
            out = np.stack([np.asarray(res.results[b]["out"], dtype=np.float32) for b in range(B)], axis=0)
    return out
```

```python
import math
from contextlib import ExitStack

import numpy as np
import ml_dtypes
import concourse.bass as bass
import concourse.mybir as mybir
from concourse.bass_utils import run_bass_kernel_spmd

F32 = mybir.dt.float32
BF16 = mybir.dt.bfloat16
AF = mybir.ActivationFunctionType
ALU = mybir.AluOpType
AX = mybir.AxisListType

D = 1024
E = 2048
NH = 4
DH = 512
CTX = 256
GW = 64
EPS = 1e-6
NDS = 12


class Buf:
    def __init__(self, ap, name=""):
        self.ap = ap
        self.name = name
        self.ws = []
        self.r = []
        self.prev = []
        self.excl = False

    def begin(self):
        self.prev = self.ws + self.r
        self.ws = []
        self.r = []
        return self

    def __getitem__(self, k):
        return self.ap[k]


class Prog:
    CE = ("pe", "dve", "act", "pool")
    DQ = ("sp", "act", "pool")

    def __init__(self, nc, es):
        self.nc = nc
        self.es = es
        self.q = {e: [] for e in ("pe", "dve", "act", "pool", "sp")}
        self.sems = {}
        self.cnt = {}
        for e in self.CE:
            self.sems[e] = es.enter_context(nc.semaphore("s_" + e))
            self.cnt[e] = 0
        self.dq_i = {}
        for qn in self.DQ:
            self.dq_i[qn] = 0
            for i in range(NDS):
                k = ("d", qn, i)
                self.sems[k] = es.enter_context(nc.semaphore("d_%s%d" % (qn, i)))
                self.cnt[k] = 0
        self.waited = {e: {} for e in self.q}
        self.n_ops = 0

    def _needed(self, eng, toks):
        out = []
        w = self.waited[eng]
        best = {}
        for t in toks:
            if t is None:
                continue
            k, v = t
            if k == eng and eng == "pe":
                continue
            if w.get(k, 0) >= v:
                continue
            if best.get(k, 0) < v:
                best[k] = v
        for k, v in best.items():
            w[k] = v
            out.append((k, v))
        return out

    def op(self, eng, fn, reads=(), writes=(), extra=(), pw=()):
        toks = list(extra)
        for b in pw:
            toks.extend(b.prev)
        for b in reads:
            toks.extend(b.ws)
            if b.excl:
                toks.extend(t for t in b.r if t[0] != eng)
        for b in writes:
            toks.extend(b.ws)
            toks.extend(b.r)
        waits = self._needed(eng, toks)
        self.cnt[eng] += 1
        tok = (eng, self.cnt[eng])
        self.q[eng].append((waits, fn, eng, 1))
        for b in reads:
            b.r.append(tok)
        for b in writes:
            b.ws = [tok]
            b.r = []
        for b in pw:
            b.ws.append(tok)
        self.n_ops += 1
        return tok

    def dma(self, qn, out_ap, in_ap, reads=(), writes=(), extra=(), also=(), **kw):
        i = self.dq_i[qn] % NDS
        self.dq_i[qn] += 1
        k = ("d", qn, i)
        toks = list(extra)
        if self.cnt[k] > 0:
            toks.append((k, self.cnt[k]))
        for b in reads:
            toks.extend(b.ws)
        for b in writes:
            toks.extend(b.ws)
            toks.extend(b.r)
        waits = self._needed(qn, toks)
        self.cnt[k] += 16
        tok = (k, self.cnt[k])

        def fn(e, out_ap=out_ap, in_ap=in_ap, kw=kw):
            return e.dma_start(out=out_ap, in_=in_ap, **kw)

        self.q[qn].append((waits, fn, k, 16))
        for b in reads:
            b.r.append(tok)
        for b in writes:
            b.ws = [tok]
            b.r = []
        for b in also:
            b.ws.append(tok)
        return tok

    def wait_all(self, eng, toks):
        waits = self._needed(eng, toks)
        self.q[eng].append((waits, None, None, 0))

    def cut(self):
        for name in self.q:
            self.q[name].append("CUT")

    def replay(self):
        nc = self.nc
        sems = self.sems
        segs = {}
        nseg = 0
        for name, items in self.q.items():
            cur = []
            lst = [cur]
            for it in items:
                if it == "CUT":
                    cur = []
                    lst.append(cur)
                else:
                    cur.append(it)
            segs[name] = lst
            nseg = max(nseg, len(lst))
        for si in range(nseg):
            if not any(len(segs[n][si]) for n in segs if si < len(segs[n])):
                continue
            with nc.Block() as block:
                def run(eng, name, si=si):
                    for waits, fn, sk, inc in segs[name][si]:
                        for k, v in waits:
                            eng.wait_ge(sems[k], v)
                        if fn is not None:
                            ins = fn(eng)
                            ins.then_inc(sems[sk], inc)

                @block.tensor
                def _(e):
                    run(e, "pe")

                @block.vector
                def _(e):
                    run(e, "dve")

                @block.scalar
                def _(e):
                    run(e, "act")

                @block.gpsimd
                def _(e):
                    run(e, "pool")

                @block.sync
                def _(e):
                    run(e, "sp")


class Pool:
    def __init__(self, bufs):
        self.bufs = bufs
        self.i = 0

    def next(self):
        b = self.bufs[self.i % len(self.bufs)]
        self.i += 1
        return b


class Arena:
    def __init__(self, ap):
        self.ap = ap
        self.top = 0
        self.W = ap.shape[1]
        self.peak = 0

    def alloc(self, name, free_shape, dt):
        n = 1
        for s in free_shape:
            n *= s
        esz = 4 if dt == F32 else 2
        words = (n * esz + 3) // 4
        words = (words + 7) // 8 * 8
        a = self.top
        self.top += words
        self.peak = max(self.peak, self.top)
        assert self.top <= self.W, "SBUF arena overflow at %s: %d > %d" % (name, self.top, self.W)
        v = self.ap[:, a:a + words]
        if dt != F32:
            v = v.bitcast(dt)
        v = v[:, 0:n]
        if len(free_shape) == 2:
            v = v.rearrange("p (a b) -> p a b", a=free_shape[0])
        elif len(free_shape) == 3:
            v = v.rearrange("p (a b c) -> p a b c", a=free_shape[0], b=free_shape[1])
        elif len(free_shape) == 4:
            v = v.rearrange("p (a b c d) -> p a b c d", a=free_shape[0], b=free_shape[1], c=free_shape[2])
        return Buf(v, name)

    def mark(self):
        return self.top

    def release(self, m):
        self.top = m


class K:
    pass


def fourier_tile_rows(dr, t):
    v = dr.rearrange("(n1 n2) d -> n2 n1 d", n2=GW)
    return [(0, 64, v[2 * t]), (64, 128, v[2 * t + 1])]


def natural_tile_rows(dr, t):
    return [(0, 128, dr[t * 128:(t + 1) * 128, :])]


def _prod(s):
    n = 1
    for v in s:
        n *= v
    return n


class KB:
    def __init__(self, N=4096, layers=(0, 1, 2, 3), final=True, dbg=()):
        self.N = N
        self.T = CTX + N
        self.layers = tuple(layers)
        self.final = final
        self.dbg = tuple(dbg)
        self.nc = bass.Bass("TRN2", target_bir_lowering=False)
        self.din = {}
        self.es = ExitStack()

    def inp(self, name, shape, dt=F32):
        t = self.nc.dram_tensor(name, list(shape), dt, kind="ExternalInput").ap()
        self.din[name] = t
        return t

    def outp(self, name, shape, dt=F32):
        return self.nc.dram_tensor(name, list(shape), dt, kind="ExternalOutput").ap()

    def scratch(self, name, shape, dt):
        kind = "ExternalOutput" if name in self.dbg else "Internal"
        return self.nc.dram_tensor(name, list(shape), dt, kind=kind).ap()

    def dbuf(self, key):
        b = self.dbufs.get(key)
        if b is None:
            b = Buf(None, str(key))
            self.dbufs[key] = b
        return b

    def barrier(self):
        P = self.P
        toks = [(k, v) for k, v in P.cnt.items() if v > 0]
        for e in ("pe", "dve", "act", "pool", "sp"):
            P.wait_all(e, toks)
        P.cut()

    def build(self):
        nc, es = self.nc, self.es
        N, T = self.N, self.T
        self.x_in = self.inp("x", [N, D])
        self.ctx_in = self.inp("ctx", [CTX, D])
        self.cvec = self.inp("cvec", [128, 8, 2])
        self.norm_g = self.inp("norm_g", [4, D])
        self.w_ada = self.inp("w_ada", [4, 128, 8, 3 * D])
        self.b_ada = self.inp("b_ada", [4, 3 * D])
        self.f_w_in = self.inp("f_w_in", [2, 128, 8, 2 * E])
        self.f_w_out = self.inp("f_w_out", [2, 128, 16, D])
        self.norm_f = self.inp("norm_f", [1, D])
        self.identb = self.inp("identb", [128, 128], BF16)
        self.c_cs = self.inp("c_cs", [128, 2, 512], BF16)
        self.c_a = self.inp("c_a", [128, 3, 128], BF16)
        self.c_bk = self.inp("c_bk", [128, 64, 64], BF16)
        self.c_d256 = self.inp("c_d256", [128, 2, 2, 256], BF16)
        self.declare_mlstm_inputs()
        self.out = self.outp("out", [N, D])
        self.xs = self.scratch("xs", [N, D], F32)
        self.xcs = self.scratch("xcs", [CTX, D], F32)
        self.dbufs = {}
        self.scr = {}
        with es:
            self.P = Prog(nc, es)
            sb_all = es.enter_context(nc.sbuf_tensor("sb_all", [128, 50 * 1024], F32))
            self.A = Arena(sb_all)
            self.psum = es.enter_context(nc.psum_tensor("ps_all", [128, 8 * 512], F32))
            self.pbank = [Buf(self.psum[:, b * 512:(b + 1) * 512], "bank%d" % b) for b in range(8)]
            for b_ in self.pbank:
                b_.excl = True
            A = self.A
            self.ident = A.alloc("ident", [128], BF16)
            self.P.dma("sp", self.ident[:], self.identb[:, :], writes=[self.ident])
            self.g_bc = A.alloc("g_bc", [D], F32)
            self.gc_bc = A.alloc("gc_bc", [D], F32)
            x_src, c_src = self.x_in, self.ctx_in
            for li in self.layers:
                upd_ctx = li < 2
                need_ctx = upd_ctx or (li % 2 == 0)
                fourier = (li % 2 == 1)
                mL = A.mark()
                if fourier:
                    self.fTc = A.alloc("fTc", [16, 256], F32)
                else:
                    self.G = A.alloc("G", [T // 128, 16], F32)
                    self.bif = A.alloc("bif", [16], F32)
                self.m_hT = A.mark()
                self.hT = A.alloc("hT", [8, T], BF16)
                m = A.mark()
                self.prep(li, x_src, c_src, need_ctx, fourier)
                self.barrier()
                A.release(m)
                if fourier:
                    self.fourier_layer(li, x_src, c_src, upd_ctx)
                else:
                    self.mlstm_layer(li, x_src, c_src, upd_ctx)
                self.barrier()
                A.release(mL)
                x_src = self.xs
                if upd_ctx:
                    c_src = self.xcs
            if self.final:
                self.final_norm(x_src)
            self.barrier()
            self.P.replay()
        return nc

    def declare_mlstm_inputs(self):
        pass

    def mlstm_layer(self, li, x_src, c_src, upd_ctx):
        raise NotImplementedError

    def tiles(self, fourier, need_ctx):
        out = []
        if need_ctx:
            for t in range(CTX // 128):
                out.append(("ctx", t, t * 128))
        for t in range(self.N // 128):
            out.append(("lat", t, CTX + t * 128))
        return out

    def tile_rows(self, stream, t, fourier, dr):
        if stream == "lat" and fourier:
            return fourier_tile_rows(dr, t)
        return natural_tile_rows(dr, t)

    def load_rows(self, q, buf, stream, t, fourier, dr, key):
        P = self.P
        toks = []
        db = self.dbuf((key, stream, t))
        first = True
        for (p0, p1, ap) in self.tile_rows(stream, t, fourier, dr):
            if first:
                tok = P.dma(q, buf.ap[p0:p1], ap, reads=[db], writes=[buf])
                first = False
            else:
                tok = P.dma(q, buf.ap[p0:p1], ap, reads=[db], also=[buf])
            toks.append(tok)
        return toks

    def store_rows(self, q, buf, stream, t, fourier, dr, key, extra=()):
        P = self.P
        db = self.dbuf((key, stream, t))
        toks = []
        for (p0, p1, ap) in self.tile_rows(stream, t, fourier, dr):
            toks.append(P.dma(q, ap, buf.ap[p0:p1], reads=[buf], writes=[db], extra=extra))
        return toks

    def prep(self, li, x_src, c_src, need_ctx, fourier):
        P, A = self.P, self.A
        cv = A.alloc("cv", [8, 2], F32)
        sv = A.alloc("sv", [8, 2], F32)
        ones = A.alloc("ones", [128], F32)
        sbc = A.alloc("sbc", [2, 8, 128], F32)
        mod = [A.alloc("mod%d" % s, [3 * D], F32) for s in range(2)]
        bbc = A.alloc("bbc", [3 * D], F32)
        gn = A.alloc("gn", [D], F32)
        abc = [A.alloc("abc%d" % s, [D], F32) for s in range(2)]
        wst = Pool([A.alloc("wst%d" % i, [8, 256], F32) for i in range(2)])
        P.dma("sp", cv[:], self.cvec[:, :, :], writes=[cv])
        P.dma("sp", bbc[:], self.b_ada[li].partition_broadcast(128), writes=[bbc])
        P.dma("sp", gn[:], self.norm_g[li].partition_broadcast(128), writes=[gn])
        P.op("act", lambda e: e.activation(sv[:], cv[:], AF.Silu), reads=[cv], writes=[sv])
        P.op("pool", lambda e: e.memset(ones[:], 1.0), writes=[ones])
        nstream = 2 if need_ctx else 1

        def mk_sbc(e):
            for s in range(nstream):
                for kc in range(8):
                    ins = e.tensor_scalar(sbc[:, s, kc, :], ones[:], sv[:, kc, s:s + 1], None, ALU.mult)
            return ins
        P.op("dve", mk_sbc, reads=[ones, sv], writes=[sbc])
        pb = Pool([self.pbank[0], self.pbank[1]])
        for blk in range(12):
            w = wst.next()
            P.dma("sp", w[:], self.w_ada[li, :, :, blk * 256:(blk + 1) * 256], writes=[w])
            for s in range(nstream):
                pm = pb.next()

                def mm(e, s=s, w=w, pm=pm):
                    for kc in range(8):
                        ins = e.matmul(pm[:, 0:256], sbc[:, s, kc, :], w[:, kc, :], start=(kc == 0), stop=(kc == 7))
                    return ins
                P.op("pe", mm, reads=[sbc, w], writes=[pm])
                sl = slice(blk * 256, (blk + 1) * 256)
                P.op("dve", lambda e, s=s, pm=pm, sl=sl: e.tensor_tensor(mod[s][:, sl], pm[:, 0:256], bbc[:, sl], ALU.add),
                     reads=[pm, bbc], writes=[mod[s]])
        for s in range(nstream):
            P.op("dve", lambda e, s=s: e.scalar_tensor_tensor(abc[s][:], mod[s][:, D:2 * D], 1.0, gn[:], ALU.add, ALU.mult),
                 reads=[mod[s], gn], writes=[abc[s]])
        P.op("pool", lambda e: e.tensor_copy(self.g_bc[:], mod[0][:, 2 * D:3 * D]), reads=[mod[0]], writes=[self.g_bc])
        if need_ctx:
            P.op("pool", lambda e: e.tensor_copy(self.gc_bc[:], mod[1][:, 2 * D:3 * D]), reads=[mod[1]], writes=[self.gc_bc])
        xt_pool = Pool([A.alloc("xt%d" % i, [D], F32) for i in range(3)])
        junk = A.alloc("junk", [D], F32)
        t1_pool = Pool([A.alloc("t1_%d" % i, [D], F32) for i in range(2)])
        xn_pool = Pool([A.alloc("xn%d" % i, [D], BF16) for i in range(2)])
        st_pool = Pool([A.alloc("st%d" % i, [2], F32) for i in range(4)])
        pt_pool = Pool([self.pbank[2], self.pbank[3]])
        key = "xc" if c_src is self.xcs else "cin"
        keyx = "xs" if x_src is self.xs else "xin"
        self.hT.begin()
        for ti, (stream, t, pos0) in enumerate(self.tiles(fourier, need_ctx)):
            s = 1 if stream == "ctx" else 0
            dr = c_src if stream == "ctx" else x_src
            xt = xt_pool.next()
            self.load_rows("sp", xt, stream, t, fourier, dr, key if stream == "ctx" else keyx)
            st = st_pool.next()
            P.op("act", lambda e, xt=xt, st=st: e.activation(junk[:], xt[:], AF.Square, accum_out=st[:, 0:1]),
                 reads=[xt], writes=[junk, st])
            P.op("act", lambda e, st=st: e.activation(st[:, 1:2], st[:, 0:1], AF.Sqrt, bias=EPS, scale=1.0 / D),
                 reads=[st], writes=[st])
            P.op("dve", lambda e, st=st: e.reciprocal(st[:, 1:2], st[:, 1:2]), reads=[st], writes=[st])
            t1 = t1_pool.next()
            P.op("dve", lambda e, xt=xt, st=st, t1=t1, s=s: e.scalar_tensor_tensor(
                t1[:], xt[:], st[:, 1:2], abc[s][:], ALU.mult, ALU.mult), reads=[xt, st, abc[s]], writes=[t1])
            xn = xn_pool.next()
            P.op("pool", lambda e, t1=t1, xn=xn, s=s: e.tensor_tensor(xn[:], t1[:], mod[s][:, 0:D], ALU.add),
                 reads=[t1, mod[s]], writes=[xn])
            pt = pt_pool.next()
            ptv = pt.ap.bitcast(BF16)

            def tr(e, xn=xn, ptv=ptv):
                for c in range(8):
                    ins = e.transpose(ptv[:, c * 128:(c + 1) * 128], xn[:, c * 128:(c + 1) * 128], self.ident[:])
                return ins
            P.op("pe", tr, reads=[xn, self.ident], writes=[pt])
            eng = "act" if ti % 2 == 0 else "dve"
            hv = self.hT[:, :, pos0:pos0 + 128]
            pv = ptv[:, 0:1024].rearrange("p (c t) -> p c t", c=8)
            if eng == "act":
                P.op("act", lambda e, hv=hv, pv=pv: e.activation(hv, pv, AF.Copy), reads=[pt], pw=[self.hT])
            else:
                P.op("dve", lambda e, hv=hv, pv=pv: e.tensor_copy(hv, pv), reads=[pt], pw=[self.hT])

    def final_norm(self, x_src):
        P, A = self.P, self.A
        m = A.mark()
        nf = A.alloc("nf", [D], F32)
        P.dma("sp", nf[:], self.norm_f[0].partition_broadcast(128), writes=[nf])
        xt_pool = Pool([A.alloc("fxt%d" % i, [D], F32) for i in range(3)])
        yo_pool = Pool([A.alloc("fyo%d" % i, [D], F32) for i in range(3)])
        junk = A.alloc("fjunk", [D], F32)
        st_pool = Pool([A.alloc("fst%d" % i, [2], F32) for i in range(4)])
        keyx = "xs" if x_src is self.xs else "xin"
        last = []
        for t in range(self.N // 128):
            xt = xt_pool.next()
            self.load_rows("sp", xt, "lat", t, False, x_src, keyx)
            st = st_pool.next()
            P.op("act", lambda e, xt=xt, st=st: e.activation(junk[:], xt[:], AF.Square, accum_out=st[:, 0:1]),
                 reads=[xt], writes=[junk, st])
            P.op("act", lambda e, st=st: e.activation(st[:, 1:2], st[:, 0:1], AF.Sqrt, bias=EPS, scale=1.0 / D),
                 reads=[st], writes=[st])
            P.op("dve", lambda e, st=st: e.reciprocal(st[:, 1:2], st[:, 1:2]), reads=[st], writes=[st])
            yo = yo_pool.next()
            P.op("dve", lambda e, xt=xt, st=st, yo=yo: e.scalar_tensor_tensor(
                yo[:], xt[:], st[:, 1:2], nf[:], ALU.mult, ALU.mult), reads=[xt, st, nf], writes=[yo])
            last += self.store_rows("sp", yo, "lat", t, False, self.out, "out")
        A.release(m)


def _load_w(self, dst_view, src_ap, kc, ncols, stage_pool, engs=("pool",)):
    P = self.P
    piece = max(1, stage_pool.bufs[0].ap.shape[1] // kc)
    c0 = 0
    i = 0
    while c0 < ncols:
        w = min(piece, ncols - c0)
        st = stage_pool.next()
        sv = st.ap[:, 0:kc * w].rearrange("p (k n) -> p k n", k=kc)
        P.dma("sp", sv, src_ap[:, :, c0:c0 + w], writes=[st])
        eng = engs[i % len(engs)]
        dv = dst_view.ap[:, :, c0:c0 + w] if isinstance(dst_view, Buf) else dst_view[:, :, c0:c0 + w]
        if eng == "act":
            P.op("act", lambda e, dv=dv, sv=sv: e.activation(dv, sv, AF.Copy), reads=[st], pw=[self._lw_buf])
        else:
            P.op(eng, lambda e, dv=dv, sv=sv: e.tensor_copy(dv, sv), reads=[st], pw=[self._lw_buf])
        c0 += w
        i += 1


def load_w(self, dst_buf, src_ap, kc, ncols, stage_pool, engs=("pool",), view=None):
    self._lw_buf = dst_buf
    dst_buf.begin()
    _load_w(self, dst_buf if view is None else view, src_ap, kc, ncols, stage_pool, engs)


KB.load_w = load_w


def fourier_layer(self, li, x_src, c_src, upd_ctx):
    P, A = self.P, self.A
    hT = self.hT
    N, T = self.N, self.T
    j = li // 2
    NT = N // 128
    Y = self.scratch("Y%d" % li, [64, 2, 64, E], BF16)
    ZS = self.scratch("ZS%d" % li, [16, 128, T], BF16)
    pb = Pool(self.pbank)
    fTc = self.fTc
    m0 = A.mark()
    cs = A.alloc("cs", [2, 512], BF16)
    ca = A.alloc("ca", [3, 128], BF16)
    d256 = A.alloc("d256", [2, 2, 256], BF16)
    P.dma("sp", cs[:], self.c_cs[:, :, :], writes=[cs])
    P.dma("sp", ca[:], self.c_a[:, :, :], writes=[ca])
    P.dma("sp", d256[:], self.c_d256[:, :, :, :], writes=[d256])
    stage = Pool([A.alloc("wstg%d" % i, [2048], F32) for i in range(2)])
    wb_pool = Pool([A.alloc("wb%d" % i, [8, 512], BF16) for i in range(2)])
    uT = A.alloc("uT", [4, T], BF16)
    UT_pool = Pool([A.alloc("UT%d" % i, [2, 2, 256], BF16) for i in range(3)])
    UTc = A.alloc("UTc", [2, 2, 2, 256], BF16) if upd_ctx else None
    Ysb_pool = Pool([A.alloc("Ysb%d" % i, [2, 512], BF16) for i in range(3)])
    zsb_pool = Pool([A.alloc("zsb%d" % i, [4, 512], BF16) for i in range(2)])
    ttiles = []
    if upd_ctx:
        ttiles.append((0, 256))
    for i in range(N // 512):
        ttiles.append((CTX + i * 512, 512))
    ptiles = []
    if upd_ctx:
        ptiles += [("ctx", 0, 0), ("ctx", 1, 128)]
    ptiles += [("lat", t, CTX + t * 128) for t in range(NT)]
    ev = 0
    if upd_ctx:
        fTc.begin()
    for fb in range(4):
        wb = wb_pool.next()
        self.load_w(wb, self.f_w_in[j, :, :, fb * 512:(fb + 1) * 512], 8, 512, stage)
        uT.begin()
        for (pos, w) in ttiles:
            for cc in range(4):
                pm = pb.next()

                def mm(e, pm=pm, wb=wb, cc=cc, pos=pos, w=w):
                    for kc in range(8):
                        ins = e.matmul(pm[:, 0:w], wb[:, kc, cc * 128:(cc + 1) * 128], hT[:, kc, pos:pos + w],
                                       start=(kc == 0), stop=(kc == 7))
                    return ins
                P.op("pe", mm, reads=[wb, hT], writes=[pm])
                ev += 1
                if ev % 2 == 0:
                    P.op("act", lambda e, pm=pm, cc=cc, pos=pos, w=w: e.activation(uT[:, cc, pos:pos + w], pm[:, 0:w], AF.Copy),
                         reads=[pm], pw=[uT])
                else:
                    P.op("dve", lambda e, pm=pm, cc=cc, pos=pos, w=w: e.tensor_copy(uT[:, cc, pos:pos + w], pm[:, 0:w]),
                         reads=[pm], pw=[uT])
        if upd_ctx:
            UTc.begin()
        for (stream, t, pos) in ptiles:
            UT = UT_pool.next().begin() if stream == "lat" else None
            for g in range(2):
                pm = pb.next()

                def mm2(e, pm=pm, g=g, pos=pos):
                    for cc in range(2):
                        ins = e.matmul(pm[:, :], uT[:, 2 * g + cc, pos:pos + 128], cs[:, cc, :],
                                       start=(cc == 0), stop=(cc == 1))
                    return ins
                P.op("pe", mm2, reads=[uT, cs], writes=[pm])
                if stream == "lat":
                    ov = UT[:, :, g, :]
                    ob = UT
                else:
                    ov = UTc[:, t, :, g, :]
                    ob = UTc
                pv = pm.ap.rearrange("p (a b) -> p a b", a=2)
                ev += 1
                if ev % 2 == 0:
                    P.op("act", lambda e, ov=ov, pv=pv: e.activation(ov, pv, AF.Copy), reads=[pm], pw=[ob])
                else:
                    P.op("dve", lambda e, ov=ov, pv=pv: e.tensor_copy(ov, pv), reads=[pm], pw=[ob])
            if stream == "lat":
                p_r = pb.next()
                p_i = pb.next()
                uc = UT[:, 0, :, :].rearrange("p g f -> p (g f)")
                us = UT[:, 1, :, :].rearrange("p g f -> p (g f)")

                def mmA(e, p_r=p_r, p_i=p_i, uc=uc, us=us):
                    e.matmul(p_r[:, :], ca[:, 0, :], uc, start=True, stop=False)
                    e.matmul(p_r[:, :], ca[:, 1, :], us, start=False, stop=True)
                    e.matmul(p_i[:, :], ca[:, 1, :], uc, start=True, stop=False)
                    return e.matmul(p_i[:, :], ca[:, 2, :], us, start=False, stop=True)
                P.op("pe", mmA, reads=[UT, ca], writes=[p_r, p_i])
                Ysb = Ysb_pool.next().begin()
                P.op("act", lambda e, Ysb=Ysb, p_r=p_r: e.activation(Ysb[:, 0, :], p_r[:, :], AF.Copy), reads=[p_r], pw=[Ysb])
                P.op("dve", lambda e, Ysb=Ysb, p_i=p_i: e.tensor_copy(Ysb[:, 1, :], p_i[:, :]), reads=[p_i], pw=[Ysb])
                for jj in range(2):
                    P.dma("sp", Y[:, :, 2 * t + jj, fb * 512:(fb + 1) * 512], Ysb.ap[jj * 64:(jj + 1) * 64, :, :],
                          reads=[Ysb], writes=[self.dbuf(("Y", t, fb, jj))])
        if upd_ctx:
            for g in range(2):
                for half in range(2):
                    pm = pb.next()

                    def mmc(e, pm=pm, g=g, half=half):
                        n = 0
                        for tile in range(2):
                            for cs_ in range(2):
                                ins = e.matmul(pm[:, 0:256], UTc[:, tile, cs_, g, half * 128:(half + 1) * 128],
                                               d256[:, tile, cs_, :], start=(n == 0), stop=(n == 3))
                                n += 1
                        return ins
                    P.op("pe", mmc, reads=[UTc, d256], writes=[pm])
                    c = fb * 4 + g * 2 + half
                    P.op("dve", lambda e, pm=pm, c=c: e.tensor_copy(fTc[:, c, :], pm[:, 0:256]), reads=[pm], pw=[fTc])
        wz = wb_pool.next()
        self.load_w(wz, self.f_w_in[j, :, :, E + fb * 512:E + (fb + 1) * 512], 8, 512, stage)
        for (pos, w) in ttiles:
            zsb = zsb_pool.next().begin()
            for cc in range(4):
                pm = pb.next()

                def mmz(e, pm=pm, wz=wz, cc=cc, pos=pos, w=w):
                    for kc in range(8):
                        ins = e.matmul(pm[:, 0:w], wz[:, kc, cc * 128:(cc + 1) * 128], hT[:, kc, pos:pos + w],
                                       start=(kc == 0), stop=(kc == 7))
                    return ins
                P.op("pe", mmz, reads=[wz, hT], writes=[pm])
                P.op("act", lambda e, zsb=zsb, pm=pm, cc=cc, w=w: e.activation(zsb[:, cc, 0:w], pm[:, 0:w], AF.Silu),
                     reads=[pm], pw=[zsb])
            P.dma("sp", ZS[fb * 4:(fb + 1) * 4, :, pos:pos + w].rearrange("c p t -> p c t"), zsb.ap[:, :, 0:w],
                  reads=[zsb], writes=[self.dbuf(("ZS", fb, pos))])
    self.barrier()
    A.release(self.m_hT)
    wout = A.alloc("wout", [16, D], BF16)
    stage = Pool([A.alloc("wstg%d" % i, [1024], F32) for i in range(2)])
    self.load_w(wout, self.f_w_out[j], 16, D, stage, engs=("pool", "dve"))
    bk = A.alloc("bk", [64, 64], BF16)
    P.dma("sp", bk[:], self.c_bk[:, :, :], writes=[bk])
    zs_pool = Pool([A.alloc("zs%d" % i, [16, 512], BF16) for i in range(2)])
    Yk_pool = Pool([A.alloc("Yk%d" % i, [E], BF16) for i in range(9)])
    yT_pool = Pool([A.alloc("yT%d" % i, [16, 512], BF16) for i in range(2)])
    xt_pool = Pool([A.alloc("fx%d" % i, [D], F32) for i in range(2)])
    xn_pool = Pool([A.alloc("fxn%d" % i, [D], F32) for i in range(2)])
    keyx = "xs" if x_src is self.xs else "xin"

    def out_stage(yT, width_off, stream, t, gb, src, key_in, key_out, dst):
        xt = xt_pool.next()
        self.load_rows("sp", xt, stream, t, stream == "lat", src, key_in)
        xn = xn_pool.next().begin()
        for half in range(2):
            po = pb.next()

            def mmo(e, po=po, half=half):
                for c in range(16):
                    ins = e.matmul(po[:, :], yT[:, c, width_off:width_off + 128], wout[:, c, half * 512:(half + 1) * 512],
                                   start=(c == 0), stop=(c == 15))
                return ins
            P.op("pe", mmo, reads=[yT, wout], writes=[po])
            hs = slice(half * 512, (half + 1) * 512)
            P.op("dve", lambda e, po=po, hs=hs: e.tensor_tensor(xn[:, hs], po[:, :], gb[:, hs], ALU.mult),
                 reads=[po, gb], pw=[xn])
        P.op("pool", lambda e: e.tensor_tensor(xn[:], xn[:], xt[:], ALU.add), reads=[xt], writes=[xn])
        self.store_rows("sp", xn, stream, t, stream == "lat", dst, key_out)

    for st in range(N // 512):
        pos = CTX + st * 512
        zs = zs_pool.next()
        P.dma("sp", zs[:], ZS[:, :, pos:pos + 512].rearrange("c p t -> p c t"), writes=[zs])
        Yks = []
        for kk in range(8):
            k1 = st * 8 + kk
            Yk = Yk_pool.next()
            P.dma("sp", Yk[:], Y[k1].rearrange("r n f -> (r n) f"), writes=[Yk])
            Yks.append(Yk)
        yT = yT_pool.next().begin()
        for c in range(16):
            pf = pb.next()

            def mmf(e, pf=pf, c=c, Yks=Yks, st=st):
                for kk in range(8):
                    ins = e.matmul(pf[:, kk * 64:(kk + 1) * 64], Yks[kk][:, c * 128:(c + 1) * 128], bk[:, st * 8 + kk, :],
                                   start=True, stop=True)
                return ins
            P.op("pe", mmf, reads=Yks + [bk], writes=[pf])
            P.op("dve", lambda e, pf=pf, c=c, yT=yT, zs=zs: e.tensor_tensor(yT[:, c, :], pf[:, :], zs[:, c, :], ALU.mult),
                 reads=[pf, zs], pw=[yT])
        for i in range(4):
            out_stage(yT, i * 128, "lat", st * 4 + i, self.g_bc, x_src, keyx, "xs", self.xs)
    if upd_ctx:
        zs = zs_pool.next()
        P.dma("sp", zs.ap[:, :, 0:256], ZS[:, :, 0:256].rearrange("c p t -> p c t"), writes=[zs])
        yT = yT_pool.next()
        P.op("dve", lambda e: e.tensor_tensor(yT.ap[:, :, 0:256], fTc[:, :, :], zs.ap[:, :, 0:256], ALU.mult),
             reads=[fTc, zs], writes=[yT])
        keyc = "xc" if c_src is self.xcs else "cin"
        for t in range(2):
            out_stage(yT, t * 128, "ctx", t, self.gc_bc, c_src, keyc, "xc", self.xcs)


KB.fourier_layer = fourier_layer


def _bf(a):
    return np.ascontiguousarray(a.astype(np.float32)).astype(ml_dtypes.bfloat16)


def make_consts():
    c = {}
    c["identb"] = _bf(np.eye(128))
    p = np.arange(128)
    cc = np.arange(2)
    ch = (cc[None, :] * 128 + p[:, None])
    k = np.arange(256)
    ang = 2 * np.pi * ((ch[:, :, None] * k[None, None, :]) % 256) / 256.0
    c["c_cs"] = _bf(np.concatenate([np.cos(ang), np.sin(ang)], axis=-1) / 16.0)
    n1 = np.arange(64)
    a64 = 2 * np.pi * ((n1[:, None] * n1[None, :]) % 64) / 64.0
    C, S = np.cos(a64), np.sin(a64)
    Z = np.zeros((64, 64))
    bd = lambda M: np.block([[M, Z], [Z, M]])
    c["c_a"] = _bf(np.stack([bd(C), bd(-S), bd(-C)], axis=1))
    n2 = np.arange(64)[:, None, None]
    k1 = np.arange(64)[None, :, None]
    k2 = np.arange(64)[None, None, :]
    num = (n2 * k2 * 64 + n2 * k1) % 4096
    th = 2 * np.pi * num / 4096.0
    c["c_bk"] = _bf(np.concatenate([np.cos(th), np.sin(th)], axis=0) / 64.0)
    n = (np.arange(2)[None, :] * 128 + p[:, None])
    a256 = 2 * np.pi * ((n[:, :, None] * k[None, None, :]) % 256) / 256.0
    c["c_d256"] = _bf(np.stack([np.cos(a256), -np.sin(a256)], axis=2) / 16.0)
    return c


def _pk(w, kc):
    n = w.shape[-1]
    return np.ascontiguousarray(w.reshape(kc, 128, n).transpose(1, 0, 2))


def host_shared(inp):
    f = lambda a: np.asarray(a, dtype=np.float32)
    sh = {}
    sh["norm_g"] = f(inp["norm_g"])
    sh["w_ada"] = np.stack([_pk(f(inp["w_ada"][l]), 8) for l in range(4)])
    sh["b_ada"] = f(inp["b_ada"])
    sh["f_w_in"] = np.stack([_pk(f(inp["f_w_in"][l]), 8) for l in range(2)])
    sh["f_w_out"] = np.stack([_pk(f(inp["f_w_out"][l]), 16) for l in range(2)])
    sh["norm_f"] = f(inp["norm_f"]).reshape(1, D)
    sh.update(make_consts())
    sh.update(host_shared_mlstm(inp))
    return sh


def host_shared_mlstm(inp):
    return {}


def host_core(inp, b):
    f = lambda a: np.asarray(a, dtype=np.float32)
    d = {}
    d["x"] = np.ascontiguousarray(f(inp["x"][b]))
    d["ctx"] = np.ascontiguousarray(f(inp["ctx"][b]))
    cv = np.stack([f(inp["c"][b]), f(inp["c_ctx"])], axis=-1)
    d["cvec"] = np.ascontiguousarray(cv.reshape(8, 128, 2).transpose(1, 0, 2))
    return d


def declare_mlstm_inputs(self):
    self.m_w_in = self.inp("m_w_in", [2, 128, 8, 3 * E])
    self.m_conv_w = self.inp("m_conv_w", [2, 128, 16, 9])
    self.m_vecs = self.inp("m_vecs", [2, 128, 3, 16])
    self.m_w_q = self.inp("m_w_q", [2, 4, 128, 4, DH])
    self.m_w_k = self.inp("m_w_k", [2, 4, 128, 4, DH])
    self.m_w_if = self.inp("m_w_if", [2, 128, 3, 16, 16])
    self.m_bif = self.inp("m_bif", [2, 16])
    self.m_w_out = self.inp("m_w_out", [2, 128, 16, D])
    self.c_tri = self.inp("c_tri", [128, 3, 128])
    self.c_mask = self.inp("c_mask", [128, 2, 128])


KB.declare_mlstm_inputs = declare_mlstm_inputs


def host_shared_mlstm(inp):
    f = lambda a: np.asarray(a, dtype=np.float32)
    sh = {}
    sh["m_w_in"] = np.stack([_pk(f(inp["m_w_in"][l]), 8) for l in range(2)])
    cw = f(inp["m_conv_w"]).reshape(2, 9, E)
    sh["m_conv_w"] = np.ascontiguousarray(cw.reshape(2, 9, 16, 128).transpose(0, 3, 2, 1))
    vec = np.stack([f(inp["m_conv_b"]), f(inp["m_ln_w"]), f(inp["m_skip"])], axis=1)
    sh["m_vecs"] = np.ascontiguousarray(vec.reshape(2, 3, 16, 128).transpose(0, 3, 1, 2))
    sh["m_w_q"] = np.stack([np.stack([_pk(f(inp["m_w_q"][l][h]), 4) for h in range(4)]) for l in range(2)])
    sh["m_w_k"] = np.stack([np.stack([_pk(f(inp["m_w_k"][l][h]), 4) for h in range(4)]) for l in range(2)])
    wif = f(inp["m_w_if"])
    wif = wif.reshape(2, 2, 3, 16, 128, 8).transpose(0, 4, 2, 3, 1, 5)
    sh["m_w_if"] = np.ascontiguousarray(wif.reshape(2, 128, 3, 16, 16))
    bif = np.concatenate([f(inp["m_b_i"]), f(inp["m_b_f"])], axis=-1)
    sh["m_bif"] = np.ascontiguousarray(bif.reshape(2, 16))
    sh["m_w_out"] = np.stack([_pk(f(inp["m_w_out"][l]), 16) for l in range(2)])
    s_ = np.arange(128)[:, None]
    j_ = np.arange(128)[None, :]
    mf = (s_ <= j_).astype(np.float32)
    mb = (s_ >= j_).astype(np.float32)
    sh["c_tri"] = np.ascontiguousarray(np.stack([-mf, -mb, -np.ones((128, 128), np.float32)], axis=1))
    sh["c_mask"] = np.ascontiguousarray(np.stack([mf, mb], axis=1))
    return sh


def out_stage(self, yT, off, stream, t, fourier, gb, src, key_in, key_out, dst, wout, xt_pool, xn_pool, pb):
    P = self.P
    xt = xt_pool.next()
    self.load_rows("sp", xt, stream, t, fourier, src, key_in)
    xn = xn_pool.next().begin()
    for half in range(2):
        po = pb.next()

        def mmo(e, po=po, half=half):
            for c in range(16):
                ins = e.matmul(po[:, :], yT[:, c, off:off + 128], wout[:, c, half * 512:(half + 1) * 512],
                               start=(c == 0), stop=(c == 15))
            return ins
        P.op("pe", mmo, reads=[yT, wout], writes=[po])
        hs = slice(half * 512, (half + 1) * 512)
        P.op("dve", lambda e, po=po, hs=hs: e.tensor_tensor(xn[:, hs], po[:, :], gb[:, hs], ALU.mult),
             reads=[po, gb], pw=[xn])
    P.op("pool", lambda e: e.tensor_tensor(xn[:], xn[:], xt[:], ALU.add), reads=[xt], writes=[xn])
    self.store_rows("sp", xn, stream, t, fourier, dst, key_out)


KB.out_stage = out_stage


def mlstm_layer(self, li, x_src, c_src, upd_ctx):
    P, A = self.P, self.A
    hT = self.hT
    N, T = self.N, self.T
    R = N // GW
    j = li // 2
    NTT = T // 128
    pb = Pool(self.pbank)
    S = self.scr.get(j)
    if S is None:
        S = {}
        S["OZX"] = self.scratch("OZX%d" % j, [3, NTT, 128, 16, 128], BF16)
        S["QT"] = self.scratch("QT%d" % j, [NTT, 128, 4, 4, 128], BF16)
        S["KT"] = self.scratch("KT%d" % j, [NTT, 128, 4, 4, 128], BF16)
        S["Kt"] = self.scratch("Kt%d" % j, [T, E], BF16)
        S["Vt"] = self.scratch("Vt%d" % j, [T, E], BF16)
        S["HF"] = self.scratch("HF%d" % j, [T, E], BF16)
        self.scr[j] = S
    m0 = A.mark()
    G = self.G
    bif = self.bif
    P.dma("sp", bif[:], self.m_bif[j].partition_broadcast(128), writes=[bif])
    mB = A.mark()
    cw = A.alloc("cw", [16, 9], F32)
    vecs = A.alloc("vecs", [3, 16], F32)
    P.dma("sp", cw[:], self.m_conv_w[j], writes=[cw])
    P.dma("sp", vecs[:], self.m_vecs[j], writes=[vecs])
    wif32 = A.alloc("wif32", [3, 16, 16], F32)
    wif = A.alloc("wif", [3, 16, 16], BF16)
    P.dma("sp", wif32[:], self.m_w_if[j], writes=[wif32])
    P.op("pool", lambda e: e.tensor_copy(wif[:], wif32[:]), reads=[wif32], writes=[wif])
    stage = Pool([A.alloc("mstg%d" % i, [1024], F32) for i in range(2)])
    wb_pool = Pool([A.alloc("mwb%d" % i, [8, 512], BF16) for i in range(2)])
    wq = A.alloc("wq", [4, DH], BF16)
    wk = A.alloc("wk", [4, DH], BF16)
    LP = 260 + (R + 2) * 68
    xmpad = A.alloc("xmpad", [LP], BF16)
    vTc = A.alloc("vTc", [T], BF16)
    xcv = A.alloc("xcv", [4, T], BF16)
    dg_pool = Pool([A.alloc("dg%d" % i, [9, 128], BF16) for i in range(2)])
    tl_pool = Pool([A.alloc("tl%d" % i, [512], BF16) for i in range(4)])
    t32_pool = Pool([A.alloc("t32_%d" % i, [512], F32) for i in range(2)])
    qt_pool = Pool([A.alloc("qt%d" % i, [4, 512], BF16) for i in range(4)])
    xl = xmpad.ap[:, 260:LP].rearrange("p (r c) -> p r c", c=68)
    P.op("pool", lambda e: e.memset(xmpad[:], 0.0), writes=[xmpad])
    ttiles = [(0, 256)] + [(CTX + i * 512, 512) for i in range(N // 512)]
    first_g = [True] * NTT
    ev = [0]

    def evac(out_ap, in_ap, reads, pw=(), writes=(), func=None, scale=None, bias=None):
        ev[0] += 1
        if func is not None or ev[0] % 2 == 0:
            kw = {}
            if scale is not None:
                kw["scale"] = scale
            if bias is not None:
                kw["bias"] = bias
            f = func if func is not None else AF.Copy
            return P.op("act", lambda e: e.activation(out_ap, in_ap, f, **kw), reads=reads, pw=pw, writes=writes)
        if scale is not None:
            return P.op("dve", lambda e: e.tensor_scalar(out_ap, in_ap, scale, None, ALU.mult), reads=reads, pw=pw, writes=writes)
        return P.op("dve", lambda e: e.tensor_copy(out_ap, in_ap), reads=reads, pw=pw, writes=writes)

    def gate_acc(pm, tt):
        if first_g[tt]:
            first_g[tt] = False
            P.op("dve", lambda e: e.tensor_tensor(G[:, tt, :], pm[:, 0:16], bif[:], ALU.add), reads=[pm, bif], pw=[G])
        else:
            P.op("dve", lambda e: e.tensor_tensor(G[:, tt, :], pm[:, 0:16], G[:, tt, :], ALU.add), reads=[pm], pw=[G])

    G.begin()
    import os
    stop = os.environ.get("MK_STOP", "")
    for hd in range(4):
        if stop.startswith("S") and hd > 0:
            return
        self.load_w(wq, self.m_w_q[j, hd], 4, DH, stage)
        self.load_w(wk, self.m_w_k[j, hd], 4, DH, stage)
        wxm = wb_pool.next()
        self.load_w(wxm, self.m_w_in[j, :, :, hd * 512:(hd + 1) * 512], 8, 512, stage)
        xcv.begin()
        if stop == "S0":
            return
        for cc in range(4):
            c = hd * 4 + cc
            xmpad.begin()
            vTc.begin()
            for (pos, w) in ttiles:
                pm = pb.next()

                def mm(e, pm=pm, cc=cc, pos=pos, w=w, wxm=wxm):
                    for kc in range(8):
                        ins = e.matmul(pm[:, 0:w], wxm[:, kc, cc * 128:(cc + 1) * 128], hT[:, kc, pos:pos + w],
                                       start=(kc == 0), stop=(kc == 7))
                    return ins
                P.op("pe", mm, reads=[wxm, hT], writes=[pm])
                if pos == 0:
                    ov = xmpad.ap[:, 2:258]
                    iv = pm[:, 0:256]
                else:
                    r0 = (pos - CTX) // 64
                    ov = xl[:, r0 + 1:r0 + 9, 2:66]
                    iv = pm.ap.rearrange("p (r c) -> p r c", c=64)
                P.op("act", lambda e, ov=ov, iv=iv: e.activation(ov, iv, AF.Copy), reads=[pm], pw=[xmpad])
                P.op("dve", lambda e, pm=pm, pos=pos, w=w: e.tensor_copy(vTc[:, pos:pos + w], pm[:, 0:w]), reads=[pm], pw=[vTc])
            if stop == "S1":
                return
            for tt in range(NTT):
                pm = pb.next()
                P.op("pe", lambda e, pm=pm, tt=tt, c=c: e.matmul(pm[:, 0:16], vTc[:, tt * 128:(tt + 1) * 128], wif[:, 2, c, :],
                                                                start=True, stop=True), reads=[vTc, wif], writes=[pm])
                gate_acc(pm, tt)
            if stop == "S2":
                return
            dg = dg_pool.next()

            def mkdg(e, dg=dg, c=c):
                for tap in range(9):
                    ins = e.tensor_scalar(dg[:, tap, :], self.ident[:], cw[:, c, tap:tap + 1], None, ALU.mult)
                return ins
            P.op("dve", mkdg, reads=[self.ident, cw], writes=[dg])
            for (pos, w) in ttiles:
                pm = pb.next()
                if pos == 0:
                    def mmc(e, pm=pm, dg=dg):
                        for k, dc in enumerate((-1, 0, 1)):
                            ins = e.matmul(pm[:, 0:256], dg[:, 3 + k, :], xmpad.ap[:, 2 + dc:258 + dc],
                                           start=(k == 0), stop=(k == 2))
                        return ins
                else:
                    r0 = (pos - CTX) // 64

                    def mmc(e, pm=pm, dg=dg, r0=r0):
                        n = 0
                        for dr in (-1, 0, 1):
                            for dc in (-1, 0, 1):
                                ins = e.matmul(pm[:, :], dg[:, 3 * (dr + 1) + (dc + 1), :],
                                               xl[:, r0 + 1 + dr:r0 + 9 + dr, 2 + dc:66 + dc],
                                               start=(n == 0), stop=(n == 8))
                                n += 1
                        return ins
                P.op("pe", mmc, reads=[dg, xmpad], writes=[pm])
                P.op("act", lambda e, pm=pm, cc=cc, pos=pos, w=w, c=c: e.activation(
                    xcv[:, cc, pos:pos + w], pm[:, 0:w], AF.Silu, bias=vecs[:, 0, c:c + 1]), reads=[pm, vecs], pw=[xcv])
                if stop == "S3":
                    continue
                tl = tl_pool.next()
                P.op("dve", lambda e, tl=tl, cc=cc, pos=pos, w=w, c=c: e.tensor_scalar(
                    tl[:, 0:w], xcv[:, cc, pos:pos + w], vecs[:, 2, c:c + 1], None, ALU.mult), reads=[xcv, vecs], writes=[tl])
                tt0 = pos // 128
                nt = w // 128
                P.dma("sp", S["OZX"][2, tt0:tt0 + nt, :, c, :].rearrange("t p k -> p t k"),
                      tl.ap[:, 0:w].rearrange("p (t k) -> p t k", k=128), reads=[tl],
                      writes=[self.dbuf(("OZX", j, 2, c, pos))])
        if stop in ("S3", "S4"):
            return
        for tt in range(NTT):
            pm = pb.next()

            def mmv(e, pm=pm, tt=tt, wxm=wxm):
                for kc in range(8):
                    ins = e.matmul(pm[:, :], hT[:, kc, tt * 128:(tt + 1) * 128], wxm[:, kc, :],
                                   start=(kc == 0), stop=(kc == 7))
                return ins
            P.op("pe", mmv, reads=[wxm, hT], writes=[pm])
            tl = tl_pool.next()
            evac(tl[:, :], pm[:, :], [pm], writes=[tl])
            P.dma("sp", S["Vt"][tt * 128:(tt + 1) * 128, hd * 512:(hd + 1) * 512], tl[:, :], reads=[tl],
                  writes=[self.dbuf(("Vt", j, tt, hd))])
        for kind in range(2):
            wz = wb_pool.next()
            col0 = E * (kind + 1) + hd * 512
            self.load_w(wz, self.m_w_in[j, :, :, col0:col0 + 512], 8, 512, stage)
            for cc in range(4):
                c = hd * 4 + cc
                for (pos, w) in ttiles:
                    pm = pb.next()

                    def mm(e, pm=pm, cc=cc, pos=pos, w=w, wz=wz):
                        for kc in range(8):
                            ins = e.matmul(pm[:, 0:w], wz[:, kc, cc * 128:(cc + 1) * 128], hT[:, kc, pos:pos + w],
                                           start=(kc == 0), stop=(kc == 7))
                        return ins
                    P.op("pe", mm, reads=[wz, hT], writes=[pm])
                    tl = tl_pool.next()
                    if kind == 0:
                        t32 = t32_pool.next()
                        P.op("act", lambda e, t32=t32, pm=pm, w=w: e.activation(t32[:, 0:w], pm[:, 0:w], AF.Sigmoid),
                             reads=[pm], writes=[t32])
                        P.op("dve", lambda e, tl=tl, t32=t32, w=w, c=c: e.tensor_scalar(
                            tl[:, 0:w], t32[:, 0:w], vecs[:, 1, c:c + 1], None, ALU.mult), reads=[t32, vecs], writes=[tl])
                    else:
                        P.op("act", lambda e, tl=tl, pm=pm, w=w: e.activation(tl[:, 0:w], pm[:, 0:w], AF.Silu),
                             reads=[pm], writes=[tl])
                    tt0 = pos // 128
                    nt = w // 128
                    P.dma("sp", S["OZX"][kind, tt0:tt0 + nt, :, c, :].rearrange("t p k -> p t k"),
                          tl.ap[:, 0:w].rearrange("p (t k) -> p t k", k=128), reads=[tl],
                          writes=[self.dbuf(("OZX", j, kind, c, pos))])
        for (pos, w) in ttiles:
            tt0 = pos // 128
            nt = w // 128
            qk = []
            for which, wmat, scl in ((0, wq, None), (1, wk, DH ** -0.5)):
                qt = qt_pool.next().begin()
                for ec in range(4):
                    pm = pb.next()

                    def mmq(e, pm=pm, ec=ec, pos=pos, w=w, wmat=wmat):
                        for dc in range(4):
                            ins = e.matmul(pm[:, 0:w], wmat[:, dc, ec * 128:(ec + 1) * 128], xcv[:, dc, pos:pos + w],
                                           start=(dc == 0), stop=(dc == 3))
                        return ins
                    P.op("pe", mmq, reads=[wmat, xcv], writes=[pm])
                    evac(qt[:, ec, 0:w], pm[:, 0:w], [pm], pw=[qt], scale=scl)
                dst = S["QT"] if which == 0 else S["KT"]
                for ec in range(4):
                    P.dma("sp", dst[tt0:tt0 + nt, :, hd, ec, :].rearrange("t p k -> p t k"),
                          qt.ap[:, ec, 0:w].rearrange("p (t k) -> p t k", k=128), reads=[qt],
                          writes=[self.dbuf(("QK", j, which, hd, ec, pos))])
                qk.append(qt)
            qt, kt = qk
            for sub in range(nt):
                tt = tt0 + sub
                sl = slice(sub * 128, (sub + 1) * 128)
                pm = pb.next()

                def mmk(e, pm=pm, sl=sl, pos=pos):
                    for dc in range(4):
                        ins = e.matmul(pm[:, :], xcv[:, dc, pos + sl.start:pos + sl.stop], wk[:, dc, :],
                                       start=(dc == 0), stop=(dc == 3))
                    return ins
                P.op("pe", mmk, reads=[wk, xcv], writes=[pm])
                tl = tl_pool.next()
                evac(tl[:, :], pm[:, :], [pm], writes=[tl], scale=DH ** -0.5)
                P.dma("sp", S["Kt"][tt * 128:(tt + 1) * 128, hd * 512:(hd + 1) * 512], tl[:, :], reads=[tl],
                      writes=[self.dbuf(("Kt", j, tt, hd))])
                pg = pb.next()

                def mmg(e, pg=pg, sl=sl, qt=qt, kt=kt, hd=hd):
                    n = 0
                    for src, buf in ((0, qt), (1, kt)):
                        for ec in range(4):
                            ins = e.matmul(pg[:, 0:16], buf[:, ec, sl], wif[:, src, hd * 4 + ec, :],
                                           start=(n == 0), stop=(n == 7))
                            n += 1
                    return ins
                P.op("pe", mmg, reads=[qt, kt, wif], writes=[pg])
                gate_acc(pg, tt)
    self.barrier()
    A.release(self.m_hT)
    import os
    self.stop = os.environ.get("MK_STOP", "")
    if self.stop == "B":
        return
    self.mlstm_scan(li, j, S, G, x_src, c_src, upd_ctx, pb)


KB.mlstm_layer = mlstm_layer


def mlstm_scan(self, li, j, S, G, x_src, c_src, upd_ctx, pb):
    P, A = self.P, self.A
    N, T = self.N, self.T
    NTT = T // 128
    NG = NTT * 4
    tri = A.alloc("tri", [3, 128], F32)
    mask = A.alloc("mask", [2, 128], F32)
    P.dma("sp", tri[:], self.c_tri[:, :, :], writes=[tri])
    P.dma("sp", mask[:], self.c_mask[:, :, :], writes=[mask])
    SP = A.alloc("SP", [2, NTT, 4], F32)
    TA = A.alloc("TA", [2, NTT, 4], F32)
    AA = A.alloc("AA", [2, NTT, 4], F32)
    WW = A.alloc("WW", [2, NTT, 4], F32)
    ENB = A.alloc("ENB", [2, NTT, 4], F32)
    EBL = A.alloc("EBL", [2, NTT, 4], F32)
    Gv = G.ap.rearrange("p t (r g) -> p r t g", r=2)
    for b_ in (SP, TA, AA, WW, ENB, EBL):
        b_.begin()
    for r in range(2):
        fpre = Gv[:, r, :, 4:8]
        liv = Gv[:, r, :, 0:4]
        P.op("act", lambda e, r=r, fpre=fpre: e.activation(SP[:, r, :, :], fpre, AF.Exp, scale=-1.0), reads=[G], pw=[SP])
        P.op("act", lambda e, r=r: e.activation(SP[:, r, :, :], SP[:, r, :, :], AF.Ln, bias=1.0), reads=[SP], pw=[SP])
        pbm = pb.next()
        pbl = pb.next()
        spf = SP[:, r, :, :].rearrange("p t h -> p (t h)")
        P.op("pe", lambda e, pbm=pbm, spf=spf, r=r: e.matmul(pbm[:, 0:NG], tri[:, r, :], spf, start=True, stop=True),
             reads=[tri, SP], writes=[pbm])
        P.op("pe", lambda e, pbl=pbl, spf=spf: e.matmul(pbl[:, 0:NG], tri[:, 2, :], spf, start=True, stop=True),
             reads=[tri, SP], writes=[pbl])
        bv = pbm[:, 0:NG].rearrange("p (t h) -> p t h", h=4)
        blv = pbl[:, 0:NG].rearrange("p (t h) -> p t h", h=4)
        P.op("dve", lambda e, r=r, liv=liv, bv=bv: e.tensor_tensor(TA[:, r, :, :], liv, bv, ALU.subtract), reads=[G, pbm], pw=[TA])
        P.op("act", lambda e, r=r: e.activation(AA[:, r, :, :], TA[:, r, :, :], AF.Exp), reads=[TA], pw=[AA])
        P.op("dve", lambda e, r=r, blv=blv: e.tensor_tensor(TA[:, r, :, :], TA[:, r, :, :], blv, ALU.add), reads=[pbl, AA], pw=[TA])
        P.op("act", lambda e, r=r: e.activation(WW[:, r, :, :], TA[:, r, :, :], AF.Exp), reads=[TA], pw=[WW])
        P.op("act", lambda e, r=r, bv=bv: e.activation(ENB[:, r, :, :], bv, AF.Exp, scale=-1.0), reads=[pbm], pw=[ENB])
        P.op("act", lambda e, r=r, blv=blv: e.activation(EBL[:, r, :, :], blv, AF.Exp), reads=[pbl], pw=[EBL])
    if "dbgG" in self.dbg:
        dG = self.outp("dbgG", [128, NTT, 16])
        P.dma("sp", dG[:, :, :], G[:], reads=[G])
        for nm, bf_ in (("dbgAA", AA), ("dbgWW", WW), ("dbgENB", ENB), ("dbgEBL", EBL), ("dbgSP", SP)):
            dd = self.outp(nm, [128, 2, NTT, 4])
            P.dma("sp", dd[:, :, :, :], bf_[:], reads=[bf_])
    if self.stop == "G":
        return
    mS = A.mark()
    keyx = "xs" if x_src is self.xs else "xin"
    keyc = "xc" if c_src is self.xcs else "cin"
    for r in range(2):
        A.release(mS)
        C32 = A.alloc("C32", [4, 4, 513], F32)
        Cbf = A.alloc("Cbf", [4, 4, 514], BF16)
        P.op("pool", lambda e: e.memset(C32[:], 0.0))
        tk0 = P.op("pool", lambda e: e.memset(Cbf[:], 0.0))
        nld = 3 if r == 0 else 2
        q_pool = Pool([A.alloc("qc%d" % i, [16, 128], BF16) for i in range(nld)])
        k_pool = Pool([A.alloc("kc%d" % i, [16, 128], BF16) for i in range(nld)])
        K_pool = Pool([A.alloc("Kc%d" % i, [E], BF16) for i in range(nld)])
        V_pool = Pool([A.alloc("Vc%d" % i, [E], BF16) for i in range(nld)])
        WT_pool = Pool([A.alloc("WT%d" % i, [128], BF16) for i in range(2)])
        Va_pool = Pool([A.alloc("Va%d" % i, [514], BF16) for i in range(2)])
        Vw_pool = Pool([A.alloc("Vw%d" % i, [514], BF16) for i in range(2)])
        rr_pool = Pool([A.alloc("rr%d" % i, [2], F32) for i in range(4)])
        hF_pool = Pool([A.alloc("hF%d" % i, [4, 512], BF16) for i in range(2 if r == 0 else 1)])
        if r == 1:
            h32 = A.alloc("h32", [4, 512], F32)
            hn = A.alloc("hn", [4, 512], BF16)
            stats = A.alloc("stats", [4, 6], F32)
            mv = A.alloc("mv", [4, 2], F32)
            rs = A.alloc("rs", [4], F32)
            t1 = A.alloc("t1", [16, 128], F32)
            yT = A.alloc("yTm", [16, 128], BF16)
            ozx = [A.alloc("ozx%d" % i, [16, 128], BF16) for i in range(3)]
            wout = A.alloc("mwout", [16, D], BF16)
            stage = Pool([A.alloc("sstg%d" % i, [512], F32) for i in range(2)])
            self.load_w(wout, self.m_w_out[j], 16, D, stage, engs=("pool",))
            xt_pool = Pool([A.alloc("mx%d" % i, [D], F32) for i in range(2)])
            xn_pool = Pool([A.alloc("mxn%d" % i, [D], F32) for i in range(1)])
        order = [0, 1] + list(range(2, NTT)) if r == 0 else [1, 0] + list(range(NTT - 1, 1, -1))
        ctok = {hd: tk0 for hd in range(4)}
        CH = {}

        def chunk_loads(tt):
            is_ctx = tt < 2
            emit = (not is_ctx) or upd_ctx
            ch = {"emit": emit, "is_ctx": is_ctx}
            Kc = K_pool.next()
            Vc = V_pool.next()
            P.dma("sp", Kc[:], S["Kt"][tt * 128:(tt + 1) * 128, :], writes=[Kc])
            P.dma("sp", Vc[:], S["Vt"][tt * 128:(tt + 1) * 128, :], writes=[Vc])
            ch["Kc"], ch["Vc"] = Kc, Vc
            if emit:
                qc = q_pool.next()
                kc_ = k_pool.next()
                P.dma("sp", qc[:], S["QT"][tt].rearrange("p h e k -> p (h e) k"), writes=[qc])
                P.dma("sp", kc_[:], S["KT"][tt].rearrange("p h e k -> p (h e) k"), writes=[kc_])
                ch["qc"], ch["kc"] = qc, kc_
            CH[tt] = ch

        def stage_A(tt, hd, r=r):
            if hd == 0:
                chunk_loads(tt)
            ch = CH[tt]
            Vc = ch["Vc"]
            st = {}
            a_s = AA[:, r, tt, hd:hd + 1]
            w_s = WW[:, r, tt, hd:hd + 1]
            Vw = Vw_pool.next().begin()
            st["Vw"] = Vw
            st["small"] = small = pb.next()
            P.op("dve", lambda e: e.tensor_scalar(Vw[:, 0:512], Vc[:, hd * 512:(hd + 1) * 512], w_s, None, ALU.mult),
                 reads=[Vc, WW], pw=[Vw])
            P.op("pool", lambda e: e.tensor_copy(Vw[:, 512:513], w_s), reads=[WW], pw=[Vw])
            if ch["emit"]:
                qc, kc_ = ch["qc"], ch["kc"]
                Va = Va_pool.next().begin()
                st["Va"] = Va
                P.op("act", lambda e: e.activation(Va[:, 0:512], Vc[:, hd * 512:(hd + 1) * 512], AF.Copy, scale=a_s),
                     reads=[Vc, AA], pw=[Va])
                P.op("act", lambda e: e.activation(Va[:, 512:513], a_s, AF.Copy), reads=[AA], pw=[Va])

                def mms(e):
                    for ec in range(4):
                        ins = e.matmul(small[:, 0:128], kc_[:, hd * 4 + ec, :], qc[:, hd * 4 + ec, :],
                                       start=(ec == 0), stop=(ec == 3))
                    return ins
                P.op("pe", mms, reads=[qc, kc_], writes=[small])
                WT = WT_pool.next()
                st["WT"] = WT
                P.op("dve", lambda e: e.tensor_tensor(WT[:], small[:, 0:128], mask[:, r, :], ALU.mult),
                     reads=[small, mask], writes=[WT])
            return st

        def stage_B(tt, hd, st, r=r):
            ch = CH[tt]
            st["tok_mmn"] = None
            if not ch["emit"]:
                return
            if hd == 0:
                hF = hF_pool.next()
                ch["hF"] = hF
                if r == 1:
                    P.dma("sp", hF[:], S["HF"][tt * 128:(tt + 1) * 128, :].rearrange("p (h v) -> p h v", h=4), writes=[hF])
                    h32.begin()
                else:
                    hF.begin()
            hF = ch["hF"]
            qc = ch["qc"]
            small, WT, Va = st["small"], st["WT"], st["Va"]
            pd = small.ap[:, 128:132]
            pn = pb.next()

            def mmn(e):
                e.matmul(pn[:, :], WT[:], Va[:, 0:512], start=True, stop=False)
                for kc in range(4):
                    e.matmul(pn[:, :], qc[:, hd * 4 + kc, :], Cbf[:, hd, kc, 0:512], start=False, stop=(kc == 3))
                e.matmul(pd[:, 0:1], WT[:], Va[:, 512:513], start=True, stop=False)
                for kc in range(4):
                    ins = e.matmul(pd[:, 0:1], qc[:, hd * 4 + kc, :], Cbf[:, hd, kc, 512:513], start=False, stop=(kc == 3))
                return ins
            st["tok_mmn"] = P.op("pe", mmn, reads=[WT, Va, qc], writes=[pn, small], extra=[ctok.get(hd)])
            rr = rr_pool.next()
            P.op("act", lambda e: e.activation(rr[:, 0:1], pd[:, 0:1], AF.Abs), reads=[small], writes=[rr])
            P.op("dve", lambda e: e.tensor_scalar(rr[:, 0:1], rr[:, 0:1], ENB[:, r, tt, hd:hd + 1], None, ALU.max),
                 reads=[rr, ENB], writes=[rr])
            P.op("dve", lambda e: e.reciprocal(rr[:, 1:2], rr[:, 0:1]), reads=[rr], writes=[rr])
            if r == 0:
                P.op("act", lambda e: e.activation(hF[:, hd, :], pn[:, :], AF.Copy, scale=rr[:, 1:2]),
                     reads=[pn, rr], pw=[hF])
            else:
                P.op("dve", lambda e: e.scalar_tensor_tensor(h32[:, hd, :], pn[:, :], rr[:, 1:2], hF[:, hd, :], ALU.mult, ALU.add),
                     reads=[pn, rr, hF], pw=[h32])

        def stage_C(tt, hd, st, r=r):
            ch = CH[tt]
            Kc = ch["Kc"]
            small, Vw = st["small"], st["Vw"]
            psn = small.ap[:, 132:136]
            if tt != order[-1]:
                ebl = EBL[:, r, tt, hd:hd + 1]
                toks = []
                for kc in range(4):
                    pk = pb.next()
                    P.op("pe", lambda e, pk=pk, kc=kc: e.matmul(pk[:, :], Kc[:, hd * 512 + kc * 128:hd * 512 + (kc + 1) * 128],
                                                                Vw[:, 0:512], start=True, stop=True), reads=[Kc, Vw], writes=[pk])
                    tk = P.op("dve", lambda e, pk=pk, kc=kc: e.scalar_tensor_tensor(
                        C32[:, hd, kc, 0:512], C32[:, hd, kc, 0:512], ebl, pk[:, :], ALU.mult, ALU.add),
                        reads=[pk, EBL], extra=[ctok.get(hd)])
                    toks.append(tk)

                def mmsn(e):
                    for kc in range(4):
                        ins = e.matmul(psn[:, kc:kc + 1], Kc[:, hd * 512 + kc * 128:hd * 512 + (kc + 1) * 128], Vw[:, 512:513],
                                       start=True, stop=True)
                    return ins
                P.op("pe", mmsn, reads=[Kc, Vw], writes=[small])
                tk = P.op("dve", lambda e: e.scalar_tensor_tensor(
                    C32[:, hd, :, 512], C32[:, hd, :, 512], ebl, psn[:, 0:4], ALU.mult, ALU.add),
                    reads=[small, EBL], extra=[ctok.get(hd)])
                toks.append(tk)
                ctok[hd] = P.op("act", lambda e: e.activation(Cbf[:, hd, :, 0:513], C32[:, hd, :, :], AF.Copy),
                                extra=toks + [ctok.get(hd), st["tok_mmn"]])
            if hd == 0:
                run_pend(2)
            if hd == 2:
                run_pend(3)
            if hd == 3:
                run_pend(2)
                run_pend(3)
                chunk_end(tt)

        def chunk_end(tt, r=r):
            ch = CH[tt]
            if not ch["emit"]:
                return
            hF = ch["hF"]
            is_ctx = ch["is_ctx"]
            if r == 0:
                P.dma("sp", S["HF"][tt * 128:(tt + 1) * 128, :].rearrange("p (h v) -> p h v", h=4), hF[:], reads=[hF],
                      writes=[self.dbuf(("HF", j, tt))])
                return
            for i in range(3):
                P.dma("sp", ozx[i][:], S["OZX"][i, tt], writes=[ozx[i]])
            P.op("dve", lambda e: [e.bn_stats(stats[:, hd, :], h32[:, hd, :]) for hd in range(4)][-1], reads=[h32], writes=[stats])
            P.op("dve", lambda e: [e.bn_aggr(mv[:, hd, :], stats[:, hd, :]) for hd in range(4)][-1], reads=[stats], writes=[mv])
            P.op("act", lambda e: e.activation(rs[:], mv[:, :, 1], AF.Sqrt, bias=EPS), reads=[mv], writes=[rs])
            P.op("dve", lambda e: e.reciprocal(rs[:], rs[:]), reads=[rs], writes=[rs])
            hn.begin()
            for hd in range(4):
                P.op("dve", lambda e, hd=hd: e.tensor_scalar(hn[:, hd, :], h32[:, hd, :], mv[:, hd, 0:1], rs[:, hd:hd + 1],
                                                             ALU.subtract, ALU.mult), reads=[h32, mv, rs], pw=[hn])

            def part2():
                t1.begin()
                for half in range(2):
                    ptb = pb.next()
                    ptv = ptb.ap.bitcast(BF16)

                    def tr(e, ptv=ptv, half=half):
                        for c8 in range(8):
                            c = half * 8 + c8
                            ins = e.transpose(ptv[:, c8 * 128:(c8 + 1) * 128], hn[:, c // 4, (c % 4) * 128:(c % 4 + 1) * 128], self.ident[:])
                        return ins
                    P.op("pe", tr, reads=[hn, self.ident], writes=[ptb])
                    pv = ptv[:, 0:1024].rearrange("p (c t) -> p c t", c=8)
                    P.op("dve", lambda e, pv=pv, half=half: e.tensor_tensor(t1[:, half * 8:(half + 1) * 8, :], pv,
                                                                             ozx[0][:, half * 8:(half + 1) * 8, :], ALU.mult),
                         reads=[ptb, ozx[0]], pw=[t1])
                P.op("pool", lambda e: e.tensor_tensor(t1[:], t1[:], ozx[2][:], ALU.add), reads=[ozx[2]], writes=[t1])
                P.op("pool", lambda e: e.tensor_tensor(yT[:], t1[:], ozx[1][:], ALU.mult), reads=[t1, ozx[1]], writes=[yT])

            def part3():
                if is_ctx:
                    self.out_stage(yT, 0, "ctx", tt, False, self.gc_bc, c_src, keyc, "xc", self.xcs, wout, xt_pool, xn_pool, pb)
                else:
                    self.out_stage(yT, 0, "lat", tt - 2, False, self.g_bc, x_src, keyx, "xs", self.xs, wout, xt_pool, xn_pool, pb)
            pend[2] = part2
            pend[3] = part3

        pend = {2: None, 3: None}

        def run_pend(k):
            f = pend[k]
            if f is not None:
                pend[k] = None
                f()

        items = [(tt, hd) for tt in order for hd in range(4)]
        prev = None
        for it in items:
            st = stage_A(*it)
            if prev is not None:
                stage_B(prev[0], prev[1], prev[2])
                stage_C(prev[0], prev[1], prev[2])
            prev = (it[0], it[1], st)
        stage_B(prev[0], prev[1], prev[2])
        stage_C(prev[0], prev[1], prev[2])
        run_pend(2)
        run_pend(3)
        self.barrier()
        if self.stop == "P1":
            return


KB.mlstm_scan = mlstm_scan


_CACHE = {}
LAUNCH_GROUPS = ((0, 1, 2, 3),)


def _get_prog(N, layers, final):
    key = (N, layers, final)
    if key not in _CACHE:
        kb = KB(N=N, layers=layers, final=final, dbg=("xs", "xcs"))
        nc = kb.build()
        _CACHE[key] = (kb, nc)
    return _CACHE[key]


def kernel(**inputs):
    x = np.asarray(inputs["x"], dtype=np.float32)
    B, N, _ = x.shape
    sh = host_shared(inputs)
    cores = [host_core(inputs, b) for b in range(B)]
    out = None
    for gi, layers in enumerate(LAUNCH_GROUPS):
        final = (3 in layers)
        kb, nc = _get_prog(N, tuple(layers), final)
        in_maps = []
        for b in range(B):
            m = dict(sh)
            m.update(cores[b])
            in_maps.append({k: v for k, v in m.items() if k in kb.din})
        res = run_bass_kernel_spmd(nc, in_maps, core_ids=list(range(B)))
        for b in range(B):
            r = res.results[b]
            if final:
                continue
            cores[b]["x"] = np.asarray(r["xs"], dtype=np.float32)
            if any(l < 2 for l in layers):
                cores[b]["ctx"] = np.asarray(r["xcs"], dtype=np.float32)
        if final:
            out = np.stack([np.asarray(res.results[b]["out"], dtype=np.float32) for b in range(B)], axis=0)
    return out
```

```python
import math
from contextlib import ExitStack

import numpy as np
import ml_dtypes
import concourse.bass as bass
import concourse.mybir as mybir
from concourse.bass_utils import run_bass_kernel_spmd

F32 = mybir.dt.float32
BF16 = mybir.dt.bfloat16
AF = mybir.ActivationFunctionType
ALU = mybir.AluOpType
AX = mybir.AxisListType

D = 1024
E = 2048
NH = 4
DH = 512
CTX = 256
GW = 64
EPS = 1e-6
NDS = 12


class Buf:
    def __init__(self, ap, name=""):
        self.ap = ap
        self.name = name
        self.ws = []
        self.r = []
        self.prev = []
        self.excl = False

    def begin(self):
        self.prev = self.ws + self.r
        self.ws = []
        self.r = []
        return self

    def __getitem__(self, k):
        return self.ap[k]


class Prog:
    CE = ("pe", "dve", "act", "pool")
    DQ = ("sp", "act", "pool")

    def __init__(self, nc, es):
        self.nc = nc
        self.es = es
        self.q = {e: [] for e in ("pe", "dve", "act", "pool", "sp")}
        self.sems = {}
        self.cnt = {}
        for e in self.CE:
            self.sems[e] = es.enter_context(nc.semaphore("s_" + e))
            self.cnt[e] = 0
        self.dq_i = {}
        for qn in self.DQ:
            self.dq_i[qn] = 0
            for i in range(NDS):
                k = ("d", qn, i)
                self.sems[k] = es.enter_context(nc.semaphore("d_%s%d" % (qn, i)))
                self.cnt[k] = 0
        self.waited = {e: {} for e in self.q}
        self.n_ops = 0

    def _needed(self, eng, toks):
        out = []
        w = self.waited[eng]
        best = {}
        for t in toks:
            if t is None:
                continue
            k, v = t
            if k == eng and eng == "pe":
                continue
            if w.get(k, 0) >= v:
                continue
            if best.get(k, 0) < v:
                best[k] = v
        for k, v in best.items():
            w[k] = v
            out.append((k, v))
        return out

    def op(self, eng, fn, reads=(), writes=(), extra=(), pw=()):
        toks = list(extra)
        for b in pw:
            toks.extend(b.prev)
        for b in reads:
            toks.extend(b.ws)
            if b.excl:
                toks.extend(t for t in b.r if t[0] != eng)
        for b in writes:
            toks.extend(b.ws)
            toks.extend(b.r)
        waits = self._needed(eng, toks)
        self.cnt[eng] += 1
        tok = (eng, self.cnt[eng])
        self.q[eng].append((waits, fn, eng, 1))
        for b in reads:
            b.r.append(tok)
        for b in writes:
            b.ws = [tok]
            b.r = []
        for b in pw:
            b.ws.append(tok)
        self.n_ops += 1
        return tok

    def dma(self, qn, out_ap, in_ap, reads=(), writes=(), extra=(), also=(), **kw):
        i = self.dq_i[qn] % NDS
        self.dq_i[qn] += 1
        k = ("d", qn, i)
        toks = list(extra)
        if self.cnt[k] > 0:
            toks.append((k, self.cnt[k]))
        for b in reads:
            toks.extend(b.ws)
        for b in writes:
            toks.extend(b.ws)
            toks.extend(b.r)
        waits = self._needed(qn, toks)
        self.cnt[k] += 16
        tok = (k, self.cnt[k])

        def fn(e, out_ap=out_ap, in_ap=in_ap, kw=kw):
            return e.dma_start(out=out_ap, in_=in_ap, **kw)

        self.q[qn].append((waits, fn, k, 16))
        for b in reads:
            b.r.append(tok)
        for b in writes:
            b.ws = [tok]
            b.r = []
        for b in also:
            b.ws.append(tok)
        return tok

    def wait_all(self, eng, toks):
        waits = self._needed(eng, toks)
        self.q[eng].append((waits, None, None, 0))

    def cut(self):
        for name in self.q:
            self.q[name].append("CUT")

    def replay(self):
        nc = self.nc
        sems = self.sems
        segs = {}
        nseg = 0
        for name, items in self.q.items():
            cur = []
            lst = [cur]
            for it in items:
                if it == "CUT":
                    cur = []
                    lst.append(cur)
                else:
                    cur.append(it)
            segs[name] = lst
            nseg = max(nseg, len(lst))
        for si in range(nseg):
            if not any(len(segs[n][si]) for n in segs if si < len(segs[n])):
                continue
            with nc.Block() as block:
                def run(eng, name, si=si):
                    for waits, fn, sk, inc in segs[name][si]:
                        for k, v in waits:
                            eng.wait_ge(sems[k], v)
                        if fn is not None:
                            ins = fn(eng)
                            ins.then_inc(sems[sk], inc)

                @block.tensor
                def _(e):
                    run(e, "pe")

                @block.vector
                def _(e):
                    run(e, "dve")

                @block.scalar
                def _(e):
                    run(e, "act")

                @block.gpsimd
                def _(e):
                    run(e, "pool")

                @block.sync
                def _(e):
                    run(e, "sp")


class Pool:
    def __init__(self, bufs):
        self.bufs = bufs
        self.i = 0

    def next(self):
        b = self.bufs[self.i % len(self.bufs)]
        self.i += 1
        return b


class Arena:
    def __init__(self, ap):
        self.ap = ap
        self.top = 0
        self.W = ap.shape[1]
        self.peak = 0

    def alloc(self, name, free_shape, dt):
        n = 1
        for s in free_shape:
            n *= s
        esz = 4 if dt == F32 else 2
        words = (n * esz + 3) // 4
        words = (words + 7) // 8 * 8
        a = self.top
        self.top += words
        self.peak = max(self.peak, self.top)
        assert self.top <= self.W, "SBUF arena overflow at %s: %d > %d" % (name, self.top, self.W)
        v = self.ap[:, a:a + words]
        if dt != F32:
            v = v.bitcast(dt)
        v = v[:, 0:n]
        if len(free_shape) == 2:
            v = v.rearrange("p (a b) -> p a b", a=free_shape[0])
        elif len(free_shape) == 3:
            v = v.rearrange("p (a b c) -> p a b c", a=free_shape[0], b=free_shape[1])
        elif len(free_shape) == 4:
            v = v.rearrange("p (a b c d) -> p a b c d", a=free_shape[0], b=free_shape[1], c=free_shape[2])
        return Buf(v, name)

    def mark(self):
        return self.top

    def release(self, m):
        self.top = m


class K:
    pass


def fourier_tile_rows(dr, t):
    v = dr.rearrange("(n1 n2) d -> n2 n1 d", n2=GW)
    return [(0, 64, v[2 * t]), (64, 128, v[2 * t + 1])]


def natural_tile_rows(dr, t):
    return [(0, 128, dr[t * 128:(t + 1) * 128, :])]


def _prod(s):
    n = 1
    for v in s:
        n *= v
    return n


class KB:
    def __init__(self, N=4096, layers=(0, 1, 2, 3), final=True, dbg=()):
        self.N = N
        self.T = CTX + N
        self.layers = tuple(layers)
        self.final = final
        self.dbg = tuple(dbg)
        self.nc = bass.Bass("TRN2", target_bir_lowering=False)
        self.din = {}
        self.es = ExitStack()

    def inp(self, name, shape, dt=F32):
        t = self.nc.dram_tensor(name, list(shape), dt, kind="ExternalInput").ap()
        self.din[name] = t
        return t

    def outp(self, name, shape, dt=F32):
        return self.nc.dram_tensor(name, list(shape), dt, kind="ExternalOutput").ap()

    def scratch(self, name, shape, dt):
        kind = "ExternalOutput" if name in self.dbg else "Internal"
        return self.nc.dram_tensor(name, list(shape), dt, kind=kind).ap()

    def dbuf(self, key):
        b = self.dbufs.get(key)
        if b is None:
            b = Buf(None, str(key))
            self.dbufs[key] = b
        return b

    def barrier(self):
        P = self.P
        toks = [(k, v) for k, v in P.cnt.items() if v > 0]
        for e in ("pe", "dve", "act", "pool", "sp"):
            P.wait_all(e, toks)
        P.cut()

    def build(self):
        nc, es = self.nc, self.es
        N, T = self.N, self.T
        self.x_in = self.inp("x", [N, D])
        self.ctx_in = self.inp("ctx", [CTX, D])
        self.cvec = self.inp("cvec", [128, 8, 2])
        self.norm_g = self.inp("norm_g", [4, D])
        self.w_ada = self.inp("w_ada", [4, 128, 8, 3 * D])
        self.b_ada = self.inp("b_ada", [4, 3 * D])
        self.f_w_in = self.inp("f_w_in", [2, 128, 8, 2 * E])
        self.f_w_out = self.inp("f_w_out", [2, 128, 16, D])
        self.norm_f = self.inp("norm_f", [1, D])
        self.identb = self.inp("identb", [128, 128], BF16)
        self.c_cs = self.inp("c_cs", [128, 2, 512], BF16)
        self.c_a = self.inp("c_a", [128, 3, 128], BF16)
        self.c_bk = self.inp("c_bk", [128, 64, 64], BF16)
        self.c_d256 = self.inp("c_d256", [128, 2, 2, 256], BF16)
        self.declare_mlstm_inputs()
        self.out = self.outp("out", [N, D])
        self.xs = self.scratch("xs", [N, D], F32)
        self.xcs = self.scratch("xcs", [CTX, D], F32)
        self.dbufs = {}
        self.scr = {}
        with es:
            self.P = Prog(nc, es)
            sb_all = es.enter_context(nc.sbuf_tensor("sb_all", [128, 50 * 1024], F32))
            self.A = Arena(sb_all)
            self.psum = es.enter_context(nc.psum_tensor("ps_all", [128, 8 * 512], F32))
            self.pbank = [Buf(self.psum[:, b * 512:(b + 1) * 512], "bank%d" % b) for b in range(8)]
            for b_ in self.pbank:
                b_.excl = True
            A = self.A
            self.ident = A.alloc("ident", [128], BF16)
            self.P.dma("sp", self.ident[:], self.identb[:, :], writes=[self.ident])
            self.g_bc = A.alloc("g_bc", [D], F32)
            self.gc_bc = A.alloc("gc_bc", [D], F32)
            x_src, c_src = self.x_in, self.ctx_in
            for li in self.layers:
                upd_ctx = li < 2
                need_ctx = upd_ctx or (li % 2 == 0)
                fourier = (li % 2 == 1)
                mL = A.mark()
                if fourier:
                    self.fTc = A.alloc("fTc", [16, 256], F32)
                else:
                    self.G = A.alloc("G", [T // 128, 16], F32)
                    self.bif = A.alloc("bif", [16], F32)
                self.m_hT = A.mark()
                self.hT = A.alloc("hT", [8, T], BF16)
                m = A.mark()
                self.prep(li, x_src, c_src, need_ctx, fourier)
                self.barrier()
                A.release(m)
                if fourier:
                    self.fourier_layer(li, x_src, c_src, upd_ctx)
                else:
                    self.mlstm_layer(li, x_src, c_src, upd_ctx)
                self.barrier()
                A.release(mL)
                x_src = self.xs
                if upd_ctx:
                    c_src = self.xcs
            if self.final:
                self.final_norm(x_src)
            self.barrier()
            self.P.replay()
        return nc

    def declare_mlstm_inputs(self):
        pass

    def mlstm_layer(self, li, x_src, c_src, upd_ctx):
        raise NotImplementedError

    def tiles(self, fourier, need_ctx):
        out = []
        if need_ctx:
            for t in range(CTX // 128):
                out.append(("ctx", t, t * 128))
        for t in range(self.N // 128):
            out.append(("lat", t, CTX + t * 128))
        return out

    def tile_rows(self, stream, t, fourier, dr):
        if stream == "lat" and fourier:
            return fourier_tile_rows(dr, t)
        return natural_tile_rows(dr, t)

    def load_rows(self, q, buf, stream, t, fourier, dr, key):
        P = self.P
        toks = []
        db = self.dbuf((key, stream, t))
        first = True
        for (p0, p1, ap) in self.tile_rows(stream, t, fourier, dr):
            if first:
                tok = P.dma(q, buf.ap[p0:p1], ap, reads=[db], writes=[buf])
                first = False
            else:
                tok = P.dma(q, buf.ap[p0:p1], ap, reads=[db], also=[buf])
            toks.append(tok)
        return toks

    def store_rows(self, q, buf, stream, t, fourier, dr, key, extra=()):
        P = self.P
        db = self.dbuf((key, stream, t))
        toks = []
        for (p0, p1, ap) in self.tile_rows(stream, t, fourier, dr):
            toks.append(P.dma(q, ap, buf.ap[p0:p1], reads=[buf], writes=[db], extra=extra))
        return toks

    def prep(self, li, x_src, c_src, need_ctx, fourier):
        P, A = self.P, self.A
        cv = A.alloc("cv", [8, 2], F32)
        sv = A.alloc("sv", [8, 2], F32)
        ones = A.alloc("ones", [128], F32)
        sbc = A.alloc("sbc", [2, 8, 128], F32)
        mod = [A.alloc("mod%d" % s, [3 * D], F32) for s in range(2)]
        bbc = A.alloc("bbc", [3 * D], F32)
        gn = A.alloc("gn", [D], F32)
        abc = [A.alloc("abc%d" % s, [D], F32) for s in range(2)]
        wst = Pool([A.alloc("wst%d" % i, [8, 256], F32) for i in range(2)])
        P.dma("sp", cv[:], self.cvec[:, :, :], writes=[cv])
        P.dma("sp", bbc[:], self.b_ada[li].partition_broadcast(128), writes=[bbc])
        P.dma("sp", gn[:], self.norm_g[li].partition_broadcast(128), writes=[gn])
        P.op("act", lambda e: e.activation(sv[:], cv[:], AF.Silu), reads=[cv], writes=[sv])
        P.op("pool", lambda e: e.memset(ones[:], 1.0), writes=[ones])
        nstream = 2 if need_ctx else 1

        def mk_sbc(e):
            for s in range(nstream):
                for kc in range(8):
                    ins = e.tensor_scalar(sbc[:, s, kc, :], ones[:], sv[:, kc, s:s + 1], None, ALU.mult)
            return ins
        P.op("dve", mk_sbc, reads=[ones, sv], writes=[sbc])
        pb = Pool([self.pbank[0], self.pbank[1]])
        for blk in range(12):
            w = wst.next()
            P.dma("sp", w[:], self.w_ada[li, :, :, blk * 256:(blk + 1) * 256], writes=[w])
            for s in range(nstream):
                pm = pb.next()

                def mm(e, s=s, w=w, pm=pm):
                    for kc in range(8):
                        ins = e.matmul(pm[:, 0:256], sbc[:, s, kc, :], w[:, kc, :], start=(kc == 0), stop=(kc == 7))
                    return ins
                P.op("pe", mm, reads=[sbc, w], writes=[pm])
                sl = slice(blk * 256, (blk + 1) * 256)
                P.op("dve", lambda e, s=s, pm=pm, sl=sl: e.tensor_tensor(mod[s][:, sl], pm[:, 0:256], bbc[:, sl], ALU.add),
                     reads=[pm, bbc], writes=[mod[s]])
        for s in range(nstream):
            P.op("dve", lambda e, s=s: e.scalar_tensor_tensor(abc[s][:], mod[s][:, D:2 * D], 1.0, gn[:], ALU.add, ALU.mult),
                 reads=[mod[s], gn], writes=[abc[s]])
        P.op("pool", lambda e: e.tensor_copy(self.g_bc[:], mod[0][:, 2 * D:3 * D]), reads=[mod[0]], writes=[self.g_bc])
        if need_ctx:
            P.op("pool", lambda e: e.tensor_copy(self.gc_bc[:], mod[1][:, 2 * D:3 * D]), reads=[mod[1]], writes=[self.gc_bc])
        xt_pool = Pool([A.alloc("xt%d" % i, [D], F32) for i in range(3)])
        junk = A.alloc("junk", [D], F32)
        t1_pool = Pool([A.alloc("t1_%d" % i, [D], F32) for i in range(2)])
        xn_pool = Pool([A.alloc("xn%d" % i, [D], BF16) for i in range(2)])
        st_pool = Pool([A.alloc("st%d" % i, [2], F32) for i in range(4)])
        pt_pool = Pool([self.pbank[2], self.pbank[3]])
        key = "xc" if c_src is self.xcs else "cin"
        keyx = "xs" if x_src is self.xs else "xin"
        self.hT.begin()
        for ti, (stream, t, pos0) in enumerate(self.tiles(fourier, need_ctx)):
            s = 1 if stream == "ctx" else 0
            dr = c_src if stream == "ctx" else x_src
            xt = xt_pool.next()
            self.load_rows("sp", xt, stream, t, fourier, dr, key if stream == "ctx" else keyx)
            st = st_pool.next()
            P.op("act", lambda e, xt=xt, st=st: e.activation(junk[:], xt[:], AF.Square, accum_out=st[:, 0:1]),
                 reads=[xt], writes=[junk, st])
            P.op("act", lambda e, st=st: e.activation(st[:, 1:2], st[:, 0:1], AF.Sqrt, bias=EPS, scale=1.0 / D),
                 reads=[st], writes=[st])
            P.op("dve", lambda e, st=st: e.reciprocal(st[:, 1:2], st[:, 1:2]), reads=[st], writes=[st])
            t1 = t1_pool.next()
            P.op("dve", lambda e, xt=xt, st=st, t1=t1, s=s: e.scalar_tensor_tensor(
                t1[:], xt[:], st[:, 1:2], abc[s][:], ALU.mult, ALU.mult), reads=[xt, st, abc[s]], writes=[t1])
            xn = xn_pool.next()
            P.op("pool", lambda e, t1=t1, xn=xn, s=s: e.tensor_tensor(xn[:], t1[:], mod[s][:, 0:D], ALU.add),
                 reads=[t1, mod[s]], writes=[xn])
            pt = pt_pool.next()
            ptv = pt.ap.bitcast(BF16)

            def tr(e, xn=xn, ptv=ptv):
                for c in range(8):
                    ins = e.transpose(ptv[:, c * 128:(c + 1) * 128], xn[:, c * 128:(c + 1) * 128], self.ident[:])
                return ins
            P.op("pe", tr, reads=[xn, self.ident], writes=[pt])
            eng = "act" if ti % 2 == 0 else "dve"
            hv = self.hT[:, :, pos0:pos0 + 128]
            pv = ptv[:, 0:1024].rearrange("p (c t) -> p c t", c=8)
            if eng == "act":
                P.op("act", lambda e, hv=hv, pv=pv: e.activation(hv, pv, AF.Copy), reads=[pt], pw=[self.hT])
            else:
                P.op("dve", lambda e, hv=hv, pv=pv: e.tensor_copy(hv, pv), reads=[pt], pw=[self.hT])

    def final_norm(self, x_src):
        P, A = self.P, self.A
        m = A.mark()
        nf = A.alloc("nf", [D], F32)
        P.dma("sp", nf[:], self.norm_f[0].partition_broadcast(128), writes=[nf])
        xt_pool = Pool([A.alloc("fxt%d" % i, [D], F32) for i in range(3)])
        yo_pool = Pool([A.alloc("fyo%d" % i, [D], F32) for i in range(3)])
        junk = A.alloc("fjunk", [D], F32)
        st_pool = Pool([A.alloc("fst%d" % i, [2], F32) for i in range(4)])
        keyx = "xs" if x_src is self.xs else "xin"
        last = []
        for t in range(self.N // 128):
            xt = xt_pool.next()
            self.load_rows("sp", xt, "lat", t, False, x_src, keyx)
            st = st_pool.next()
            P.op("act", lambda e, xt=xt, st=st: e.activation(junk[:], xt[:], AF.Square, accum_out=st[:, 0:1]),
                 reads=[xt], writes=[junk, st])
            P.op("act", lambda e, st=st: e.activation(st[:, 1:2], st[:, 0:1], AF.Sqrt, bias=EPS, scale=1.0 / D),
                 reads=[st], writes=[st])
            P.op("dve", lambda e, st=st: e.reciprocal(st[:, 1:2], st[:, 1:2]), reads=[st], writes=[st])
            yo = yo_pool.next()
            P.op("dve", lambda e, xt=xt, st=st, yo=yo: e.scalar_tensor_tensor(
                yo[:], xt[:], st[:, 1:2], nf[:], ALU.mult, ALU.mult), reads=[xt, st, nf], writes=[yo])
            last += self.store_rows("sp", yo, "lat", t, False, self.out, "out")
        A.release(m)


def _load_w(self, dst_view, src_ap, kc, ncols, stage_pool, engs=("pool",)):
    P = self.P
    piece = max(1, stage_pool.bufs[0].ap.shape[1] // kc)
    c0 = 0
    i = 0
    while c0 < ncols:
        w = min(piece, ncols - c0)
        st = stage_pool.next()
        sv = st.ap[:, 0:kc * w].rearrange("p (k n) -> p k n", k=kc)
        P.dma("sp", sv, src_ap[:, :, c0:c0 + w], writes=[st])
        eng = engs[i % len(engs)]
        dv = dst_view.ap[:, :, c0:c0 + w] if isinstance(dst_view, Buf) else dst_view[:, :, c0:c0 + w]
        if eng == "act":
            P.op("act", lambda e, dv=dv, sv=sv: e.activation(dv, sv, AF.Copy), reads=[st], pw=[self._lw_buf])
        else:
            P.op(eng, lambda e, dv=dv, sv=sv: e.tensor_copy(dv, sv), reads=[st], pw=[self._lw_buf])
        c0 += w
        i += 1


def load_w(self, dst_buf, src_ap, kc, ncols, stage_pool, engs=("pool",), view=None):
    self._lw_buf = dst_buf
    dst_buf.begin()
    _load_w(self, dst_buf if view is None else view, src_ap, kc, ncols, stage_pool, engs)


KB.load_w = load_w


def fourier_layer(self, li, x_src, c_src, upd_ctx):
    P, A = self.P, self.A
    hT = self.hT
    N, T = self.N, self.T
    j = li // 2
    NT = N // 128
    Y = self.scratch("Y%d" % li, [64, 2, 64, E], BF16)
    ZS = self.scratch("ZS%d" % li, [16, 128, T], BF16)
    pb = Pool(self.pbank)
    fTc = self.fTc
    m0 = A.mark()
    cs = A.alloc("cs", [2, 512], BF16)
    ca = A.alloc("ca", [3, 128], BF16)
    d256 = A.alloc("d256", [2, 2, 256], BF16)
    P.dma("sp", cs[:], self.c_cs[:, :, :], writes=[cs])
    P.dma("sp", ca[:], self.c_a[:, :, :], writes=[ca])
    P.dma("sp", d256[:], self.c_d256[:, :, :, :], writes=[d256])
    stage = Pool([A.alloc("wstg%d" % i, [2048], F32) for i in range(2)])
    wb_pool = Pool([A.alloc("wb%d" % i, [8, 512], BF16) for i in range(2)])
    uT = A.alloc("uT", [4, T], BF16)
    UT_pool = Pool([A.alloc("UT%d" % i, [2, 2, 256], BF16) for i in range(3)])
    UTc = A.alloc("UTc", [2, 2, 2, 256], BF16) if upd_ctx else None
    Ysb_pool = Pool([A.alloc("Ysb%d" % i, [2, 512], BF16) for i in range(3)])
    zsb_pool = Pool([A.alloc("zsb%d" % i, [4, 512], BF16) for i in range(2)])
    ttiles = []
    if upd_ctx:
        ttiles.append((0, 256))
    for i in range(N // 512):
        ttiles.append((CTX + i * 512, 512))
    ptiles = []
    if upd_ctx:
        ptiles += [("ctx", 0, 0), ("ctx", 1, 128)]
    ptiles += [("lat", t, CTX + t * 128) for t in range(NT)]
    ev = 0
    if upd_ctx:
        fTc.begin()
    for fb in range(4):
        wb = wb_pool.next()
        self.load_w(wb, self.f_w_in[j, :, :, fb * 512:(fb + 1) * 512], 8, 512, stage)
        uT.begin()
        for (pos, w) in ttiles:
            for cc in range(4):
                pm = pb.next()

                def mm(e, pm=pm, wb=wb, cc=cc, pos=pos, w=w):
                    for kc in range(8):
                        ins = e.matmul(pm[:, 0:w], wb[:, kc, cc * 128:(cc + 1) * 128], hT[:, kc, pos:pos + w],
                                       start=(kc == 0), stop=(kc == 7))
                    return ins
                P.op("pe", mm, reads=[wb, hT], writes=[pm])
                ev += 1
                if ev % 2 == 0:
                    P.op("act", lambda e, pm=pm, cc=cc, pos=pos, w=w: e.activation(uT[:, cc, pos:pos + w], pm[:, 0:w], AF.Copy),
                         reads=[pm], pw=[uT])
                else:
                    P.op("dve", lambda e, pm=pm, cc=cc, pos=pos, w=w: e.tensor_copy(uT[:, cc, pos:pos + w], pm[:, 0:w]),
                         reads=[pm], pw=[uT])
        if upd_ctx:
            UTc.begin()
        for (stream, t, pos) in ptiles:
            UT = UT_pool.next().begin() if stream == "lat" else None
            for g in range(2):
                pm = pb.next()

                def mm2(e, pm=pm, g=g, pos=pos):
                    for cc in range(2):
                        ins = e.matmul(pm[:, :], uT[:, 2 * g + cc, pos:pos + 128], cs[:, cc, :],
                                       start=(cc == 0), stop=(cc == 1))
                    return ins
                P.op("pe", mm2, reads=[uT, cs], writes=[pm])
                if stream == "lat":
                    ov = UT[:, :, g, :]
                    ob = UT
                else:
                    ov = UTc[:, t, :, g, :]
                    ob = UTc
                pv = pm.ap.rearrange("p (a b) -> p a b", a=2)
                ev += 1
                if ev % 2 == 0:
                    P.op("act", lambda e, ov=ov, pv=pv: e.activation(ov, pv, AF.Copy), reads=[pm], pw=[ob])
                else:
                    P.op("dve", lambda e, ov=ov, pv=pv: e.tensor_copy(ov, pv), reads=[pm], pw=[ob])
            if stream == "lat":
                p_r = pb.next()
                p_i = pb.next()
                uc = UT[:, 0, :, :].rearrange("p g f -> p (g f)")
                us = UT[:, 1, :, :].rearrange("p g f -> p (g f)")

                def mmA(e, p_r=p_r, p_i=p_i, uc=uc, us=us):
                    e.matmul(p_r[:, :], ca[:, 0, :], uc, start=True, stop=False)
                    e.matmul(p_r[:, :], ca[:, 1, :], us, start=False, stop=True)
                    e.matmul(p_i[:, :], ca[:, 1, :], uc, start=True, stop=False)
                    return e.matmul(p_i[:, :], ca[:, 2, :], us, start=False, stop=True)
                P.op("pe", mmA, reads=[UT, ca], writes=[p_r, p_i])
                Ysb = Ysb_pool.next().begin()
                P.op("act", lambda e, Ysb=Ysb, p_r=p_r: e.activation(Ysb[:, 0, :], p_r[:, :], AF.Copy), reads=[p_r], pw=[Ysb])
                P.op("dve", lambda e, Ysb=Ysb, p_i=p_i: e.tensor_copy(Ysb[:, 1, :], p_i[:, :]), reads=[p_i], pw=[Ysb])
                for jj in range(2):
                    P.dma("sp", Y[:, :, 2 * t + jj, fb * 512:(fb + 1) * 512], Ysb.ap[jj * 64:(jj + 1) * 64, :, :],
                          reads=[Ysb], writes=[self.dbuf(("Y", t, fb, jj))])
        if upd_ctx:
            for g in range(2):
                for half in range(2):
                    pm = pb.next()

                    def mmc(e, pm=pm, g=g, half=half):
                        n = 0
                        for tile in range(2):
                            for cs_ in range(2):
                                ins = e.matmul(pm[:, 0:256], UTc[:, tile, cs_, g, half * 128:(half + 1) * 128],
                                               d256[:, tile, cs_, :], start=(n == 0), stop=(n == 3))
                                n += 1
                        return ins
                    P.op("pe", mmc, reads=[UTc, d256], writes=[pm])
                    c = fb * 4 + g * 2 + half
                    P.op("dve", lambda e, pm=pm, c=c: e.tensor_copy(fTc[:, c, :], pm[:, 0:256]), reads=[pm], pw=[fTc])
        wz = wb_pool.next()
        self.load_w(wz, self.f_w_in[j, :, :, E + fb * 512:E + (fb + 1) * 512], 8, 512, stage)
        for (pos, w) in ttiles:
            zsb = zsb_pool.next().begin()
            for cc in range(4):
                pm = pb.next()

                def mmz(e, pm=pm, wz=wz, cc=cc, pos=pos, w=w):
                    for kc in range(8):
                        ins = e.matmul(pm[:, 0:w], wz[:, kc, cc * 128:(cc + 1) * 128], hT[:, kc, pos:pos + w],
                                       start=(kc == 0), stop=(kc == 7))
                    return ins
                P.op("pe", mmz, reads=[wz, hT], writes=[pm])
                P.op("act", lambda e, zsb=zsb, pm=pm, cc=cc, w=w: e.activation(zsb[:, cc, 0:w], pm[:, 0:w], AF.Silu),
                     reads=[pm], pw=[zsb])
            P.dma("sp", ZS[fb * 4:(fb + 1) * 4, :, pos:pos + w].rearrange("c p t -> p c t"), zsb.ap[:, :, 0:w],
                  reads=[zsb], writes=[self.dbuf(("ZS", fb, pos))])
    self.barrier()
    A.release(self.m_hT)
    wout = A.alloc("wout", [16, D], BF16)
    stage = Pool([A.alloc("wstg%d" % i, [1024], F32) for i in range(2)])
    self.load_w(wout, self.f_w_out[j], 16, D, stage, engs=("pool", "dve"))
    bk = A.alloc("bk", [64, 64], BF16)
    P.dma("sp", bk[:], self.c_bk[:, :, :], writes=[bk])
    zs_pool = Pool([A.alloc("zs%d" % i, [16, 512], BF16) for i in range(2)])
    Yk_pool = Pool([A.alloc("Yk%d" % i, [E], BF16) for i in range(9)])
    yT_pool = Pool([A.alloc("yT%d" % i, [16, 512], BF16) for i in range(2)])
    xt_pool = Pool([A.alloc("fx%d" % i, [D], F32) for i in range(2)])
    xn_pool = Pool([A.alloc("fxn%d" % i, [D], F32) for i in range(2)])
    keyx = "xs" if x_src is self.xs else "xin"

    def out_stage(yT, width_off, stream, t, gb, src, key_in, key_out, dst):
        xt = xt_pool.next()
        self.load_rows("sp", xt, stream, t, stream == "lat", src, key_in)
        xn = xn_pool.next().begin()
        for half in range(2):
            po = pb.next()

            def mmo(e, po=po, half=half):
                for c in range(16):
                    ins = e.matmul(po[:, :], yT[:, c, width_off:width_off + 128], wout[:, c, half * 512:(half + 1) * 512],
                                   start=(c == 0), stop=(c == 15))
                return ins
            P.op("pe", mmo, reads=[yT, wout], writes=[po])
            hs = slice(half * 512, (half + 1) * 512)
            P.op("dve", lambda e, po=po, hs=hs: e.tensor_tensor(xn[:, hs], po[:, :], gb[:, hs], ALU.mult),
                 reads=[po, gb], pw=[xn])
        P.op("pool", lambda e: e.tensor_tensor(xn[:], xn[:], xt[:], ALU.add), reads=[xt], writes=[xn])
        self.store_rows("pool", xn, stream, t, stream == "lat", dst, key_out)

    for st in range(N // 512):
        pos = CTX + st * 512
        zs = zs_pool.next()
        P.dma("sp", zs[:], ZS[:, :, pos:pos + 512].rearrange("c p t -> p c t"), writes=[zs])
        Yks = []
        for kk in range(8):
            k1 = st * 8 + kk
            Yk = Yk_pool.next()
            P.dma("sp", Yk[:], Y[k1].rearrange("r n f -> (r n) f"), writes=[Yk])
            Yks.append(Yk)
        yT = yT_pool.next().begin()
        for c in range(16):
            pf = pb.next()

            def mmf(e, pf=pf, c=c, Yks=Yks, st=st):
                for kk in range(8):
                    ins = e.matmul(pf[:, kk * 64:(kk + 1) * 64], Yks[kk][:, c * 128:(c + 1) * 128], bk[:, st * 8 + kk, :],
                                   start=True, stop=True)
                return ins
            P.op("pe", mmf, reads=Yks + [bk], writes=[pf])
            P.op("dve", lambda e, pf=pf, c=c, yT=yT, zs=zs: e.tensor_tensor(yT[:, c, :], pf[:, :], zs[:, c, :], ALU.mult),
                 reads=[pf, zs], pw=[yT])
        for i in range(4):
            out_stage(yT, i * 128, "lat", st * 4 + i, self.g_bc, x_src, keyx, "xs", self.xs)
    if upd_ctx:
        zs = zs_pool.next()
        P.dma("sp", zs.ap[:, :, 0:256], ZS[:, :, 0:256].rearrange("c p t -> p c t"), writes=[zs])
        yT = yT_pool.next()
        P.op("dve", lambda e: e.tensor_tensor(yT.ap[:, :, 0:256], fTc[:, :, :], zs.ap[:, :, 0:256], ALU.mult),
             reads=[fTc, zs], writes=[yT])
        keyc = "xc" if c_src is self.xcs else "cin"
        for t in range(2):
            out_stage(yT, t * 128, "ctx", t, self.gc_bc, c_src, keyc, "xc", self.xcs)


KB.fourier_layer = fourier_layer


def _bf(a):
    return np.ascontiguousarray(a.astype(np.float32)).astype(ml_dtypes.bfloat16)


def make_consts():
    c = {}
    c["identb"] = _bf(np.eye(128))
    p = np.arange(128)
    cc = np.arange(2)
    ch = (cc[None, :] * 128 + p[:, None])
    k = np.arange(256)
    ang = 2 * np.pi * ((ch[:, :, None] * k[None, None, :]) % 256) / 256.0
    c["c_cs"] = _bf(np.concatenate([np.cos(ang), np.sin(ang)], axis=-1) / 16.0)
    n1 = np.arange(64)
    a64 = 2 * np.pi * ((n1[:, None] * n1[None, :]) % 64) / 64.0
    C, S = np.cos(a64), np.sin(a64)
    Z = np.zeros((64, 64))
    bd = lambda M: np.block([[M, Z], [Z, M]])
    c["c_a"] = _bf(np.stack([bd(C), bd(-S), bd(-C)], axis=1))
    n2 = np.arange(64)[:, None, None]
    k1 = np.arange(64)[None, :, None]
    k2 = np.arange(64)[None, None, :]
    num = (n2 * k2 * 64 + n2 * k1) % 4096
    th = 2 * np.pi * num / 4096.0
    c["c_bk"] = _bf(np.concatenate([np.cos(th), np.sin(th)], axis=0) / 64.0)
    n = (np.arange(2)[None, :] * 128 + p[:, None])
    a256 = 2 * np.pi * ((n[:, :, None] * k[None, None, :]) % 256) / 256.0
    c["c_d256"] = _bf(np.stack([np.cos(a256), -np.sin(a256)], axis=2) / 16.0)
    return c


def _pk(w, kc):
    n = w.shape[-1]
    return np.ascontiguousarray(w.reshape(kc, 128, n).transpose(1, 0, 2))


def host_shared(inp):
    f = lambda a: np.asarray(a, dtype=np.float32)
    sh = {}
    sh["norm_g"] = f(inp["norm_g"])
    sh["w_ada"] = np.stack([_pk(f(inp["w_ada"][l]), 8) for l in range(4)])
    sh["b_ada"] = f(inp["b_ada"])
    sh["f_w_in"] = np.stack([_pk(f(inp["f_w_in"][l]), 8) for l in range(2)])
    sh["f_w_out"] = np.stack([_pk(f(inp["f_w_out"][l]), 16) for l in range(2)])
    sh["norm_f"] = f(inp["norm_f"]).reshape(1, D)
    sh.update(make_consts())
    sh.update(host_shared_mlstm(inp))
    return sh


def host_shared_mlstm(inp):
    return {}


def host_core(inp, b):
    f = lambda a: np.asarray(a, dtype=np.float32)
    d = {}
    d["x"] = np.ascontiguousarray(f(inp["x"][b]))
    d["ctx"] = np.ascontiguousarray(f(inp["ctx"][b]))
    cv = np.stack([f(inp["c"][b]), f(inp["c_ctx"])], axis=-1)
    d["cvec"] = np.ascontiguousarray(cv.reshape(8, 128, 2).transpose(1, 0, 2))
    return d


def declare_mlstm_inputs(self):
    self.m_w_in = self.inp("m_w_in", [2, 128, 8, 3 * E])
    self.m_conv_w = self.inp("m_conv_w", [2, 128, 16, 9])
    self.m_vecs = self.inp("m_vecs", [2, 128, 3, 16])
    self.m_w_q = self.inp("m_w_q", [2, 4, 128, 4, DH])
    self.m_w_k = self.inp("m_w_k", [2, 4, 128, 4, DH])
    self.m_w_if = self.inp("m_w_if", [2, 128, 3, 16, 16])
    self.m_bif = self.inp("m_bif", [2, 16])
    self.m_w_out = self.inp("m_w_out", [2, 128, 16, D])
    self.c_tri = self.inp("c_tri", [128, 3, 128])
    self.c_mask = self.inp("c_mask", [128, 2, 128])


KB.declare_mlstm_inputs = declare_mlstm_inputs


def host_shared_mlstm(inp):
    f = lambda a: np.asarray(a, dtype=np.float32)
    sh = {}
    sh["m_w_in"] = np.stack([_pk(f(inp["m_w_in"][l]), 8) for l in range(2)])
    cw = f(inp["m_conv_w"]).reshape(2, 9, E)
    sh["m_conv_w"] = np.ascontiguousarray(cw.reshape(2, 9, 16, 128).transpose(0, 3, 2, 1))
    vec = np.stack([f(inp["m_conv_b"]), f(inp["m_ln_w"]), f(inp["m_skip"])], axis=1)
    sh["m_vecs"] = np.ascontiguousarray(vec.reshape(2, 3, 16, 128).transpose(0, 3, 1, 2))
    sh["m_w_q"] = np.stack([np.stack([_pk(f(inp["m_w_q"][l][h]), 4) for h in range(4)]) for l in range(2)])
    sh["m_w_k"] = np.stack([np.stack([_pk(f(inp["m_w_k"][l][h]), 4) for h in range(4)]) for l in range(2)])
    wif = f(inp["m_w_if"])
    wif = wif.reshape(2, 2, 3, 16, 128, 8).transpose(0, 4, 2, 3, 1, 5)
    sh["m_w_if"] = np.ascontiguousarray(wif.reshape(2, 128, 3, 16, 16))
    bif = np.concatenate([f(inp["m_b_i"]), f(inp["m_b_f"])], axis=-1)
    sh["m_bif"] = np.ascontiguousarray(bif.reshape(2, 16))
    sh["m_w_out"] = np.stack([_pk(f(inp["m_w_out"][l]), 16) for l in range(2)])
    s_ = np.arange(128)[:, None]
    j_ = np.arange(128)[None, :]
    mf = (s_ <= j_).astype(np.float32)
    mb = (s_ >= j_).astype(np.float32)
    sh["c_tri"] = np.ascontiguousarray(np.stack([-mf, -mb, -np.ones((128, 128), np.float32)], axis=1))
    sh["c_mask"] = np.ascontiguousarray(np.stack([mf, mb], axis=1))
    return sh


def out_stage(self, yT, off, stream, t, fourier, gb, src, key_in, key_out, dst, wout, xt_pool, xn_pool, pb):
    P = self.P
    xt = xt_pool.next()
    self.load_rows("sp", xt, stream, t, fourier, src, key_in)
    xn = xn_pool.next().begin()
    for half in range(2):
        po = pb.next()

        def mmo(e, po=po, half=half):
            for c in range(16):
                ins = e.matmul(po[:, :], yT[:, c, off:off + 128], wout[:, c, half * 512:(half + 1) * 512],
                               start=(c == 0), stop=(c == 15))
            return ins
        P.op("pe", mmo, reads=[yT, wout], writes=[po])
        hs = slice(half * 512, (half + 1) * 512)
        P.op("dve", lambda e, po=po, hs=hs: e.tensor_tensor(xn[:, hs], po[:, :], gb[:, hs], ALU.mult),
             reads=[po, gb], pw=[xn])
    P.op("pool", lambda e: e.tensor_tensor(xn[:], xn[:], xt[:], ALU.add), reads=[xt], writes=[xn])
    self.store_rows("pool", xn, stream, t, fourier, dst, key_out)


KB.out_stage = out_stage


def mlstm_layer(self, li, x_src, c_src, upd_ctx):
    P, A = self.P, self.A
    hT = self.hT
    N, T = self.N, self.T
    R = N // GW
    j = li // 2
    NTT = T // 128
    pb = Pool(self.pbank)
    S = self.scr.get(j)
    if S is None:
        S = {}
        S["OZX"] = self.scratch("OZX%d" % j, [3, NTT, 128, 16, 128], BF16)
        S["QT"] = self.scratch("QT%d" % j, [NTT, 128, 4, 4, 128], BF16)
        S["KT"] = self.scratch("KT%d" % j, [NTT, 128, 4, 4, 128], BF16)
        S["Kt"] = self.scratch("Kt%d" % j, [T, E], BF16)
        S["Vt"] = self.scratch("Vt%d" % j, [T, E], BF16)
        S["HF"] = self.scratch("HF%d" % j, [T, E], BF16)
        self.scr[j] = S
    m0 = A.mark()
    G = self.G
    bif = self.bif
    P.dma("sp", bif[:], self.m_bif[j].partition_broadcast(128), writes=[bif])
    mB = A.mark()
    cw = A.alloc("cw", [16, 9], F32)
    vecs = A.alloc("vecs", [3, 16], F32)
    P.dma("sp", cw[:], self.m_conv_w[j], writes=[cw])
    P.dma("sp", vecs[:], self.m_vecs[j], writes=[vecs])
    wif32 = A.alloc("wif32", [3, 16, 16], F32)
    wif = A.alloc("wif", [3, 16, 16], BF16)
    P.dma("sp", wif32[:], self.m_w_if[j], writes=[wif32])
    P.op("pool", lambda e: e.tensor_copy(wif[:], wif32[:]), reads=[wif32], writes=[wif])
    stage = Pool([A.alloc("mstg%d" % i, [1024], F32) for i in range(2)])
    wb_pool = Pool([A.alloc("mwb%d" % i, [8, 512], BF16) for i in range(2)])
    wq = A.alloc("wq", [4, DH], BF16)
    wk = A.alloc("wk", [4, DH], BF16)
    LP = 260 + (R + 2) * 68
    xmpad = A.alloc("xmpad", [LP], BF16)
    vTc = A.alloc("vTc", [T], BF16)
    xcv = A.alloc("xcv", [4, T], BF16)
    dg_pool = Pool([A.alloc("dg%d" % i, [9, 128], BF16) for i in range(2)])
    tl_pool = Pool([A.alloc("tl%d" % i, [512], BF16) for i in range(4)])
    t32_pool = Pool([A.alloc("t32_%d" % i, [512], F32) for i in range(2)])
    qt_pool = Pool([A.alloc("qt%d" % i, [4, 512], BF16) for i in range(4)])
    xl = xmpad.ap[:, 260:LP].rearrange("p (r c) -> p r c", c=68)
    P.op("pool", lambda e: e.memset(xmpad[:], 0.0), writes=[xmpad])
    ttiles = [(0, 256)] + [(CTX + i * 512, 512) for i in range(N // 512)]
    first_g = [True] * NTT
    ev = [0]

    def evac(out_ap, in_ap, reads, pw=(), writes=(), func=None, scale=None, bias=None):
        ev[0] += 1
        if func is not None or ev[0] % 2 == 0:
            kw = {}
            if scale is not None:
                kw["scale"] = scale
            if bias is not None:
                kw["bias"] = bias
            f = func if func is not None else AF.Copy
            return P.op("act", lambda e: e.activation(out_ap, in_ap, f, **kw), reads=reads, pw=pw, writes=writes)
        if scale is not None:
            return P.op("dve", lambda e: e.tensor_scalar(out_ap, in_ap, scale, None, ALU.mult), reads=reads, pw=pw, writes=writes)
        return P.op("dve", lambda e: e.tensor_copy(out_ap, in_ap), reads=reads, pw=pw, writes=writes)

    def gate_acc(pm, tt):
        if first_g[tt]:
            first_g[tt] = False
            P.op("dve", lambda e: e.tensor_tensor(G[:, tt, :], pm[:, 0:16], bif[:], ALU.add), reads=[pm, bif], pw=[G])
        else:
            P.op("dve", lambda e: e.tensor_tensor(G[:, tt, :], pm[:, 0:16], G[:, tt, :], ALU.add), reads=[pm], pw=[G])

    G.begin()
    import os
    stop = os.environ.get("MK_STOP", "")
    for hd in range(4):
        if stop.startswith("S") and hd > 0:
            return
        self.load_w(wq, self.m_w_q[j, hd], 4, DH, stage)
        self.load_w(wk, self.m_w_k[j, hd], 4, DH, stage)
        wxm = wb_pool.next()
        self.load_w(wxm, self.m_w_in[j, :, :, hd * 512:(hd + 1) * 512], 8, 512, stage)
        xcv.begin()
        if stop == "S0":
            return
        for cc in range(4):
            c = hd * 4 + cc
            xmpad.begin()
            vTc.begin()
            for (pos, w) in ttiles:
                pm = pb.next()

                def mm(e, pm=pm, cc=cc, pos=pos, w=w, wxm=wxm):
                    for kc in range(8):
                        ins = e.matmul(pm[:, 0:w], wxm[:, kc, cc * 128:(cc + 1) * 128], hT[:, kc, pos:pos + w],
                                       start=(kc == 0), stop=(kc == 7))
                    return ins
                P.op("pe", mm, reads=[wxm, hT], writes=[pm])
                if pos == 0:
                    ov = xmpad.ap[:, 2:258]
                    iv = pm[:, 0:256]
                else:
                    r0 = (pos - CTX) // 64
                    ov = xl[:, r0 + 1:r0 + 9, 2:66]
                    iv = pm.ap.rearrange("p (r c) -> p r c", c=64)
                P.op("act", lambda e, ov=ov, iv=iv: e.activation(ov, iv, AF.Copy), reads=[pm], pw=[xmpad])
                P.op("dve", lambda e, pm=pm, pos=pos, w=w: e.tensor_copy(vTc[:, pos:pos + w], pm[:, 0:w]), reads=[pm], pw=[vTc])
            if stop == "S1":
                return
            for tt in range(NTT):
                pm = pb.next()
                P.op("pe", lambda e, pm=pm, tt=tt, c=c: e.matmul(pm[:, 0:16], vTc[:, tt * 128:(tt + 1) * 128], wif[:, 2, c, :],
                                                                start=True, stop=True), reads=[vTc, wif], writes=[pm])
                gate_acc(pm, tt)
            if stop == "S2":
                return
            dg = dg_pool.next()

            def mkdg(e, dg=dg, c=c):
                for tap in range(9):
                    ins = e.tensor_scalar(dg[:, tap, :], self.ident[:], cw[:, c, tap:tap + 1], None, ALU.mult)
                return ins
            P.op("dve", mkdg, reads=[self.ident, cw], writes=[dg])
            for (pos, w) in ttiles:
                pm = pb.next()
                if pos == 0:
                    def mmc(e, pm=pm, dg=dg):
                        for k, dc in enumerate((-1, 0, 1)):
                            ins = e.matmul(pm[:, 0:256], dg[:, 3 + k, :], xmpad.ap[:, 2 + dc:258 + dc],
                                           start=(k == 0), stop=(k == 2))
                        return ins
                else:
                    r0 = (pos - CTX) // 64

                    def mmc(e, pm=pm, dg=dg, r0=r0):
                        n = 0
                        for dr in (-1, 0, 1):
                            for dc in (-1, 0, 1):
                                ins = e.matmul(pm[:, :], dg[:, 3 * (dr + 1) + (dc + 1), :],
                                               xl[:, r0 + 1 + dr:r0 + 9 + dr, 2 + dc:66 + dc],
                                               start=(n == 0), stop=(n == 8))
                                n += 1
                        return ins
                P.op("pe", mmc, reads=[dg, xmpad], writes=[pm])
                P.op("act", lambda e, pm=pm, cc=cc, pos=pos, w=w, c=c: e.activation(
                    xcv[:, cc, pos:pos + w], pm[:, 0:w], AF.Silu, bias=vecs[:, 0, c:c + 1]), reads=[pm, vecs], pw=[xcv])
                if stop == "S3":
                    continue
                tl = tl_pool.next()
                P.op("dve", lambda e, tl=tl, cc=cc, pos=pos, w=w, c=c: e.tensor_scalar(
                    tl[:, 0:w], xcv[:, cc, pos:pos + w], vecs[:, 2, c:c + 1], None, ALU.mult), reads=[xcv, vecs], writes=[tl])
                tt0 = pos // 128
                nt = w // 128
                P.dma("sp", S["OZX"][2, tt0:tt0 + nt, :, c, :].rearrange("t p k -> p t k"),
                      tl.ap[:, 0:w].rearrange("p (t k) -> p t k", k=128), reads=[tl],
                      writes=[self.dbuf(("OZX", j, 2, c, pos))])
        if stop in ("S3", "S4"):
            return
        for tt in range(NTT):
            pm = pb.next()

            def mmv(e, pm=pm, tt=tt, wxm=wxm):
                for kc in range(8):
                    ins = e.matmul(pm[:, :], hT[:, kc, tt * 128:(tt + 1) * 128], wxm[:, kc, :],
                                   start=(kc == 0), stop=(kc == 7))
                return ins
            P.op("pe", mmv, reads=[wxm, hT], writes=[pm])
            tl = tl_pool.next()
            evac(tl[:, :], pm[:, :], [pm], writes=[tl])
            P.dma("sp", S["Vt"][tt * 128:(tt + 1) * 128, hd * 512:(hd + 1) * 512], tl[:, :], reads=[tl],
                  writes=[self.dbuf(("Vt", j, tt, hd))])
        for kind in range(2):
            wz = wb_pool.next()
            col0 = E * (kind + 1) + hd * 512
            self.load_w(wz, self.m_w_in[j, :, :, col0:col0 + 512], 8, 512, stage)
            for cc in range(4):
                c = hd * 4 + cc
                for (pos, w) in ttiles:
                    pm = pb.next()

                    def mm(e, pm=pm, cc=cc, pos=pos, w=w, wz=wz):
                        for kc in range(8):
                            ins = e.matmul(pm[:, 0:w], wz[:, kc, cc * 128:(cc + 1) * 128], hT[:, kc, pos:pos + w],
                                           start=(kc == 0), stop=(kc == 7))
                        return ins
                    P.op("pe", mm, reads=[wz, hT], writes=[pm])
                    tl = tl_pool.next()
                    if kind == 0:
                        t32 = t32_pool.next()
                        P.op("act", lambda e, t32=t32, pm=pm, w=w: e.activation(t32[:, 0:w], pm[:, 0:w], AF.Sigmoid),
                             reads=[pm], writes=[t32])
                        P.op("dve", lambda e, tl=tl, t32=t32, w=w, c=c: e.tensor_scalar(
                            tl[:, 0:w], t32[:, 0:w], vecs[:, 1, c:c + 1], None, ALU.mult), reads=[t32, vecs], writes=[tl])
                    else:
                        P.op("act", lambda e, tl=tl, pm=pm, w=w: e.activation(tl[:, 0:w], pm[:, 0:w], AF.Silu),
                             reads=[pm], writes=[tl])
                    tt0 = pos // 128
                    nt = w // 128
                    P.dma("sp", S["OZX"][kind, tt0:tt0 + nt, :, c, :].rearrange("t p k -> p t k"),
                          tl.ap[:, 0:w].rearrange("p (t k) -> p t k", k=128), reads=[tl],
                          writes=[self.dbuf(("OZX", j, kind, c, pos))])
        for (pos, w) in ttiles:
            tt0 = pos // 128
            nt = w // 128
            qk = []
            for which, wmat, scl in ((0, wq, None), (1, wk, DH ** -0.5)):
                qt = qt_pool.next().begin()
                for ec in range(4):
                    pm = pb.next()

                    def mmq(e, pm=pm, ec=ec, pos=pos, w=w, wmat=wmat):
                        for dc in range(4):
                            ins = e.matmul(pm[:, 0:w], wmat[:, dc, ec * 128:(ec + 1) * 128], xcv[:, dc, pos:pos + w],
                                           start=(dc == 0), stop=(dc == 3))
                        return ins
                    P.op("pe", mmq, reads=[wmat, xcv], writes=[pm])
                    evac(qt[:, ec, 0:w], pm[:, 0:w], [pm], pw=[qt], scale=scl)
                dst = S["QT"] if which == 0 else S["KT"]
                for ec in range(4):
                    P.dma("sp", dst[tt0:tt0 + nt, :, hd, ec, :].rearrange("t p k -> p t k"),
                          qt.ap[:, ec, 0:w].rearrange("p (t k) -> p t k", k=128), reads=[qt],
                          writes=[self.dbuf(("QK", j, which, hd, ec, pos))])
                qk.append(qt)
            qt, kt = qk
            for sub in range(nt):
                tt = tt0 + sub
                sl = slice(sub * 128, (sub + 1) * 128)
                pm = pb.next()

                def mmk(e, pm=pm, sl=sl, pos=pos):
                    for dc in range(4):
                        ins = e.matmul(pm[:, :], xcv[:, dc, pos + sl.start:pos + sl.stop], wk[:, dc, :],
                                       start=(dc == 0), stop=(dc == 3))
                    return ins
                P.op("pe", mmk, reads=[wk, xcv], writes=[pm])
                tl = tl_pool.next()
                evac(tl[:, :], pm[:, :], [pm], writes=[tl], scale=DH ** -0.5)
                P.dma("sp", S["Kt"][tt * 128:(tt + 1) * 128, hd * 512:(hd + 1) * 512], tl[:, :], reads=[tl],
                      writes=[self.dbuf(("Kt", j, tt, hd))])
                pg = pb.next()

                def mmg(e, pg=pg, sl=sl, qt=qt, kt=kt, hd=hd):
                    n = 0
                    for src, buf in ((0, qt), (1, kt)):
                        for ec in range(4):
                            ins = e.matmul(pg[:, 0:16], buf[:, ec, sl], wif[:, src, hd * 4 + ec, :],
                                           start=(n == 0), stop=(n == 7))
                            n += 1
                    return ins
                P.op("pe", mmg, reads=[qt, kt, wif], writes=[pg])
                gate_acc(pg, tt)
    self.barrier()
    A.release(self.m_hT)
    import os
    self.stop = os.environ.get("MK_STOP", "")
    if self.stop == "B":
        return
    self.mlstm_scan(li, j, S, G, x_src, c_src, upd_ctx, pb)


KB.mlstm_layer = mlstm_layer


def mlstm_scan(self, li, j, S, G, x_src, c_src, upd_ctx, pb):
    P, A = self.P, self.A
    N, T = self.N, self.T
    NTT = T // 128
    NG = NTT * 4
    tri = A.alloc("tri", [3, 128], F32)
    mask = A.alloc("mask", [2, 128], F32)
    P.dma("sp", tri[:], self.c_tri[:, :, :], writes=[tri])
    P.dma("sp", mask[:], self.c_mask[:, :, :], writes=[mask])
    SP = A.alloc("SP", [2, NTT, 4], F32)
    TA = A.alloc("TA", [2, NTT, 4], F32)
    AA = A.alloc("AA", [2, NTT, 4], F32)
    WW = A.alloc("WW", [2, NTT, 4], F32)
    ENB = A.alloc("ENB", [2, NTT, 4], F32)
    EBL = A.alloc("EBL", [2, NTT, 4], F32)
    Gv = G.ap.rearrange("p t (r g) -> p r t g", r=2)
    for b_ in (SP, TA, AA, WW, ENB, EBL):
        b_.begin()
    for r in range(2):
        fpre = Gv[:, r, :, 4:8]
        liv = Gv[:, r, :, 0:4]
        P.op("act", lambda e, r=r, fpre=fpre: e.activation(SP[:, r, :, :], fpre, AF.Exp, scale=-1.0), reads=[G], pw=[SP])
        P.op("act", lambda e, r=r: e.activation(SP[:, r, :, :], SP[:, r, :, :], AF.Ln, bias=1.0), reads=[SP], pw=[SP])
        pbm = pb.next()
        pbl = pb.next()
        spf = SP[:, r, :, :].rearrange("p t h -> p (t h)")
        P.op("pe", lambda e, pbm=pbm, spf=spf, r=r: e.matmul(pbm[:, 0:NG], tri[:, r, :], spf, start=True, stop=True),
             reads=[tri, SP], writes=[pbm])
        P.op("pe", lambda e, pbl=pbl, spf=spf: e.matmul(pbl[:, 0:NG], tri[:, 2, :], spf, start=True, stop=True),
             reads=[tri, SP], writes=[pbl])
        bv = pbm[:, 0:NG].rearrange("p (t h) -> p t h", h=4)
        blv = pbl[:, 0:NG].rearrange("p (t h) -> p t h", h=4)
        P.op("dve", lambda e, r=r, liv=liv, bv=bv: e.tensor_tensor(TA[:, r, :, :], liv, bv, ALU.subtract), reads=[G, pbm], pw=[TA])
        P.op("act", lambda e, r=r: e.activation(AA[:, r, :, :], TA[:, r, :, :], AF.Exp), reads=[TA], pw=[AA])
        P.op("dve", lambda e, r=r, blv=blv: e.tensor_tensor(TA[:, r, :, :], TA[:, r, :, :], blv, ALU.add), reads=[pbl, AA], pw=[TA])
        P.op("act", lambda e, r=r: e.activation(WW[:, r, :, :], TA[:, r, :, :], AF.Exp), reads=[TA], pw=[WW])
        P.op("act", lambda e, r=r, bv=bv: e.activation(ENB[:, r, :, :], bv, AF.Exp, scale=-1.0), reads=[pbm], pw=[ENB])
        P.op("act", lambda e, r=r, blv=blv: e.activation(EBL[:, r, :, :], blv, AF.Exp), reads=[pbl], pw=[EBL])
    if "dbgG" in self.dbg:
        dG = self.outp("dbgG", [128, NTT, 16])
        P.dma("sp", dG[:, :, :], G[:], reads=[G])
        for nm, bf_ in (("dbgAA", AA), ("dbgWW", WW), ("dbgENB", ENB), ("dbgEBL", EBL), ("dbgSP", SP)):
            dd = self.outp(nm, [128, 2, NTT, 4])
            P.dma("sp", dd[:, :, :, :], bf_[:], reads=[bf_])
    if self.stop == "G":
        return
    mS = A.mark()
    keyx = "xs" if x_src is self.xs else "xin"
    keyc = "xc" if c_src is self.xcs else "cin"
    for r in range(2):
        A.release(mS)
        C32 = A.alloc("C32", [4, 4, 513], F32)
        Cbf = A.alloc("Cbf", [4, 4, 514], BF16)
        P.op("pool", lambda e: e.memset(C32[:], 0.0))
        tk0 = P.op("pool", lambda e: e.memset(Cbf[:], 0.0))
        nld = 3 if r == 0 else 2
        q_pool = Pool([A.alloc("qc%d" % i, [16, 128], BF16) for i in range(nld)])
        k_pool = Pool([A.alloc("kc%d" % i, [16, 128], BF16) for i in range(nld)])
        K_pool = Pool([A.alloc("Kc%d" % i, [E], BF16) for i in range(nld)])
        V_pool = Pool([A.alloc("Vc%d" % i, [E], BF16) for i in range(nld)])
        WT_pool = Pool([A.alloc("WT%d" % i, [128], BF16) for i in range(2)])
        Va_pool = Pool([A.alloc("Va%d" % i, [514], BF16) for i in range(2)])
        Vw_pool = Pool([A.alloc("Vw%d" % i, [514], BF16) for i in range(2)])
        rr_pool = Pool([A.alloc("rr%d" % i, [2], F32) for i in range(4)])
        hF_pool = Pool([A.alloc("hF%d" % i, [4, 512], BF16) for i in range(2 if r == 0 else 1)])
        if r == 1:
            h32 = A.alloc("h32", [4, 512], F32)
            hn = A.alloc("hn", [4, 512], BF16)
            stats = A.alloc("stats", [4, 6], F32)
            mv = A.alloc("mv", [4, 2], F32)
            rs = A.alloc("rs", [4], F32)
            t1 = A.alloc("t1", [16, 128], F32)
            yT = A.alloc("yTm", [16, 128], BF16)
            ozx = [A.alloc("ozx%d" % i, [16, 128], BF16) for i in range(3)]
            wout = A.alloc("mwout", [16, D], BF16)
            stage = Pool([A.alloc("sstg%d" % i, [512], F32) for i in range(2)])
            self.load_w(wout, self.m_w_out[j], 16, D, stage, engs=("pool",))
            xt_pool = Pool([A.alloc("mx%d" % i, [D], F32) for i in range(2)])
            xn_pool = Pool([A.alloc("mxn%d" % i, [D], F32) for i in range(1)])
        order = [0, 1] + list(range(2, NTT)) if r == 0 else [1, 0] + list(range(NTT - 1, 1, -1))
        ctok = {hd: tk0 for hd in range(4)}
        CH = {}

        def chunk_loads(tt):
            is_ctx = tt < 2
            emit = (not is_ctx) or upd_ctx
            ch = {"emit": emit, "is_ctx": is_ctx}
            Kc = K_pool.next()
            Vc = V_pool.next()
            P.dma("sp", Kc[:], S["Kt"][tt * 128:(tt + 1) * 128, :], writes=[Kc])
            P.dma("sp", Vc[:], S["Vt"][tt * 128:(tt + 1) * 128, :], writes=[Vc])
            ch["Kc"], ch["Vc"] = Kc, Vc
            if emit:
                qc = q_pool.next()
                kc_ = k_pool.next()
                P.dma("sp", qc[:], S["QT"][tt].rearrange("p h e k -> p (h e) k"), writes=[qc])
                P.dma("sp", kc_[:], S["KT"][tt].rearrange("p h e k -> p (h e) k"), writes=[kc_])
                ch["qc"], ch["kc"] = qc, kc_
            CH[tt] = ch

        def stage_A(tt, hd, r=r):
            if hd == 0:
                chunk_loads(tt)
            ch = CH[tt]
            Vc = ch["Vc"]
            st = {}
            a_s = AA[:, r, tt, hd:hd + 1]
            w_s = WW[:, r, tt, hd:hd + 1]
            Vw = Vw_pool.next().begin()
            st["Vw"] = Vw
            st["small"] = small = pb.next()
            P.op("dve", lambda e: e.tensor_scalar(Vw[:, 0:512], Vc[:, hd * 512:(hd + 1) * 512], w_s, None, ALU.mult),
                 reads=[Vc, WW], pw=[Vw])
            P.op("pool", lambda e: e.tensor_copy(Vw[:, 512:513], w_s), reads=[WW], pw=[Vw])
            if ch["emit"]:
                qc, kc_ = ch["qc"], ch["kc"]
                Va = Va_pool.next().begin()
                st["Va"] = Va
                P.op("act", lambda e: e.activation(Va[:, 0:512], Vc[:, hd * 512:(hd + 1) * 512], AF.Copy, scale=a_s),
                     reads=[Vc, AA], pw=[Va])
                P.op("act", lambda e: e.activation(Va[:, 512:513], a_s, AF.Copy), reads=[AA], pw=[Va])

                def mms(e):
                    for ec in range(4):
                        ins = e.matmul(small[:, 0:128], kc_[:, hd * 4 + ec, :], qc[:, hd * 4 + ec, :],
                                       start=(ec == 0), stop=(ec == 3))
                    return ins
                P.op("pe", mms, reads=[qc, kc_], writes=[small])
                WT = WT_pool.next()
                st["WT"] = WT
                P.op("dve", lambda e: e.tensor_tensor(WT[:], small[:, 0:128], mask[:, r, :], ALU.mult),
                     reads=[small, mask], writes=[WT])
            return st

        def stage_B(tt, hd, st, r=r):
            ch = CH[tt]
            st["tok_mmn"] = None
            if not ch["emit"]:
                return
            if hd == 0:
                hF = hF_pool.next()
                ch["hF"] = hF
                if r == 1:
                    P.dma("sp", hF[:], S["HF"][tt * 128:(tt + 1) * 128, :].rearrange("p (h v) -> p h v", h=4), writes=[hF])
                    h32.begin()
                else:
                    hF.begin()
            hF = ch["hF"]
            qc = ch["qc"]
            small, WT, Va = st["small"], st["WT"], st["Va"]
            pd = small.ap[:, 128:132]
            pn = pb.next()

            def mmn(e):
                e.matmul(pn[:, :], WT[:], Va[:, 0:512], start=True, stop=False)
                for kc in range(4):
                    e.matmul(pn[:, :], qc[:, hd * 4 + kc, :], Cbf[:, hd, kc, 0:512], start=False, stop=(kc == 3))
                e.matmul(pd[:, 0:1], WT[:], Va[:, 512:513], start=True, stop=False)
                for kc in range(4):
                    ins = e.matmul(pd[:, 0:1], qc[:, hd * 4 + kc, :], Cbf[:, hd, kc, 512:513], start=False, stop=(kc == 3))
                return ins
            st["tok_mmn"] = P.op("pe", mmn, reads=[WT, Va, qc], writes=[pn, small], extra=[ctok.get(hd)])
            rr = rr_pool.next()
            P.op("act", lambda e: e.activation(rr[:, 0:1], pd[:, 0:1], AF.Abs), reads=[small], writes=[rr])
            P.op("dve", lambda e: e.tensor_scalar(rr[:, 0:1], rr[:, 0:1], ENB[:, r, tt, hd:hd + 1], None, ALU.max),
                 reads=[rr, ENB], writes=[rr])
            P.op("dve", lambda e: e.reciprocal(rr[:, 1:2], rr[:, 0:1]), reads=[rr], writes=[rr])
            if r == 0:
                P.op("act", lambda e: e.activation(hF[:, hd, :], pn[:, :], AF.Copy, scale=rr[:, 1:2]),
                     reads=[pn, rr], pw=[hF])
            else:
                P.op("dve", lambda e: e.scalar_tensor_tensor(h32[:, hd, :], pn[:, :], rr[:, 1:2], hF[:, hd, :], ALU.mult, ALU.add),
                     reads=[pn, rr, hF], pw=[h32])

        def stage_C(tt, hd, st, r=r):
            ch = CH[tt]
            Kc = ch["Kc"]
            small, Vw = st["small"], st["Vw"]
            psn = small.ap[:, 132:136]
            if tt != order[-1]:
                ebl = EBL[:, r, tt, hd:hd + 1]
                toks = []
                for kc in range(4):
                    pk = pb.next()
                    P.op("pe", lambda e, pk=pk, kc=kc: e.matmul(pk[:, :], Kc[:, hd * 512 + kc * 128:hd * 512 + (kc + 1) * 128],
                                                                Vw[:, 0:512], start=True, stop=True), reads=[Kc, Vw], writes=[pk])
                    tk = P.op("dve", lambda e, pk=pk, kc=kc: e.scalar_tensor_tensor(
                        C32[:, hd, kc, 0:512], C32[:, hd, kc, 0:512], ebl, pk[:, :], ALU.mult, ALU.add),
                        reads=[pk, EBL], extra=[ctok.get(hd)])
                    toks.append(tk)

                def mmsn(e):
                    for kc in range(4):
                        ins = e.matmul(psn[:, kc:kc + 1], Kc[:, hd * 512 + kc * 128:hd * 512 + (kc + 1) * 128], Vw[:, 512:513],
                                       start=True, stop=True)
                    return ins
                P.op("pe", mmsn, reads=[Kc, Vw], writes=[small])
                tk = P.op("dve", lambda e: e.scalar_tensor_tensor(
                    C32[:, hd, :, 512], C32[:, hd, :, 512], ebl, psn[:, 0:4], ALU.mult, ALU.add),
                    reads=[small, EBL], extra=[ctok.get(hd)])
                toks.append(tk)
                ctok[hd] = P.op("act", lambda e: e.activation(Cbf[:, hd, :, 0:513], C32[:, hd, :, :], AF.Copy),
                                extra=toks + [ctok.get(hd), st["tok_mmn"]])
            if hd == 0:
                run_pend(2)
            if hd == 2:
                run_pend(3)
            if hd == 3:
                run_pend(2)
                run_pend(3)
                chunk_end(tt)

        def chunk_end(tt, r=r):
            ch = CH[tt]
            if not ch["emit"]:
                return
            hF = ch["hF"]
            is_ctx = ch["is_ctx"]
            if r == 0:
                P.dma("sp", S["HF"][tt * 128:(tt + 1) * 128, :].rearrange("p (h v) -> p h v", h=4), hF[:], reads=[hF],
                      writes=[self.dbuf(("HF", j, tt))])
                return
            for i in range(3):
                P.dma("sp", ozx[i][:], S["OZX"][i, tt], writes=[ozx[i]])
            P.op("dve", lambda e: [e.bn_stats(stats[:, hd, :], h32[:, hd, :]) for hd in range(4)][-1], reads=[h32], writes=[stats])
            P.op("dve", lambda e: [e.bn_aggr(mv[:, hd, :], stats[:, hd, :]) for hd in range(4)][-1], reads=[stats], writes=[mv])
            P.op("act", lambda e: e.activation(rs[:], mv[:, :, 1], AF.Sqrt, bias=EPS), reads=[mv], writes=[rs])
            P.op("dve", lambda e: e.reciprocal(rs[:], rs[:]), reads=[rs], writes=[rs])
            hn.begin()
            for hd in range(4):
                P.op("dve", lambda e, hd=hd: e.tensor_scalar(hn[:, hd, :], h32[:, hd, :], mv[:, hd, 0:1], rs[:, hd:hd + 1],
                                                             ALU.subtract, ALU.mult), reads=[h32, mv, rs], pw=[hn])

            def part2():
                t1.begin()
                for half in range(2):
                    ptb = pb.next()
                    ptv = ptb.ap.bitcast(BF16)

                    def tr(e, ptv=ptv, half=half):
                        for c8 in range(8):
                            c = half * 8 + c8
                            ins = e.transpose(ptv[:, c8 * 128:(c8 + 1) * 128], hn[:, c // 4, (c % 4) * 128:(c % 4 + 1) * 128], self.ident[:])
                        return ins
                    P.op("pe", tr, reads=[hn, self.ident], writes=[ptb])
                    pv = ptv[:, 0:1024].rearrange("p (c t) -> p c t", c=8)
                    P.op("dve", lambda e, pv=pv, half=half: e.tensor_tensor(t1[:, half * 8:(half + 1) * 8, :], pv,
                                                                             ozx[0][:, half * 8:(half + 1) * 8, :], ALU.mult),
                         reads=[ptb, ozx[0]], pw=[t1])
                P.op("pool", lambda e: e.tensor_tensor(t1[:], t1[:], ozx[2][:], ALU.add), reads=[ozx[2]], writes=[t1])
                P.op("pool", lambda e: e.tensor_tensor(yT[:], t1[:], ozx[1][:], ALU.mult), reads=[t1, ozx[1]], writes=[yT])

            def part3():
                if is_ctx:
                    self.out_stage(yT, 0, "ctx", tt, False, self.gc_bc, c_src, keyc, "xc", self.xcs, wout, xt_pool, xn_pool, pb)
                else:
                    self.out_stage(yT, 0, "lat", tt - 2, False, self.g_bc, x_src, keyx, "xs", self.xs, wout, xt_pool, xn_pool, pb)
            pend[2] = part2
            pend[3] = part3

        pend = {2: None, 3: None}

        def run_pend(k):
            f = pend[k]
            if f is not None:
                pend[k] = None
                f()

        items = [(tt, hd) for tt in order for hd in range(4)]
        prev = None
        for it in items:
            st = stage_A(*it)
            if prev is not None:
                stage_B(prev[0], prev[1], prev[2])
                stage_C(prev[0], prev[1], prev[2])
            prev = (it[0], it[1], st)
        stage_B(prev[0], prev[1], prev[2])
        stage_C(prev[0], prev[1], prev[2])
        run_pend(2)
        run_pend(3)
        self.barrier()
        if self.stop == "P1":
            return


KB.mlstm_scan = mlstm_scan


_CACHE = {}
LAUNCH_GROUPS = ((0, 1, 2, 3),)


def _get_prog(N, layers, final):
    key = (N, layers, final)
    if key not in _CACHE:
        kb = KB(N=N, layers=layers, final=final, dbg=("xs", "xcs"))
        nc = kb.build()
        _CACHE[key] = (kb, nc)
    return _CACHE[key]


def kernel(**inputs):
    x = np.asarray(inputs["x"], dtype=np.float32)
    B, N, _ = x.shape
    sh = host_shared(inputs)
    cores = [host_core(inputs, b) for b in range(B)]
    out = None
    for gi, layers in enumerate(LAUNCH_GROUPS):
        final = (3 in layers)
        kb, nc = _get_prog(N, tuple(layers), final)
        in_maps = []
        for b in range(B):
            m = dict(sh)
            m.update(cores[b])
            in_maps.append({k: v for k, v in m.items() if k in kb.din})
        res = run_bass_kernel_spmd(nc, in_maps, core_ids=list(range(B)))
        for b in range(B):
            r = res.results[b]
            if final:
                continue
            cores[b]["x"] = np.asarray(r["xs"], dtype=np.float32)
            if any(l < 2 for l in layers):
                cores[b]["ctx"] = np.asarray(r["xcs"], dtype=np.float32)
        if final:
            out = np.stack([np.asarray(res.results[b]["out"], dtype=np.float32) for b in range(B)], axis=0)
    return out
```
